# Optimizing a Trainium2 kernel written in Bass

```python
import jax, jax.numpy as jnp
from jax import lax
import numpy as np

D_MODEL = 1024
BATCH = 2
SEQ = 16384
DEPTH = 2

GRID_W = 64
CTX_LEN = 256
N_MIXERS = 2
N_GLA_LAYERS = (DEPTH + 1) // 2
N_CONV_LAYERS = DEPTH // 2

GLA_HEADS = 4
GLA_DK = D_MODEL // 2
GLA_DV = D_MODEL
GLA_HK = GLA_DK // GLA_HEADS
GLA_HV = GLA_DV // GLA_HEADS
GLA_RANK = 16
GLA_TAU = 16.0
GLA_CHUNK = 64

CONV_WIDTH = 3

N_EXPERTS = 16
EC_CAPACITY_FACTOR = 2
D_EXPERT = 2 * D_MODEL

EPS = 1e-6

kernel_name = "hybrid_gla_shortconv_ecmoe_diffusion_prefix"


def rmsnorm(x, g):
    xf = x.astype(jnp.float32)
    y = xf * lax.rsqrt(jnp.mean(xf * xf, axis=-1, keepdims=True) + EPS)
    return (y * g.astype(jnp.float32)).astype(x.dtype)


def modulate(h, shift, scale):
    return h * (1 + scale) + shift


def _to_chunks(t):
    b, t_len, h, d = t.shape
    return t.reshape(b, t_len // GLA_CHUNK, GLA_CHUNK, h, d).transpose(1, 0, 3, 2, 4)


def gla_chunk_scan(q, k, v, log_a, s0):
    b, t_len, h, _ = q.shape
    qc, kc, vc, gc = _to_chunks(q), _to_chunks(k), _to_chunks(v), _to_chunks(log_a)
    tri = jnp.tril(jnp.ones((GLA_CHUNK, GLA_CHUNK), dtype=bool))[:, :, None]

    def step(state, inp):
        qi, ki, vi, gi = inp
        a_cum = jnp.cumsum(gi.astype(jnp.float32), axis=2)
        a_last = a_cum[:, :, -1:, :]
        diff = a_cum[:, :, :, None, :] - a_cum[:, :, None, :, :]
        decay = jnp.exp(jnp.where(tri, diff, -jnp.inf))
        scores = jnp.einsum('bhid,bhjd,bhijd->bhij', qi, ki, decay)
        o_intra = jnp.einsum('bhij,bhjv->bhiv', scores, vi)
        o_inter = jnp.einsum('bhid,bhdv->bhiv', qi * jnp.exp(a_cum), state)
        k_dec = ki * jnp.exp(a_last - a_cum)
        new_state = state * jnp.exp(a_last)[:, :, 0, :, None] + jnp.einsum('bhjd,bhjv->bhdv', k_dec, vi)
        return new_state, (o_intra + o_inter)

    s_final, o = lax.scan(step, s0, (qc, kc, vc, gc))
    o = o.transpose(1, 0, 3, 2, 4).reshape(b, t_len, h, GLA_HV)
    return o.astype(v.dtype), s_final


def gla_bidirectional(q, k, v, la_f, la_b, s0_f, s0_b):
    o_f, s_f = gla_chunk_scan(q, k, v, la_f, s0_f)
    flip = lambda t: jnp.flip(t, axis=1)
    o_b, s_b = gla_chunk_scan(flip(q), flip(k), flip(v), flip(la_b), s0_b)
    return o_f + flip(o_b), s_f, s_b


def gla_project(h, w_in, w_a2, b_a2):
    b, t_len, _ = h.shape
    sizes = [GLA_DK, GLA_DK, GLA_DV, GLA_DV, GLA_RANK, GLA_RANK]
    q, k, v, r, a_f, a_b = jnp.split(h @ w_in, list(np.cumsum(sizes)[:-1]), axis=-1)
    q = q.reshape(b, t_len, GLA_HEADS, GLA_HK) * (GLA_HK ** -0.5)
    k = k.reshape(b, t_len, GLA_HEADS, GLA_HK)
    v = v.reshape(b, t_len, GLA_HEADS, GLA_HV)

    def log_gate(a, d):
        z = (a @ w_a2[d] + b_a2[d]).astype(jnp.float32)
        return (jax.nn.log_sigmoid(z) / GLA_TAU).reshape(b, t_len, GLA_HEADS, GLA_HK)

    return q, k, v, r, log_gate(a_f, 0), log_gate(a_b, 1)


def gla_finish(o, r, g_norm, w_out):
    b, t_len = o.shape[:2]
    o = rmsnorm(o, g_norm) * jax.nn.silu(r).reshape(b, t_len, GLA_HEADS, GLA_HV)
    return o.reshape(b, t_len, GLA_DV) @ w_out


def gla_mixer(h_x, h_c, w_in, w_a2, b_a2, g_norm, w_out, need_ctx_out):
    b = h_x.shape[0]
    qc, kc, vc, rc, lfc, lbc = gla_project(h_c, w_in, w_a2, b_a2)
    s0 = jnp.zeros((b, GLA_HEADS, GLA_HK, GLA_HV), jnp.float32)
    o_c, s_cf, s_cb = gla_bidirectional(qc, kc, vc, lfc, lbc, s0, s0)
    qx, kx, vx, rx, lfx, lbx = gla_project(h_x, w_in, w_a2, b_a2)
    o_x, _, _ = gla_bidirectional(qx, kx, vx, lfx, lbx, s_cf, s_cb)
    y_x = gla_finish(o_x, rx, g_norm, w_out)
    y_c = gla_finish(o_c, rc, g_norm, w_out) if need_ctx_out else None
    return y_x, y_c


def shortconv_mixer(h, w_in, conv_k, w_out, n_seq, seg_len):
    bg, cg, v = jnp.split(h @ w_in, 3, axis=-1)
    u = (cg * v).reshape(n_seq, seg_len, D_MODEL)
    u = lax.conv_general_dilated(
        u, conv_k.reshape(CONV_WIDTH, 1, D_MODEL).astype(u.dtype),
        window_strides=(1,), padding=[((CONV_WIDTH - 1) // 2, (CONV_WIDTH - 1) // 2)],
        dimension_numbers=('NWC', 'WIO', 'NWC'), feature_group_count=D_MODEL)
    return (bg * u.reshape(h.shape)) @ w_out


def ec_moe(h, w_router, w_gate, w_up, w_down):
    b, t_len, d = h.shape
    cap = EC_CAPACITY_FACTOR * t_len // N_EXPERTS
    aff = jax.nn.softmax((h @ w_router).astype(jnp.float32), axis=-1)
    vals, idx = lax.top_k(jnp.swapaxes(aff, 1, 2), cap)
    idx_flat = idx.reshape(b, N_EXPERTS * cap)
    xe = jnp.take_along_axis(h, idx_flat[..., None], axis=1).reshape(b, N_EXPERTS, cap, d)
    hid = jax.nn.silu(jnp.einsum('becd,edf->becf', xe, w_gate)) * jnp.einsum('becd,edf->becf', xe, w_up)
    ye = jnp.einsum('becf,efd->becd', hid, w_down) * vals[..., None].astype(h.dtype)
    scatter = lambda y, i: jnp.zeros((t_len, d), y.dtype).at[i].add(y)
    return jax.vmap(scatter)(ye.reshape(b, N_EXPERTS * cap, d), idx_flat)


def setup_inputs(seed: int = 0) -> dict:
    key = jax.random.key(seed)
    ks = jax.random.split(key, 24)
    nrm = lambda k, shape, scale: jax.random.normal(k, shape, jnp.float32) * scale
    d = D_MODEL
    gla_in_width = 2 * GLA_DK + 2 * GLA_DV + 2 * GLA_RANK
    return {
        "x": nrm(ks[0], (BATCH, SEQ, d), 1.0),
        "c": nrm(ks[1], (BATCH, d), 1.0),
        "ctx": nrm(ks[2], (BATCH, CTX_LEN, d), 1.0),
        "c_ctx": nrm(ks[3], (d,), 1.0),
        "ada_w": nrm(ks[4], (DEPTH, d, 6 * d), 0.5 * d ** -0.5),
        "ada_b": nrm(ks[5], (DEPTH, 6 * d), 0.01),
        "norm_g": 1.0 + nrm(ks[6], (DEPTH, 2, d), 0.05),
        "gla_w_in": nrm(ks[7], (N_GLA_LAYERS, d, gla_in_width), d ** -0.5),
        "gla_w_a2": nrm(ks[8], (N_GLA_LAYERS, 2, GLA_RANK, GLA_DK), GLA_RANK ** -0.5),
        "gla_b_a2": nrm(ks[9], (N_GLA_LAYERS, 2, GLA_DK), 0.1),
        "gla_norm_g": 1.0 + nrm(ks[10], (N_GLA_LAYERS, GLA_HV), 0.05),
        "gla_w_out": nrm(ks[11], (N_GLA_LAYERS, GLA_DV, d), GLA_DV ** -0.5),
        "conv_w_in": nrm(ks[12], (N_CONV_LAYERS, d, 3 * d), d ** -0.5),
        "conv_k": nrm(ks[13], (N_CONV_LAYERS, CONV_WIDTH, d), CONV_WIDTH ** -0.5),
        "conv_w_out": nrm(ks[14], (N_CONV_LAYERS, d, d), d ** -0.5),
        "router_w": nrm(ks[15], (DEPTH, d, N_EXPERTS), d ** -0.5),
        "expert_w_gate": nrm(ks[16], (DEPTH, N_EXPERTS, d, D_EXPERT), d ** -0.5),
        "expert_w_up": nrm(ks[17], (DEPTH, N_EXPERTS, d, D_EXPERT), d ** -0.5),
        "expert_w_down": nrm(ks[18], (DEPTH, N_EXPERTS, D_EXPERT, d), D_EXPERT ** -0.5),
        "final_norm_g": 1.0 + nrm(ks[19], (d,), 0.05),
    }


def reference(x, c, ctx, c_ctx, ada_w, ada_b, norm_g, gla_w_in, gla_w_a2, gla_b_a2, gla_norm_g,
              gla_w_out, conv_w_in, conv_k, conv_w_out, router_w, expert_w_gate, expert_w_up,
              expert_w_down, final_norm_g):
    b, t_len, d = x.shape
    rows = t_len // GRID_W
    ctx_len = ctx.shape[1]
    for i in range(DEPTH):
        last = i == DEPTH - 1
        j = i // N_MIXERS
        is_gla = (i % N_MIXERS) == 0
        ctx_needed = is_gla or not last
        mod_x = jax.nn.silu(c) @ ada_w[i] + ada_b[i]
        mod_c = jax.nn.silu(c_ctx) @ ada_w[i] + ada_b[i]
        sh1x, sc1x, g1x, sh2x, sc2x, g2x = jnp.split(mod_x[:, None, :], 6, axis=-1)
        sh1c, sc1c, g1c, sh2c, sc2c, g2c = jnp.split(mod_c, 6, axis=-1)

        h_x = modulate(rmsnorm(x, norm_g[i, 0]), sh1x, sc1x)
        h_c = modulate(rmsnorm(ctx, norm_g[i, 0]), sh1c, sc1c) if ctx_needed else None
        if is_gla:
            y_x, y_c = gla_mixer(h_x, h_c, gla_w_in[j], gla_w_a2[j], gla_b_a2[j], gla_norm_g[j],
                                 gla_w_out[j], need_ctx_out=not last)
        else:
            y_x = shortconv_mixer(h_x, conv_w_in[j], conv_k[j], conv_w_out[j], b * rows, GRID_W)
            y_c = (shortconv_mixer(h_c, conv_w_in[j], conv_k[j], conv_w_out[j], b, ctx_len)
                   if not last else None)
        x = x + g1x * y_x

        h2x = modulate(rmsnorm(x, norm_g[i, 1]), sh2x, sc2x)
        x = x + g2x * ec_moe(h2x, router_w[i], expert_w_gate[i], expert_w_up[i], expert_w_down[i])

        if not last:
            ctx = ctx + g1c * y_c
            h2c = modulate(rmsnorm(ctx, norm_g[i, 1]), sh2c, sc2c)
            ctx = ctx + g2c * ec_moe(h2c, router_w[i], expert_w_gate[i], expert_w_up[i], expert_w_down[i])
    return rmsnorm(x, final_norm_g)
```

```python
import os
import numpy as np
import ml_dtypes
import concourse.bass as bass
import concourse.mybir as mybir
from concourse.bass_utils import run_bass_kernel_spmd

F32 = mybir.dt.float32
BF16 = mybir.dt.bfloat16
I32 = mybir.dt.int32
AF = mybir.ActivationFunctionType
ALU = mybir.AluOpType
AX = mybir.AxisListType

D = 1024
T = 16384
TL = 4096
NT = 32
NTA = 128
CTX = 256
RW = 1088
CAP = 2048
GROUPS = [[0, 1, 2, 3], [4, 5, 6, 7]]
NDS = 24
BIG = 1.0e6
NITER = 24


class Scope:
    ctr = 0

    def __init__(self, nc):
        self.nc = nc
        self.items = []

    def sb(self, name, shape, dt):
        Scope.ctr += 1
        t = self.nc.sbuf_tensor("sb%d_%s" % (Scope.ctr, name), list(shape), dt)
        self.items.append(t)
        return t.__enter__()

    def close(self):
        for t in reversed(self.items):
            t.__exit__(None, None, None)
        self.items = []


class Buf:
    __slots__ = ("name", "w", "r")

    def __init__(self, name):
        self.name = name
        self.w = {}
        self.r = {}


def _norm(items):
    out = []
    for it in items:
        if isinstance(it, tuple):
            out.append(it)
        else:
            out.append((it, None))
    return out


class TK:
    def __init__(self, nc):
        self.nc = nc
        self.E = {"pe": nc.tensor, "act": nc.scalar, "dve": nc.vector, "pool": nc.gpsimd, "sp": nc.sync}
        self.sem = {k: nc.semaphore("s_" + k).__enter__() for k in self.E}
        self.cnt = {k: 0 for k in self.E}
        self.dpool = {"sp": 16, "pool": 12, "act": 4}
        self.dsem = {q: [nc.semaphore("d%s%d" % (q, i)).__enter__() for i in range(n)] for q, n in self.dpool.items()}
        self.dcnt = {q: [0] * n for q, n in self.dpool.items()}
        self.dnext = {q: 0 for q in self.dpool}
        self.ded = {}
        self.waited = {k: {} for k in self.E}
        self.ncc = 0

    def _semobj(self, key):
        if key[0] == "e":
            return self.sem[key[1]]
        if key[0] == "d":
            return self.dsem[key[1]][key[2]]
        if key[0] == "x":
            return self.ded[key[1]][0]
        return key[1]

    def _deps(self, reads, writes):
        deps = []
        for b, p in reads:
            for q, d in b.w.items():
                if p is None or q is None or p == q:
                    deps.append(d)
        for b, p in writes:
            for q, d in b.w.items():
                if p is None or q is None or p == q:
                    deps.append(d)
            for q, dd in b.r.items():
                if p is None or q is None or p == q:
                    deps.extend(dd.items())
        return deps

    def _wait(self, eng, deps):
        E = self.E[eng]
        best = {}
        for k, v in deps:
            if k == ("e", eng) and v > self.cnt[eng]:
                continue
            if best.get(k, 0) < v:
                best[k] = v
        for k, v in best.items():
            if self.waited[eng].get(k, 0) < v:
                E.wait_ge(self._semobj(k), v)
                self.waited[eng][k] = v

    def _record(self, reads, writes, dep):
        k, v = dep
        for b, p in reads:
            dd = b.r.setdefault(p, {})
            if dd.get(k, 0) < v:
                dd[k] = v
        for b, p in writes:
            if p is None:
                b.w = {None: dep}
                b.r = {}
            else:
                b.w[p] = dep
                b.r.pop(p, None)

    def op(self, eng, reads, writes, emit, mark=True):
        reads = _norm(reads)
        writes = _norm(writes)
        self._wait(eng, self._deps(reads, writes))
        inst = emit(self.E[eng])
        dep = (("e", eng), self.cnt[eng] + 1)
        if mark:
            self.cnt[eng] += 1
            inst.then_inc(self.sem[eng], 1)
        self._record(reads, writes, dep)
        return inst

    def dma(self, q, reads, writes, emit, sem=None):
        reads = _norm(reads)
        writes = _norm(writes)
        self._wait(q, self._deps(reads, writes))
        inst = emit(self.E[q])
        if sem is not None:
            if sem not in self.ded:
                self.ded[sem] = [self.nc.semaphore("x_" + sem).__enter__(), 0]
            self.ded[sem][1] += 16
            inst.then_inc(self.ded[sem][0], 16)
            dep = (("x", sem), self.ded[sem][1])
        else:
            i = self.dnext[q]
            self.dnext[q] = (i + 1) % self.dpool[q]
            self.dcnt[q][i] += 16
            inst.then_inc(self.dsem[q][i], 16)
            dep = (("d", q, i), self.dcnt[q][i])
        self._record(reads, writes, dep)
        return inst

    def coll(self, kind, op, in_ap, out_ap, reads, writes, qos=None):
        reads = _norm(reads)
        writes = _norm(writes)
        self._wait("pool", self._deps(reads, writes))
        if self.ncc == 0:
            self.ccsem = self.nc.semaphore("ccsem").__enter__()
        self.ncc += 1
        kw = {} if qos is None else {"dma_qos": qos}
        self.nc.gpsimd.collective_compute(kind, op, replica_groups=GROUPS, ins=[in_ap], outs=[out_ap], **kw).then_inc(self.ccsem)
        self._record(reads, writes, (("c", self.ccsem), self.ncc))

    def barrier(self):
        deps = [(("e", k), self.cnt[k]) for k in self.E if self.cnt[k] > 0]
        deps += [(("d", q, i), self.dcnt[q][i]) for q in self.dpool for i in range(self.dpool[q]) if self.dcnt[q][i] > 0]
        deps += [(("x", n), v[1]) for n, v in self.ded.items()]
        for eng in self.E:
            self._wait(eng, deps)


def build(debug=None):
    nc = bass.Bass("TRN2", target_bir_lowering=False)
    tk = TK(nc)
    dbg_out = {}

    in_names = []
    nc._in_names = in_names

    def din(name, shape, dt=F32):
        in_names.append(name)
        return nc.dram_tensor(name, list(shape), dt, kind="ExternalInput")

    def dscr(name, shape, dt):
        return nc.dram_tensor(name, list(shape), dt)

    x_in = din("x", [TL, D])
    ctx_in = din("ctx", [CTX, D])
    cc_in = din("cc", [128, 8, 2])
    adaw_in = din("ada_w", [2, D, 6 * D])
    adab_in = din("ada_b", [2, 6 * D])
    ng_in = din("norm_g", [2, 2, D])
    fng_in = din("final_g", [D])
    wfm_in = din("wfm", [D, 288])
    wtm_in = din("wtm", [D, 640])
    a2_in = din("a2", [2, 32, 128])
    gn_in = din("gn", [128, 2])
    wo_in = din("wo", [256, D])
    ident_in = din("ident", [128, 128])
    tri_in = din("tri", [6, 128, 128])
    wg_in = din("wg", [2, 4, D, 2 * D])
    wu_in = din("wu", [2, 4, D, 2 * D])
    wd_in = din("wd", [2, 4, 2 * D, D])
    out_d = nc.dram_tensor("out", [TL, D], F32, kind="ExternalOutput")

    mod_d = dscr("mod_d", [2, 2, 6 * D], F32)
    NCH = 8
    h1_loc = [dscr("h1_loc%d" % k, [TL // NCH, D], BF16) for k in range(NCH)]
    h1_all = [dscr("h1_all%d" % k, [4 * TL // NCH, D], BF16) for k in range(NCH)]
    hc_d = dscr("hc_d", [CTX, D], BF16)
    of_d = dscr("of_d", [T, 256], F32)
    yp_d = [dscr("yp_d%d" % k, [4 * TL // NCH, D], BF16) for k in range(NCH)]
    ymix_d = [dscr("ymix_d%d" % k, [TL // NCH, D], BF16) for k in range(NCH)]
    x1_d = dscr("x1_d", [TL, D], F32)
    aff_loc = dscr("aff_loc", [128, NT, 16], F32)
    aff_all = dscr("aff_all", [4 * 128, NT, 16], F32)
    xe_d = [dscr("xe_d%d" % e, [CAP, RW], BF16) for e in range(4)]
    z_d = dscr("z_d", [T, D], BF16)

    fence_in = dscr("fence_in", [128, 64], BF16)
    fence_out = [dscr("fence_out%d" % k, [512, 64], BF16) for k in range(8)]
    fence_n = [0]

    def fence(buf):
        k = fence_n[0]
        fence_n[0] += 1
        tk.coll("AllGather", ALU.bypass, fence_in.ap(), fence_out[k].ap(), [], [buf])

    def loc_rows(lst, i):
        k, w = i // 4, i % 4
        return lst[k][w * 128:(w + 1) * 128, :]

    def all_rows(lst, gi):
        r, i = gi % 4, gi // 4
        k, w = i // 4, i % 4
        return lst[k][r * 512 + w * 128:r * 512 + (w + 1) * 128, :]

    wgb_d = [[dscr("wgb_%d_%d" % (l, e), [D, 2 * D], BF16) for e in range(4)] for l in range(2)]
    wub_d = [[dscr("wub_%d_%d" % (l, e), [D, 2 * D], BF16) for e in range(4)] for l in range(2)]
    wdb_d = [[dscr("wdb_%d_%d" % (l, e), [2 * D, D], BF16) for e in range(4)] for l in range(2)]
    BWB = Buf("wb16")

    B = {n: Buf(n) for n in ["mod_d", "h1_loc", "h1_all", "hc_d", "of_d", "yp_d", "ymix_d", "x1_d", "rows_loc",
                             "rows_all", "aff_loc", "aff_all", "xe_d", "z_d", "zs_d", "out"]}

    def dbg_dump(name, src, shape, dt):
        o = nc.dram_tensor("dbg_" + name, list(shape), dt, kind="ExternalOutput")
        dbg_out[name] = o
        ob = Buf("dbg_" + name)
        tk.dma("sp", [B[name]], [ob], lambda E: E.dma_start(out=o.ap(), in_=src.ap()))
        return ob

    def finish(extra):
        tk.barrier()
        return nc

    def sb(name, shape, dt):
        return nc.sbuf_tensor("sb_" + name, list(shape), dt).__enter__()

    ident_f = sb("ident_f", [128, 128], F32)
    ident = sb("ident", [128, 128], BF16)
    ones_f = sb("ones_f", [128, 128], F32)
    ones_b = sb("ones_b", [128, 128], BF16)
    Bc = Buf("consts")
    tk.dma("sp", [], [Bc], lambda E: E.dma_start(out=ident_f[:], in_=ident_in.ap()))
    tk.op("dve", [Bc], [Bc], lambda E: E.tensor_copy(out=ident[:], in_=ident_f[:]))
    tk.op("pool", [], [(Bc, "of")], lambda E: E.memset(ones_f[:], 1.0))
    tk.op("pool", [], [(Bc, "ob")], lambda E: E.memset(ones_b[:], 1.0))

    PS = [nc.psum_tensor("ps%d" % i, [128, 512], F32).__enter__() for i in range(8)]
    PB = [Buf("ps%d" % i) for i in range(8)]

    sc = Scope(nc)
    ccs = sc.sb("ccs", [128, 8, 2], F32)
    csil = sc.sb("csil", [128, 8, 2], F32)
    adab = sc.sb("adab", [1, 2, 6 * D], F32)
    modsb = sc.sb("modsb", [2, 2, 6 * D], F32)
    aw0 = sc.sb("aw0", [128, 8, 512], F32)
    aw1 = sc.sb("aw1", [128, 8, 512], F32)
    if True:
        bcs, baw, bmod, bab = Buf("ccs"), [Buf("aw0"), Buf("aw1")], Buf("modsb"), Buf("adab")
        aws = [aw0, aw1]
        tk.dma("sp", [], [bcs], lambda E: E.dma_start(out=ccs[:], in_=cc_in.ap()))
        tk.dma("sp", [], [bab], lambda E: E.dma_start(out=adab[:], in_=adab_in.ap().unsqueeze(0)))
        tk.op("act", [bcs], [(bcs, "s")], lambda E: E.activation(out=csil[:], in_=ccs[:], func=AF.Silu))
        kk0 = [0]

        def mod_layer(l):
            for n in range(12):
                k = kk0[0]
                kk0[0] += 1
                aw, bw = aws[k % 2], baw[k % 2]
                ps, pb = PS[k % 2], PB[k % 2]
                tk.dma("sp", [], [bw], lambda E: E.dma_start(
                    out=aw[:], in_=adaw_in[l, :, n * 512:(n + 1) * 512].rearrange("(kc p) f -> p kc f", p=128)))
                for kc in range(8):
                    tk.op("pe", [bw, (bcs, "s")], [pb] if kc == 0 else [],
                          lambda E: E.matmul(ps[0:2, :], lhsT=csil[:, kc, :], rhs=aw[:, kc, :], start=(kc == 0), stop=False),
                          mark=False)
                tk.op("pe", [bab, (Bc, "of")], [pb],
                      lambda E: E.matmul(ps[0:2, :], lhsT=ones_f[0:1, 0:2], rhs=adab[0:1, l, n * 512:(n + 1) * 512],
                                         start=False, stop=True))
                tk.op("act", [], [(bmod, k), pb], lambda E: E.activation(out=modsb[0:2, l, n * 512:(n + 1) * 512],
                                                                         in_=ps[0:2, :], func=AF.Copy))
            tk.dma("sp", [bmod], [(B["mod_d"], l)], lambda E: E.dma_start(out=mod_d[l, :, :], in_=modsb[0:2, l, :]))

        mod_layer(0)
    sc0 = sc
    if debug == "p0":
        dbg_dump("mod_d", mod_d, [2, 2, 6 * D], F32)
        return finish(None), dbg_out

    def load_row(q, dst, dbuf, src_ap):
        tk.dma(q, [B["mod_d"]], [dbuf], lambda E: E.dma_start(out=dst, in_=src_ap.partition_broadcast(128)))

    def mod_row(l, v, ci):
        return mod_d[l, v, ci * D:(ci + 1) * D]

    def make_gm(dst, dbuf, tmp, tbuf, l, v, ci_scale, s):
        load_row("sp", dst, dbuf, mod_row(l, v, ci_scale))
        tk.dma("sp", [], [tbuf], lambda E: E.dma_start(out=tmp, in_=ng_in[l, s, :].partition_broadcast(128)))
        tk.op("dve", [dbuf, tbuf], [dbuf], lambda E: E.scalar_tensor_tensor(
            out=dst, in0=dst, scalar=1.0, in1=tmp, op0=ALU.add, op1=ALU.mult))

    def rstd_of(ss_ap, out_ap, tmp_ap, rb, n):
        tk.op("act", [rb], [rb], lambda E: E.activation(out=tmp_ap, in_=ss_ap, func=AF.Ln, scale=1.0 / n, bias=1e-6))
        tk.op("act", [rb], [rb], lambda E: E.activation(out=out_ap, in_=tmp_ap, func=AF.Exp, scale=-0.5))

    sc = Scope(nc)
    gm = sc.sb("gm", [128, D], F32)
    sh = sc.sb("sh", [128, D], F32)
    gmc = sc.sb("gmc", [128, D], F32)
    shc = sc.sb("shc", [128, D], F32)
    tmpr = sc.sb("tmpr", [128, D], F32)
    xt0 = sc.sb("xt0", [128, D], F32)
    xt1 = sc.sb("xt1", [128, D], F32)
    sq = sc.sb("sq", [128, D], F32)
    st = sc.sb("st", [128, 8], F32)
    hb0 = sc.sb("hb0", [128, D], BF16)
    hb1 = sc.sb("hb1", [128, D], BF16)
    if True:
        bgm, bsh, bgmc, bshc, btmp = Buf("gm"), Buf("sh"), Buf("gmc"), Buf("shc"), Buf("tmpr")
        make_gm(gm[:], bgm, tmpr[:], btmp, 0, 0, 1, 0)
        load_row("sp", sh[:], bsh, mod_row(0, 0, 0))
        make_gm(gmc[:], bgmc, tmpr[:], btmp, 0, 1, 1, 0)
        load_row("sp", shc[:], bshc, mod_row(0, 1, 0))
        xts, bxt = [xt0, xt1], [Buf("xt0"), Buf("xt1")]
        hbs, bhb = [hb0, hb1], [Buf("hb0"), Buf("hb1")]
        htb = [sc.sb("htb%d" % k_, [128, D], BF16) for k_ in range(2)]
        bhtb = [Buf("htb0"), Buf("htb1")]
        bsq, bst = Buf("sq"), Buf("st")
        for k in range(NT + 2):
            xt, bx, hb, bh = xts[k % 2], bxt[k % 2], hbs[k % 2], bhb[k % 2]
            isc = k >= NT
            src = ctx_in[(k - NT) * 128:(k - NT + 1) * 128, :] if isc else x_in[k * 128:(k + 1) * 128, :]
            g_, s_, bg_, bs_ = (gmc, shc, bgmc, bshc) if isc else (gm, sh, bgm, bsh)
            tk.dma("sp", [], [bx], lambda E: E.dma_start(out=xt[:], in_=src))
            tk.op("act", [bx], [bsq, bst], lambda E: E.activation(out=sq[:], in_=xt[:], func=AF.Square, accum_out=st[:, 0:1]))
            rstd_of(st[:, 0:1], st[:, 2:3], st[:, 1:2], bst, D)
            tk.op("dve", [bx, bst, bg_], [bx], lambda E: E.scalar_tensor_tensor(
                out=xt[:], in0=xt[:], scalar=st[:, 2:3], in1=g_[:], op0=ALU.mult, op1=ALU.mult))
            tk.op("dve", [bx, bs_], [bh], lambda E: E.tensor_tensor(out=hb[:], in0=xt[:], in1=s_[:], op=ALU.add))
            psT = PS[k % 2][:].bitcast(BF16)
            for kc in range(8):
                tk.op("pe", [bh, Bc], [PB[k % 2]] if kc in (0, 7) else [],
                      lambda E: E.transpose(out=psT[:, kc * 128:(kc + 1) * 128], in_=hb[:, kc * 128:(kc + 1) * 128], identity=ident[:]),
                      mark=(kc == 7))
            hb, bh = htb[k % 2], bhtb[k % 2]
            tk.op("act", [], [bh, PB[k % 2]], lambda E: E.activation(out=hb[:], in_=psT, func=AF.Copy))
            if isc:
                tk.dma("sp", [bh], [B["hc_d"]], lambda E: E.dma_start(out=hc_d[(k - NT) * 128:(k - NT + 1) * 128, :], in_=hb[:]))
            else:
                tk.dma("sp", [bh], [(B["h1_loc"], k)], lambda E: E.dma_start(out=loc_rows(h1_loc, k), in_=hb[:]))
                if k % 4 == 3:
                    ck = k // 4
                    tk.coll("AllGather", ALU.bypass, h1_loc[ck].ap(), h1_all[ck].ap(), [(B["h1_loc"], t_) for t_ in range(4 * ck, 4 * ck + 4)], [(B["h1_all"], ck)])
    if debug == "p1a":
        dbg_dump("h1_loc", h1_loc, [TL, D], BF16)
        dbg_dump("hc_d", hc_d, [CTX, D], BF16)
        return finish(None), dbg_out
    mod_layer(1)
    tk.barrier()
    sc.close()
    sc0.close()
    if debug == "p1":
        dbg_dump("h1_all", h1_all[3], [2048, D], BF16)
        dbg_dump("hc_d", hc_d, [CTX, D], BF16)
        dbg_dump("mod_d", mod_d, [2, 2, 6 * D], F32)
        return finish(None), dbg_out

    sc = Scope(nc)
    wfm = sc.sb("wfm", [128, 8, 288], BF16)
    wtm = sc.sb("wtm", [128, 8, 640], BF16)
    wstg = sc.sb("wstg", [128, 8, 288], F32)
    a2s = sc.sb("a2s", [32, 2, 128], F32)
    tri = sc.sb("tri", [128, 6, 128], F32)
    wo_f = sc.sb("wo_f", [128, 2, D], F32)
    wo_b = sc.sb("wo_b", [128, 2, D], BF16)
    gns = sc.sb("gns", [128, 2], F32)
    g1r = sc.sb("g1r", [128, D], F32)
    hbA = sc.sb("hbA", [128, D], BF16)
    hbB = sc.sb("hbB", [128, D], BF16)
    hT = sc.sb("hT", [128, 8, 128], BF16)
    aT = sc.sb("aT", [32, 128], F32)
    e1 = sc.sb("e1", [128, 128], F32)
    l1 = sc.sb("l1", [128, 128], F32)
    EqT = sc.sb("EqT", [128, 128], F32)
    EkT = sc.sb("EkT", [128, 128], F32)
    Ekd = sc.sb("Ekd", [128, 128], F32)
    qt = sc.sb("qt", [128, 128], BF16)
    kt = sc.sb("kt", [128, 128], BF16)
    kd = sc.sb("kd", [128, 128], BF16)
    vs = sc.sb("vs", [128, 256], BF16)
    sT = sc.sb("sT", [128, 128], BF16)
    S = sc.sb("S", [128, 256], F32)
    Sb = sc.sb("Sb", [128, 256], BF16)
    osb = sc.sb("osb", [128, 256], F32)
    ofl = sc.sb("ofl", [128, 256], F32)
    osq = sc.sb("osq", [128, 256], F32)
    sr = sc.sb("sr", [128, 256], F32)
    ofin = sc.sb("ofin", [128, 256], BF16)
    ofT = sc.sb("ofT", [128, 2, 128], BF16)
    ypo = sc.sb("ypo", [128, D], BF16)
    st2 = sc.sb("st2", [128, 8], F32)
    if True:
        bw = Buf("gla_w")
        tk.dma("sp", [], [(bw, "stg")], lambda E: E.dma_start(out=wstg[:], in_=wfm_in.ap().rearrange("(kc p) f -> p kc f", p=128)))
        tk.op("act", [(bw, "stg")], [(bw, "fm")], lambda E: E.activation(out=wfm[:, :, 0:128], in_=wstg[:, :, 0:128], func=AF.Copy, scale=128.0 ** -0.5))
        tk.op("act", [(bw, "stg")], [(bw, "fm2")], lambda E: E.activation(out=wfm[:, :, 128:288], in_=wstg[:, :, 128:288], func=AF.Copy))
        tk.dma("pool", [], [(bw, "tm")], lambda E: E.dma_start(out=wtm[:], in_=wtm_in.ap().rearrange("(kc p) f -> p kc f", p=128)))
        tk.dma("sp", [], [(bw, "a2")], lambda E: E.dma_start(out=a2s[:], in_=a2_in.ap().rearrange("d r f -> r d f")))
        tk.dma("sp", [], [(bw, "tri")], lambda E: E.dma_start(out=tri[:], in_=tri_in.ap().rearrange("s p f -> p s f")))
        tk.dma("sp", [], [(bw, "gn")], lambda E: E.dma_start(out=gns[:], in_=gn_in.ap()))
        tk.dma("sp", [], [(bw, "wof")], lambda E: E.dma_start(out=wo_f[:], in_=wo_in.ap().rearrange("(vc p) f -> p vc f", p=128)))
        bg1 = Buf("g1r")
        load_row("sp", g1r[:], bg1, mod_row(0, 0, 2))
        for vc in range(2):
            tk.op("dve", [(bw, "wof"), (bw, "gn"), bg1], [(bw, "wob%d" % vc)], lambda E: E.scalar_tensor_tensor(
                out=wo_b[:, vc, :], in0=wo_f[:, vc, :], scalar=gns[:, vc:vc + 1], in1=g1r[:], op0=ALU.mult, op1=ALU.mult))
        def mk(name, shape, dt, n):
            return [sc.sb("%s_%d" % (name, k), shape, dt) for k in range(n)], [Buf("%s_%d" % (name, k)) for k in range(n)]
        hbs, bhb = mk("hbp", [128, D], BF16, 2)
        hTs, bhT = mk("hTp", [128, 8, 128], BF16, 6)
        qss, bqs = mk("qs", [128, 128], F32, 2)
        kss, bks = mk("ks", [128, 128], F32, 2)
        kms, bkm = mk("km", [128, 128], F32, 2)
        vss, bvs = mk("vsp", [128, 256], BF16, 3)
        srs, bsr = mk("srp", [128, 256], F32, 3)
        aTs, baT = mk("aTp", [32, 128], F32, 2)
        l1s, bl1 = mk("l1p", [128, 128], F32, 2)
        Eqs, bEq = mk("Eq", [128, 128], F32, 2)
        Eks, bEk = mk("Ek", [128, 128], F32, 2)
        Eds, bEd = mk("Ed", [128, 128], F32, 2)
        qts, bqt = mk("qtp", [128, 128], BF16, 2)
        kts, bkt = mk("ktp", [128, 128], BF16, 2)
        kds, bkd = mk("kdp", [128, 128], BF16, 2)
        sTs, bsT = mk("sTp", [128, 128], BF16, 2)
        osbs, bosb = mk("osbp", [128, 256], F32, 2)
        ofls, bofl = mk("oflp", [128, 256], F32, 6)
        ofins, bofin = mk("ofinp", [128, 256], BF16, 2)
        ofTs, bofT = mk("ofTp", [128, 2, 128], BF16, 2)
        ypos, bypo = mk("ypop", [128, D], BF16, 2)
        st2s, bst2 = mk("st2p", [128, 8], F32, 2)
        be1, bS, bSb, bosq = Buf("e1"), Buf("S"), Buf("Sb"), Buf("osq")
        recs, brec = mk("rec", [128, 896], F32, 6)
        ers = sc.sb("ers", [128, 256], F32)
        bers = Buf("ers")
        aTb, baTb = mk("aTb", [32, 128], F32, 2)
        e1b = sc.sb("e1b", [128, 128], F32)
        be1b = Buf("e1b")
        rec_d = dscr("rec_d", [NTA + 2, 128, 896], F32)
        Brec = Buf("rec_d")
        for k in range(2):
            tk.op("pool", [], [baTb[k]], lambda E: E.memset(aTb[k][:], 1.0))
        for k in range(2):
            tk.op("pool", [], [baT[k]], lambda E: E.memset(aTs[k][:], 1.0))
        WR = [(bw, "fm"), (bw, "fm2")]
        P = lambda b, nm: (PB[b], nm)
        precast = []
        if debug is None:
            for l in range(2):
                for e in range(4):
                    precast.append((wgb_d[l][e], wg_in[l, e, :, :]))
                    precast.append((wub_d[l][e], wu_in[l, e, :, :]))
                    precast.append((wdb_d[l][e], wd_in[l, e, :, :]))
        for dr in range(int(os.environ.get('KDR', '2'))):
            if dr == 0:
                seq = [("c", 0), ("c", 1)] + [("x", g) for g in range(NTA)]
            else:
                seq = [("c", 1), ("c", 0)] + [("x", g) for g in range(NTA - 1, -1, -1)]
            lastcol = 127 if dr == 0 else 0
            N = min(len(seq), int(os.environ.get('KNT', '1000')))

            def stage_l(n):
                kind, gi = seq[n]
                isc = kind == "c"
                p5 = n % 6
                ridx = (NTA + gi) if isc else gi
                if dr == 1:
                    tk.dma("sp", [(Brec, ridx)], [brec[p5]], lambda E: E.dma_start(out=recs[p5][:], in_=rec_d[ridx, :, :]))
                    if not isc:
                        tk.dma("sp", [(B["of_d"], gi)], [bofl[p5]], lambda E: E.dma_start(out=ofls[p5][:], in_=of_d[gi * 128:(gi + 1) * 128, :]))
                    return
                src = hc_d[gi * 128:(gi + 1) * 128, :] if isc else all_rows(h1_all, gi)
                sbuf_ = B["hc_d"] if isc else (B["h1_all"], gi // 16)
                tk.dma("sp", [sbuf_], [bhT[p5]], lambda E: E.dma_start(out=hTs[p5][:].rearrange("p k t -> p (k t)"), in_=src))

            def stage_a(n):
                kind, gi = seq[n]
                isc = kind == "c"
                p2, p3 = n % 2, n % 6
                rec, br = recs[p3], brec[p3]
                ridx = (NTA + gi) if isc else gi
                if dr == 1:
                    return
                hT_, bhT_ = hTs[p3], bhT[p3]
                for (c0, c1, m, o0) in ((0, 128, 128, 0), (128, 256, 128, 128), (256, 272, 16, 256), (272, 288, 16, 384)):
                    for kc in range(8):
                        tk.op("pe", [bhT_] + WR, [PB[1]] if kc in (0, 7) else [],
                              lambda E: E.matmul(PS[1][0:m, o0:o0 + 128], lhsT=wfm[:, kc, c0:c1], rhs=hT_[:, kc, :], start=(kc == 0), stop=(kc == 7)),
                              mark=(kc == 7))
                for kc in range(8):
                    tk.op("pe", [bhT_, (bw, "tm")], [PB[2]] if kc in (0, 7) else [],
                          lambda E: E.matmul(PS[2][:, 0:384], lhsT=hT_[:, kc, :], rhs=wtm[:, kc, 0:384], start=(kc == 0), stop=(kc == 7)),
                          mark=(kc == 7))
                if not isc:
                    for kc in range(8):
                        tk.op("pe", [bhT_, (bw, "tm")], [PB[3]] if kc in (0, 7) else [],
                              lambda E: E.matmul(PS[3][:, 0:256], lhsT=hT_[:, kc, :], rhs=wtm[:, kc, 384:640], start=(kc == 0), stop=(kc == 7)),
                              mark=(kc == 7))
                tk.op("act", [], [baT[p2], PB[1]], lambda E: E.activation(out=aTs[p2][0:16, :], in_=PS[1][0:16, 256:384], func=AF.Copy))
                tk.op("act", [], [baTb[p2], PB[1]], lambda E: E.activation(out=aTb[p2][0:16, :], in_=PS[1][0:16, 384:512], func=AF.Copy))
                tk.op("act", [], [(br, "q"), PB[1]], lambda E: E.activation(out=rec[:, 0:128], in_=PS[1][:, 0:128], func=AF.Copy))
                tk.op("dve", [], [(br, "k"), PB[1]], lambda E: E.tensor_copy(out=rec[:, 128:256], in_=PS[1][:, 128:256]))
                tk.op("dve", [], [(br, "km"), PB[2]], lambda E: E.tensor_copy(out=rec[:, 256:384], in_=PS[2][:, 0:128]))
                tk.op("act", [], [(br, "v"), PB[2]], lambda E: E.activation(out=rec[:, 512:640].bitcast(BF16), in_=PS[2][:, 128:384], func=AF.Copy))
                if not isc:
                    tk.op("act", [], [bers, PB[3]], lambda E: E.activation(out=ers[:], in_=PS[3][:, 0:256], func=AF.Exp, scale=-1.0))
                    tk.op("dve", [bers], [bers], lambda E: E.tensor_scalar(out=ers[:], in0=ers[:], scalar1=1.0, scalar2=1.0, op0=ALU.mult, op1=ALU.add))
                    tk.op("dve", [bers], [bers], lambda E: E.reciprocal(out=ers[:], in_=ers[:]))
                    tk.op("dve", [bers], [(br, "sr"), PB[3]], lambda E: E.tensor_tensor(out=rec[:, 640:896], in0=ers[:], in1=PS[3][:, 0:256], op=ALU.mult))
                tk.op("pe", [baT[p2], (bw, "a2")], [PB[0]],
                      lambda E: E.matmul(PS[0][:, 0:128], lhsT=aTs[p2][0:32, :], rhs=a2s[0:32, 0, :], start=True, stop=True), mark=False)
                tk.op("pe", [baTb[p2], (bw, "a2")], [PB[0]],
                      lambda E: E.matmul(PS[0][:, 128:256], lhsT=aTb[p2][0:32, :], rhs=a2s[0:32, 1, :], start=True, stop=True))
                tk.op("act", [], [be1, PB[0]], lambda E: E.activation(out=e1[:], in_=PS[0][:, 0:128], func=AF.Exp, scale=-1.0))
                tk.op("act", [], [be1b, PB[0]], lambda E: E.activation(out=e1b[:], in_=PS[0][:, 128:256], func=AF.Exp, scale=-1.0))
                tk.op("act", [be1], [bl1[p2]], lambda E: E.activation(out=l1s[p2][:], in_=e1[:], func=AF.Ln, bias=1.0))
                tk.op("act", [be1b], [(br, "l1b")], lambda E: E.activation(out=rec[:, 384:512], in_=e1b[:], func=AF.Ln, bias=1.0))
                tk.dma("sp", [br], [(Brec, ridx)], lambda E: E.dma_start(out=rec_d[ridx, :, :], in_=rec[:]))

            def stage_b(n):
                p2, p3 = n % 2, n % 6
                rec, br = recs[p3], brec[p3]
                l1ap = l1s[p2][:] if dr == 0 else rec[:, 384:512]
                l1b_ = bl1[p2] if dr == 0 else br
                tk.op("pe", [l1b_, (bw, "tri")], [PB[4]],
                      lambda E: E.matmul(PS[4][:, 0:128], lhsT=l1ap, rhs=tri[:, dr, :], start=True, stop=True), mark=False)
                tk.op("pe", [l1b_, (bw, "tri")], [PB[4]],
                      lambda E: E.matmul(PS[4][:, 128:256], lhsT=tri[:, 2 + dr, :], rhs=l1ap, start=True, stop=True))
                tk.op("act", [], [bEq[p2], PB[4]], lambda E: E.activation(out=Eqs[p2][:], in_=PS[4][:, 0:128], func=AF.Exp))
                tk.op("act", [], [bEk[p2], PB[4]], lambda E: E.activation(out=Eks[p2][:], in_=PS[4][:, 0:128], func=AF.Exp, scale=-1.0))
                tk.op("act", [], [bEd[p2], PB[4]], lambda E: E.activation(out=Eds[p2][:], in_=PS[4][:, 128:256], func=AF.Exp))
                tk.op("dve", [br, bEq[p2]], [bqt[p2]], lambda E: E.tensor_tensor(out=qts[p2][:], in0=rec[:, 0:128], in1=Eqs[p2][:], op=ALU.mult))
                tk.op("dve", [br, bEk[p2]], [bkt[p2]], lambda E: E.tensor_tensor(out=kts[p2][:], in0=rec[:, 128:256], in1=Eks[p2][:], op=ALU.mult))
                tk.op("dve", [br, bEd[p2]], [bkd[p2]], lambda E: E.tensor_tensor(out=kds[p2][:], in0=rec[:, 256:384], in1=Eds[p2][:], op=ALU.mult))

            def stage_c(n):
                kind, gi = seq[n]
                isc = kind == "c"
                p2, p3 = n % 2, n % 6
                qt, kt, kd = qts[p2], kts[p2], kds[p2]
                rec, br = recs[p3], brec[p3]
                vs_ap = rec[:, 512:640].bitcast(BF16)
                if n == 0:
                    tk.op("pool", [], [bS], lambda E: E.memset(S[:], 0.0))
                    tk.op("pool", [], [bSb], lambda E: E.memset(Sb[:], 0.0))
                if not isc:
                    tk.op("pe", [bkt[p2], bqt[p2]], [PB[5]],
                          lambda E: E.matmul(PS[5][:, 0:128], lhsT=kt[:], rhs=qt[:], start=True, stop=True))
                    tk.op("dve", [(bw, "tri")], [bsT[p2], PB[5]], lambda E: E.tensor_tensor(out=sTs[p2][:], in0=PS[5][:, 0:128], in1=tri[:, 4 + dr, :], op=ALU.mult))
                    tk.op("pe", [bsT[p2], br], [PB[5]],
                          lambda E: E.matmul(PS[5][:, 128:384], lhsT=sTs[p2][:], rhs=vs_ap, start=True, stop=False), mark=False)
                    tk.op("pe", [bqt[p2], bSb], [PB[5]],
                          lambda E: E.matmul(PS[5][:, 128:384], lhsT=qt[:], rhs=Sb[:], start=False, stop=True))
                tk.op("pe", [bkd[p2], br], [PB[6]],
                      lambda E: E.matmul(PS[6][:, 0:256], lhsT=kd[:], rhs=vs_ap, start=True, stop=True))
                tk.op("dve", [bEq[p2]], [bS, PB[6]], lambda E: E.scalar_tensor_tensor(
                    out=S[:], in0=S[:], scalar=Eqs[p2][:, lastcol:lastcol + 1], in1=PS[6][:, 0:256], op0=ALU.mult, op1=ALU.add))
                tk.op("act", [bS], [bSb], lambda E: E.activation(out=Sb[:], in_=S[:], func=AF.Copy))
                if isc:
                    return
                osb_, bosb_ = osbs[p2], bosb[p2]
                if dr == 0:
                    tk.op("act", [], [bosb_, PB[5]], lambda E: E.activation(out=osb_[:], in_=PS[5][:, 128:384], func=AF.Copy))
                    tk.dma("sp", [bosb_], [(B["of_d"], gi)], lambda E: E.dma_start(out=of_d[gi * 128:(gi + 1) * 128, :], in_=osb_[:]))
                    return
                ofl_, bofl_ = ofls[p3], bofl[p3]
                tk.op("dve", [bofl_], [bosb_, PB[5]], lambda E: E.tensor_tensor(out=osb_[:], in0=ofl_[:], in1=PS[5][:, 128:384], op=ALU.add))
                return

            def stage_d(n):
                kind, gi = seq[n]
                if kind == "c":
                    return
                p2, p3 = n % 2, n % 6
                rec, br = recs[p3], brec[p3]
                osb_, bosb_ = osbs[p2], bosb[p2]
                st2_, bst2_ = st2s[p2], bst2[p2]
                ofin_, bofin_, ofT_, bofT_, ypo_, bypo_ = ofins[p2], bofin[p2], ofTs[p2], bofT[p2], ypos[p2], bypo[p2]
                tk.op("act", [bosb_], [bosq, bst2_], lambda E: E.activation(out=osq[:], in_=osb_[:], func=AF.Square, accum_out=st2_[:, 0:1]))
                rstd_of(st2_[:, 0:1], st2_[:, 2:3], st2_[:, 1:2], bst2_, 256)
                tk.op("dve", [bosb_, bst2_, br], [bofin_], lambda E: E.scalar_tensor_tensor(
                    out=ofin_[:], in0=osb_[:], scalar=st2_[:, 2:3], in1=rec[:, 640:896], op0=ALU.mult, op1=ALU.mult))
                psT2 = PS[0][:].bitcast(BF16)
                for vc in range(2):
                    tk.op("pe", [bofin_, Bc], [PB[0]],
                          lambda E: E.transpose(out=psT2[:, 768 + vc * 128:768 + (vc + 1) * 128], in_=ofin_[:, vc * 128:(vc + 1) * 128], identity=ident[:]),
                          mark=(vc == 1))
                tk.op("act", [], [bofT_, PB[0]], lambda E: E.activation(out=ofT_[:].rearrange("p k t -> p (k t)"), in_=psT2[:, 768:1024], func=AF.Copy))
                for hf in range(2):
                    for vc in range(2):
                        tk.op("pe", [bofT_, (bw, "wob%d" % vc)], [PB[7]],
                              lambda E: E.matmul(PS[7][:, :], lhsT=ofT_[:, vc, :], rhs=wo_b[:, vc, hf * 512:(hf + 1) * 512], start=(vc == 0), stop=(vc == 1)),
                              mark=(vc == 1))
                    if hf == 0:
                        tk.op("act", [], [(bypo_, hf), PB[7]], lambda E: E.activation(out=ypo_[:, 0:512], in_=PS[7][:, :], func=AF.Copy))
                    else:
                        tk.op("dve", [], [(bypo_, hf), PB[7]], lambda E: E.tensor_copy(out=ypo_[:, 512:1024], in_=PS[7][:, :]))
                tk.dma("pool", [bypo_], [(B["yp_d"], gi)], lambda E: E.dma_start(out=all_rows(yp_d, gi), in_=ypo_[:]))
                if gi % 16 == 0:
                    ck = gi // 16
                    tk.coll("ReduceScatter", ALU.add, yp_d[ck].ap(), ymix_d[ck].ap(), [(B["yp_d"], g_) for g_ in range(16 * ck, 16 * ck + 16)], [(B["ymix_d"], ck)], qos="P1")

            for n in range(-4, N):
                if n + 4 < N:
                    stage_l(n + 4)
                if 0 <= n + 2 < N:
                    stage_a(n + 2)
                if 0 <= n + 1 < N:
                    stage_b(n + 1)
                if n >= 0:
                    stage_c(n)
                if dr == 1 and n >= 1:
                    stage_d(n - 1)
                if n >= 0 and n % 10 == 5 and precast:
                    dst, srcw = precast.pop(0)
                    tk._wait("pool", tk._deps([(bSb, None)], []))
                    tk.dma("pool", [], [], lambda E: E.dma_start(out=dst.ap(), in_=srcw), sem="precast")
            if dr == 1:
                stage_d(N - 1)
        while precast:
            dst, srcw = precast.pop(0)
            tk.dma("pool", [], [], lambda E: E.dma_start(out=dst.ap(), in_=srcw), sem="precast")
        if debug is None:
            BWB.w = {None: (("x", "precast"), tk.ded["precast"][1])}
            BWB.r = {}
    tk.barrier()
    sc.close()
    if debug == "p2":
        for k in range(NCH):
            o = nc.dram_tensor("dbg_ymix%d" % k, [512, D], BF16, kind="ExternalOutput")
            dbg_out["ymix%d" % k] = o
            tk.dma("sp", [B["ymix_d"]], [Buf("x")], lambda E: E.dma_start(out=o.ap(), in_=ymix_d[k].ap()))
        return finish(None), dbg_out

    cwin_in = din("cw_in", [D, 3 * D])
    ck_in = din("ck", [128, 8, 3])
    cwo_in = din("cw_out", [D, D])
    rw_in = din("router_w", [2, D, 16])
    sel_in = din("sel", [4, 16])
    tid_in = din("tid", [128, NT], I32)
    lu_in = din("lu", [2, 128, 128])

    NCR = 16
    rows_loc = [dscr("rows_loc%d" % k, [256, RW], BF16) for k in range(NCR)]
    rows_all = [dscr("rows_all%d" % k, [1024, RW], BF16) for k in range(NCR)]
    zs_c = [dscr("zs_c%d" % k, [512, D], BF16) for k in range(NCH)]

    def rows_all_tile(gi):
        r, i = gi // NT, gi % NT
        k, w = i // 2, i % 2
        return rows_all[k][r * 256 + w * 128:r * 256 + (w + 1) * 128, :]

    tid_sb = sb("tid_sb", [128, NT], I32)
    btid = Buf("tid")
    tk.dma("sp", [], [btid], lambda E: E.dma_start(out=tid_sb[:], in_=tid_in.ap()))
    lub = sb("lub", [128, 128], BF16)
    luf = sb("luf", [128, 128], F32)
    tk.dma("sp", [], [(Bc, "luf")], lambda E: E.dma_start(out=luf[:], in_=lu_in[0, :, :]))
    tk.op("dve", [(Bc, "luf")], [(Bc, "lub")], lambda E: E.tensor_copy(out=lub[:], in_=luf[:]))

    class TailCtx:
        pass

    chunk_order = [list(range(NCR))]

    def tail_setup(sc, l):
        c = TailCtx()
        c.l = l
        c.order = []
        c.deferred = []
        c.gm2 = sc.sb("gm2_%d" % l, [128, D], F32); c.sh2 = sc.sb("sh2_%d" % l, [128, D], F32)
        c.tmpr = sc.sb("tmpr2_%d" % l, [128, D], F32)
        c.bgm2, c.bsh2, c.btmp = Buf("gm2"), Buf("sh2"), Buf("tmpr2")
        make_gm(c.gm2[:], c.bgm2, c.tmpr[:], c.btmp, l, 0, 4, 1)
        load_row("sp", c.sh2[:], c.bsh2, mod_row(l, 0, 3))
        c.rwf = sc.sb("rwf%d" % l, [128, 8, 16], F32); c.rwb = sc.sb("rwb%d" % l, [128, 8, 16], BF16)
        c.brw = Buf("rw")
        tk.dma("sp", [], [(c.brw, "f")], lambda E: E.dma_start(out=c.rwf[:], in_=rw_in[l, :, :].rearrange("(kc p) e -> p kc e", p=128)))
        tk.op("dve", [(c.brw, "f")], [(c.brw, "b")], lambda E: E.tensor_copy(out=c.rwb[:], in_=c.rwf[:]))
        c.rowt = [sc.sb("rowt%d_%d" % (l, k), [128, RW], BF16) for k in range(2)]
        c.brow = [Buf("rowt0"), Buf("rowt1")]
        for k in range(2):
            tk.op("pool", [], [c.brow[k]], lambda E: E.memset(c.rowt[k][:], 0.0))
        c.sq = sc.sb("sq2_%d" % l, [128, D], F32); c.st = sc.sb("st2t_%d" % l, [128, 8], F32)
        c.bsq, c.bst = Buf("sq2"), Buf("st2t")
        c.hT2 = sc.sb("hT2_%d" % l, [128, 8, 128], BF16); c.bhT2 = Buf("hT2")
        c.ex = sc.sb("ex_%d" % l, [128, 16], F32); c.bex = Buf("ex")
        c.affs = sc.sb("affs_%d" % l, [128, NT, 16], F32); c.baff = Buf("affs")
        c.xw = sc.sb("xw_%d" % l, [128, D], F32); c.bxw = Buf("xw")
        return c

    def tail(c, xt, bx, i, desc=False, split=False):
        rowt, brow = c.rowt[i % 2], c.brow[i % 2]
        tk.op("act", [bx], [c.bsq, c.bst], lambda E: E.activation(out=c.sq[:], in_=xt, func=AF.Square, accum_out=c.st[:, 0:1]))
        rstd_of(c.st[:, 0:1], c.st[:, 2:3], c.st[:, 1:2], c.bst, D)
        tk.op("dve", [bx, c.bst, c.bgm2], [c.bxw], lambda E: E.scalar_tensor_tensor(
            out=c.xw[:], in0=xt, scalar=c.st[:, 2:3], in1=c.gm2[:], op0=ALU.mult, op1=ALU.mult))
        tk.op("dve", [c.bxw, c.bsh2], [(brow, "h")], lambda E: E.tensor_tensor(out=rowt[:, 0:D], in0=c.xw[:], in1=c.sh2[:], op=ALU.add))
        if not split:
            tail2(c, i, desc)

    def tail2(c, i, desc=False):
        rowt, brow = c.rowt[i % 2], c.brow[i % 2]
        psT = PS[0][:].bitcast(BF16)
        for kc in range(8):
            tk.op("pe", [(brow, "h"), Bc], [PB[0]] if kc in (0, 7) else [],
                  lambda E: E.transpose(out=psT[:, kc * 128:(kc + 1) * 128], in_=rowt[:, kc * 128:(kc + 1) * 128], identity=ident[:]),
                  mark=(kc == 7))
        tk.op("act", [PB[0]], [c.bhT2], lambda E: E.activation(out=c.hT2[:].rearrange("p k t -> p (k t)"), in_=psT, func=AF.Copy))
        for kc in range(8):
            tk.op("pe", [c.bhT2, (c.brw, "b")], [PB[7]] if kc in (0, 7) else [],
                  lambda E: E.matmul(PS[7][:, 0:16], lhsT=c.hT2[:, kc, :], rhs=c.rwb[:, kc, :], start=(kc == 0), stop=(kc == 7)),
                  mark=(kc == 7))
        tk.op("dve", [PB[7]], [c.bst], lambda E: E.tensor_reduce(out=c.st[:, 3:4], in_=PS[7][:, 0:16], axis=AX.X, op=ALU.max))
        tk.op("dve", [c.bst], [c.bst], lambda E: E.tensor_scalar(out=c.st[:, 4:5], in0=c.st[:, 3:4], scalar1=-1.0, scalar2=0.0, op0=ALU.mult, op1=ALU.add))
        tk.op("act", [PB[7], c.bst], [c.bex, c.bst], lambda E: E.activation(out=c.ex[:], in_=PS[7][:, 0:16], func=AF.Exp, bias=c.st[:, 4:5], accum_out=c.st[:, 5:6]))
        tk.op("dve", [c.bst], [c.bst], lambda E: E.reciprocal(out=c.st[:, 6:7], in_=c.st[:, 5:6]))
        tk.op("dve", [c.bex, c.bst], [(c.baff, i)], lambda E: E.tensor_scalar(out=c.affs[:, i, :], in0=c.ex[:], scalar1=c.st[:, 6:7], scalar2=0.0, op0=ALU.mult, op1=ALU.add))
        tk.op("dve", [(c.baff, i)], [(brow, "a")], lambda E: E.tensor_copy(out=rowt[:, D:D + 32].bitcast(F32), in_=c.affs[:, i, :]))
        tk.op("dve", [btid], [(brow, "t")], lambda E: E.tensor_copy(out=rowt[:, D + 32:D + 34].bitcast(I32), in_=tid_sb[:, i:i + 1]))
        k, w = i // 2, i % 2
        tk.dma("sp", [brow], [(B["rows_loc"], i)], lambda E: E.dma_start(out=rows_loc[k][w * 128:(w + 1) * 128, :], in_=rowt[:]))
        if w == (0 if desc else 1):
            if len(c.order) < 6:
                tk.coll("AllGather", ALU.bypass, rows_loc[k].ap(), rows_all[k].ap(), [(B["rows_loc"], 2 * k), (B["rows_loc"], 2 * k + 1)], [(B["rows_all"], k)])
            else:
                c.deferred.append(k)
            c.order.append(k)

    def tail_finish(c):
        tk.dma("sp", [c.baff], [B["aff_loc"]], lambda E: E.dma_start(out=aff_loc.ap(), in_=c.affs[:]))
        tk.coll("AllGather", ALU.bypass, aff_loc.ap(), aff_all.ap(), [B["aff_loc"]], [B["aff_all"]])
        for k in c.deferred:
            tk.coll("AllGather", ALU.bypass, rows_loc[k].ap(), rows_all[k].ap(), [(B["rows_loc"], 2 * k), (B["rows_loc"], 2 * k + 1)], [(B["rows_all"], k)])
        chunk_order[0] = list(c.order)

    sc = Scope(nc)
    tc0 = tail_setup(sc, 0)
    xa = [sc.sb("xa%d" % k, [128, D], F32) for k in range(2)]
    bxa = [Buf("xa0"), Buf("xa1")]
    ym = [sc.sb("ym%d" % k, [128, D], BF16) for k in range(2)]
    bym = [Buf("ym0"), Buf("ym1")]
    for i in reversed(range(NT)):
        xt, bx, yt, by = xa[i % 2], bxa[i % 2], ym[i % 2], bym[i % 2]
        tk.dma("sp", [], [bx], lambda E: E.dma_start(out=xt[:], in_=x_in[i * 128:(i + 1) * 128, :]))
        tk.dma("sp", [(B["ymix_d"], i // 4)], [by], lambda E: E.dma_start(out=yt[:], in_=loc_rows(ymix_d, i)))
        tk.op("dve", [bx, by], [bx], lambda E: E.tensor_tensor(out=xt[:], in0=xt[:], in1=yt[:], op=ALU.add))
        tk.dma("sp", [bx], [(B["x1_d"], i)], lambda E: E.dma_start(out=x1_d[i * 128:(i + 1) * 128, :], in_=xt[:]))
        tail(tc0, xt[:], bx, i, desc=True)
    tail_finish(tc0)
    tk.barrier()
    sc.close()

    r_cap = nc.gpsimd.alloc_register("r_cap")
    nc.gpsimd.reg_mov(r_cap, CAP - 1)
    r_tok = nc.gpsimd.alloc_register("r_tok")
    nc.gpsimd.reg_mov(r_tok, T - 1)

    def moe(l):
        sc = Scope(nc)
        Aall = sc.sb("Aall", [128, 128, 16], F32); tmpA = sc.sb("tmpA", [128, 128, 16], F32)
        selt = sc.sb("selt", [128, 4, 16], F32); Asel = sc.sb("Asel", [128, 4, 128], F32)
        cmp = sc.sb("cmp", [128, 4, 128], F32)
        bs = sc.sb("bs", [128, 8, 4], F32)
        Mexp = sc.sb("Mexp", [128, 4, 128], BF16); Xs = sc.sb("Xs", [128, 4, 128], BF16)
        offf = sc.sb("offf", [128, 4, 128], F32); offi = sc.sb("offi", [128, 4, 128], I32)
        zt = sc.sb("zt", [128, D], BF16)
        bA, bsel, bAs, bcmp, bbs, bM, bXs, boff, bzt = (Buf(n) for n in ("Aall", "selt", "Asel", "cmp", "bs", "Mexp", "Xs", "off", "zt"))
        tk.op("pool", [], [bzt], lambda E: E.memset(zt[:], 0.0))
        for g in range(NTA):
            tk.dma("sp", [bzt], [(B["z_d"], "z%d" % g)], lambda E: E.dma_start(out=z_d[g * 128:(g + 1) * 128, :], in_=zt[:]))
        tk.dma("sp", [B["aff_all"]], [bA], lambda E: E.dma_start(
            out=Aall[:].rearrange("q (k i) e -> q k i e", k=4), in_=aff_all.ap().rearrange("(k q) i e -> q k i e", q=128)))

        tk.dma("sp", [], [bsel], lambda E: E.dma_start(out=selt[:], in_=sel_in.ap().partition_broadcast(128)))
        for e in range(4):
            tk.op("dve", [bA, bsel], [Buf("t")] and [bcmp], lambda E: E.tensor_tensor(
                out=tmpA[:], in0=Aall[:], in1=selt[:, e, :].unsqueeze(1).broadcast_to([128, 128, 16]), op=ALU.mult))
            tk.op("dve", [bcmp], [(bAs, e)], lambda E: E.tensor_reduce(out=Asel[:, e, :], in_=tmpA[:], axis=AX.X, op=ALU.add))
        lo, hi, mid, cntp, ge, dd = (bs[:, k, :] for k in range(6))
        tk.op("pool", [], [bbs], lambda E: E.memset(bs[:], 0.0))
        tk.op("pool", [bbs], [bbs], lambda E: E.memset(bs[:, 1, :], 1.0))
        for it in range(NITER):
            tk.op("dve", [bbs], [bbs], lambda E: E.tensor_tensor(out=mid, in0=lo, in1=hi, op=ALU.add))
            tk.op("dve", [bbs], [bbs], lambda E: E.tensor_scalar(out=mid, in0=mid, scalar1=0.5, scalar2=0.0, op0=ALU.mult, op1=ALU.add))
            tk.op("dve", [bAs, bbs], [bcmp], lambda E: E.tensor_tensor(
                out=cmp[:], in0=Asel[:], in1=bs[:, 2, :].unsqueeze(2).broadcast_to([128, 4, 128]), op=ALU.is_gt))
            tk.op("dve", [bcmp], [bbs], lambda E: E.tensor_reduce(out=cntp, in_=cmp[:], axis=AX.X, op=ALU.add))
            tk.op("pe", [bbs, (Bc, "of")], [PB[7]], lambda E: E.matmul(PS[7][:, 0:4], lhsT=ones_f[:], rhs=cntp, start=True, stop=True))
            tk.op("dve", [PB[7]], [bbs], lambda E: E.tensor_scalar(out=ge, in0=PS[7][:, 0:4], scalar1=float(CAP), scalar2=0.0, op0=ALU.is_ge, op1=ALU.add))
            tk.op("dve", [bbs], [bbs], lambda E: E.tensor_tensor(out=dd, in0=mid, in1=lo, op=ALU.subtract))
            tk.op("dve", [bbs], [bbs], lambda E: E.tensor_tensor(out=dd, in0=dd, in1=ge, op=ALU.mult))
            tk.op("dve", [bbs], [bbs], lambda E: E.tensor_tensor(out=lo, in0=lo, in1=dd, op=ALU.add))
            tk.op("dve", [bbs], [bbs], lambda E: E.tensor_tensor(out=dd, in0=hi, in1=mid, op=ALU.subtract))
            tk.op("dve", [bbs], [bbs], lambda E: E.tensor_tensor(out=dd, in0=dd, in1=ge, op=ALU.mult))
            tk.op("dve", [bbs], [bbs], lambda E: E.tensor_tensor(out=hi, in0=mid, in1=dd, op=ALU.add))
        for e in range(4):
            tk.op("dve", [bAs, bbs], [(bM, e)], lambda E: E.tensor_scalar(
                out=Mexp[:, e, :], in0=Asel[:, e, :], scalar1=bs[:, 0, e:e + 1], scalar2=0.0, op0=ALU.is_gt, op1=ALU.add))
        for e in range(4):
            tk.op("pe", [bM, (Bc, "ob")], [PB[1]], lambda E: E.matmul(PS[1][:, e * 128:(e + 1) * 128], lhsT=Mexp[:, e, :], rhs=ones_b[:], start=True, stop=True),
                  mark=(e == 3))
        tk.op("act", [PB[1]], [bXs], lambda E: E.activation(out=Xs[:].rearrange("p e t -> p (e t)"), in_=PS[1][:, :], func=AF.Copy))
        tk.op("pe", [bM, (Bc, "lub")], [PB[2]], lambda E: E.matmul(PS[2][:, :], lhsT=lub[:], rhs=Mexp[:].rearrange("p e t -> p (e t)"), start=True, stop=False), mark=False)
        for e in range(4):
            tk.op("pe", [bXs, (Bc, "lub")], [PB[2]] if e == 3 else [], lambda E: E.matmul(PS[2][:, e * 128:(e + 1) * 128], lhsT=Xs[:, e, :], rhs=lub[:], start=False, stop=(e == 3)),
                  mark=(e == 3))
        tk.op("dve", [bM], [boff], lambda E: E.tensor_scalar(out=offf[:], in0=Mexp[:], scalar1=-BIG, scalar2=BIG, op0=ALU.mult, op1=ALU.add))
        tk.op("dve", [boff, PB[2]], [boff], lambda E: E.tensor_tensor(out=offf[:].rearrange("p e t -> p (e t)"), in0=offf[:].rearrange("p e t -> p (e t)"), in1=PS[2][:, :], op=ALU.add))
        tk.op("dve", [boff], [(boff, "i")], lambda E: E.tensor_copy(out=offi[:], in_=offf[:]))

        NRT = 10
        rt = [sc.sb("rt%d" % k, [128, RW], BF16) for k in range(NRT)]
        brt = [Buf("rt%d" % k) for k in range(NRT)]
        kctr = [0]

        gi_order = [r_ * NT + 2 * ck + w_ for ck in chunk_order[0] for r_ in range(4) for w_ in range(2)]

        def scatter_rows(e):
            for n_, gi in enumerate(gi_order):
                k = kctr[0] % NRT
                kctr[0] += 1
                tk.dma("sp", [(B["rows_all"], (gi % NT) // 2)], [brt[k]], lambda E: E.dma_start(out=rt[k][:], in_=rows_all_tile(gi)))
                tk.dma("pool", [brt[k], (boff, "i")] + ([] if n_ == 0 else [(B["xe_d"], e)]), [(B["xe_d"], e)] if n_ == 0 else [], lambda E: E.indirect_dma_start(
                    out=xe_d[e][:, :], out_offset=bass.IndirectOffsetOnAxis(ap=offi[:, e, gi:gi + 1], axis=0),
                    in_=rt[k][:], in_offset=None, bounds_check=r_cap, oob_is_err=False))

        Wg = sc.sb("Wg", [128, 8, 2 * D], BF16); Wu = sc.sb("Wu", [128, 8, 2 * D], BF16); Wd = sc.sb("Wd", [128, 16, D], BF16)
        bWg, bWu, bWd = Buf("Wg"), Buf("Wu"), Buf("Wd")
        XeT = sc.sb("XeT", [128, 8, 512], BF16); hidT = sc.sb("hidT", [128, 16, 512], BF16)
        bXeT, bhid = Buf("XeT"), Buf("hidT")
        xr = [sc.sb("xr%d" % k, [128, RW], BF16) for k in range(4)]
        bxr = [Buf("xr%d" % k) for k in range(4)]
        sg = [sc.sb("sg%d" % k, [128, 512], F32) for k in range(2)]
        bsg = [Buf("sg0"), Buf("sg1")]
        ysb = [sc.sb("ysb%d" % k, [128, D], BF16) for k in range(2)]
        bys = [Buf("ys0"), Buf("ys1")]
        val = sc.sb("val", [128, 16], F32); idxs = sc.sb("idxs", [128, 16], I32); t16 = sc.sb("t16", [128, 16], F32)
        bval, bidx, bt16 = Buf("val"), Buf("idxs"), Buf("t16")

        def load_gu(e, q="sp"):
            tk.dma(q, [(BWB, "g%d%d" % (l, e))], [bWg], lambda E: E.dma_start(out=Wg[:], in_=wgb_d[l][e].ap().rearrange("(kc p) f -> p kc f", p=128)), sem="wg")
            tk.dma(q, [(BWB, "u%d%d" % (l, e))], [bWu], lambda E: E.dma_start(out=Wu[:], in_=wub_d[l][e].ap().rearrange("(kc p) f -> p kc f", p=128)), sem="wu")

        def load_d(e):
            tk.dma("sp", [(BWB, "d%d%d" % (l, e))], [bWd], lambda E: E.dma_start(out=Wd[:], in_=wdb_d[l][e].ap().rearrange("(fc p) f -> p fc f", p=128)), sem="wd")

        yk = [0]

        def ffn(e):
            def xe_loads(p):
                for stt in range(4):
                    s_ = p * 4 + stt
                    xw, bxw = xr[stt], bxr[stt]
                    tk.dma("act", [] if s_ == 0 else [(B["xe_d"], e)], [bxw] + ([(B["xe_d"], e)] if s_ == 0 else []),
                           lambda E: E.dma_start(out=xw[:], in_=xe_d[e][s_ * 128:(s_ + 1) * 128, :]))

            xe_loads(0)
            for p in range(4):
                for stt in range(4):
                    s_ = p * 4 + stt
                    xw, bxw = xr[stt], bxr[stt]
                    psT = PS[0][:].bitcast(BF16)
                    for kc in range(8):
                        tk.op("pe", [bxw, Bc], [PB[0]] if kc in (0, 7) else [],
                              lambda E: E.transpose(out=psT[:, kc * 128:(kc + 1) * 128], in_=xw[:, kc * 128:(kc + 1) * 128], identity=ident[:]),
                              mark=(kc == 7))
                    tk.op("act", [PB[0]], [(bXeT, stt)], lambda E: E.activation(
                        out=XeT[:, :, stt * 128:(stt + 1) * 128], in_=psT.rearrange("p (k t) -> p k t", k=8), func=AF.Copy))
                    tk.op("dve", [bxw, bsel], [bt16], lambda E: E.tensor_tensor(out=t16[:], in0=xw[:, D:D + 32].bitcast(F32), in1=selt[:, e, :], op=ALU.mult))
                    tk.op("dve", [bt16], [(bval, s_)], lambda E: E.tensor_reduce(out=val[:, s_:s_ + 1], in_=t16[:], axis=AX.X, op=ALU.add))
                    tk.op("dve", [bxw], [(bidx, s_)], lambda E: E.tensor_copy(out=idxs[:, s_:s_ + 1], in_=xw[:, D + 32:D + 34].bitcast(I32)))
                if p + 1 < 4:
                    xe_loads(p + 1)
                for fc in range(16):
                    pg, pbg = PS[1 + (fc % 2) * 2], PB[1 + (fc % 2) * 2]
                    pu, pbu = PS[2 + (fc % 2) * 2], PB[2 + (fc % 2) * 2]
                    for (W_, bW_, ps, pb) in ((Wg, bWg, pg, pbg), (Wu, bWu, pu, pbu)):
                        for kc in range(8):
                            tk.op("pe", [bXeT, (bW_, kc)], [pb] if kc in (0, 7) else [],
                                  lambda E: E.matmul(ps[:, :], lhsT=W_[:, kc, fc * 128:(fc + 1) * 128], rhs=XeT[:, kc, :], start=(kc == 0), stop=(kc == 7)),
                                  mark=(kc == 7))
                    sgt, bsgt = sg[fc % 2], bsg[fc % 2]
                    tk.op("act", [pbg], [bsgt], lambda E: E.activation(out=sgt[:], in_=pg[:, :], func=AF.Silu))
                    tk.op("dve", [pbu, bsgt], [(bhid, fc)], lambda E: E.tensor_tensor(out=hidT[:, fc, :], in0=pu[:, :], in1=sgt[:], op=ALU.mult))
                if p == 3 and e + 1 < 4:
                    load_gu(e + 1, "act")
                for stt in range(4):
                    s_ = p * 4 + stt
                    yt, byt = ysb[yk[0] % 2], bys[yk[0] % 2]
                    yk[0] += 1
                    for hf in range(2):
                        ps, pb = PS[5 + hf], PB[5 + hf]
                        for fc in range(16):
                            tk.op("pe", [bhid, (bWd, fc)], [pb] if fc in (0, 15) else [],
                                  lambda E: E.matmul(ps[:, :], lhsT=hidT[:, fc, stt * 128:(stt + 1) * 128], rhs=Wd[:, fc, hf * 512:(hf + 1) * 512], start=(fc == 0), stop=(fc == 15)),
                                  mark=(fc == 15))
                        tk.op("act", [pb, (bval, s_)], [(byt, hf)], lambda E: E.activation(out=yt[:, hf * 512:(hf + 1) * 512], in_=ps[:, :], func=AF.Copy, scale=val[:, s_:s_ + 1]))
                    first = (p == 0 and stt == 0)
                    tk.dma("pool", [byt, (bidx, s_)] + ([] if first else [B["z_d"]]), [B["z_d"]] if first else [],
                           lambda E: E.indirect_dma_start(
                               out=z_d[:, :], out_offset=bass.IndirectOffsetOnAxis(ap=idxs[:, s_:s_ + 1], axis=0),
                               in_=yt[:], in_offset=None, bounds_check=r_tok, oob_is_err=False, compute_op=ALU.add))

        load_gu(0)
        load_d(0)
        scatter_rows(0)
        for e in range(4):
            if e + 1 < 4:
                scatter_rows(e + 1)
            ffn(e)
            if e + 1 < 4:
                load_d(e + 1)
        for k in range(NCH):
            tk.coll("ReduceScatter", ALU.add, z_d[k * 2048:(k + 1) * 2048, :], zs_c[k].ap(), [B["z_d"]], [(B["zs_d"], k)])
        tk.barrier()
        sc.close()

    moe(0)

    sc = Scope(nc)
    tc1 = tail_setup(sc, 1)
    g2r = sc.sb("g2r", [128, D], F32); bg2 = Buf("g2r")
    load_row("sp", g2r[:], bg2, mod_row(0, 0, 5))
    gmA = sc.sb("gmA", [128, D], F32); shA = sc.sb("shA", [128, D], F32)
    bgmA, bshA = Buf("gmA"), Buf("shA")
    make_gm(gmA[:], bgmA, tc1.tmpr[:], tc1.btmp, 1, 0, 1, 0)
    load_row("sp", shA[:], bshA, mod_row(1, 0, 0))
    g1B = sc.sb("g1B", [128, D], F32); bg1B = Buf("g1B")
    load_row("sp", g1B[:], bg1B, mod_row(1, 0, 2))
    cwi = sc.sb("cwi", [128, 8, 3 * D], BF16); bcwi = Buf("cwi")
    for kc in range(8):
        tk.dma("pool", [], [(bcwi, kc)], lambda E: E.dma_start(out=cwi[:, kc, :], in_=cwin_in[kc * 128:(kc + 1) * 128, :]))
    cwo = sc.sb("cwo", [128, 8, D], BF16); bcwo = Buf("cwo")
    cks = sc.sb("cks", [128, 8, 3], F32); bck = Buf("cks")
    tk.dma("sp", [], [bck], lambda E: E.dma_start(out=cks[:], in_=ck_in.ap()))
    for kc in range(8):
        tk.dma("sp", [], [tc1.bxw], lambda E: E.dma_start(out=tc1.xw[:], in_=cwo_in[kc * 128:(kc + 1) * 128, :]))
        tk.op("dve", [tc1.bxw, bg1B], [(bcwo, kc)], lambda E: E.tensor_tensor(out=cwo[:, kc, :], in0=tc1.xw[:], in1=g1B[:], op=ALU.mult))
    x4 = [[sc.sb("x4_%d_%d" % (a, k), [128, D], F32) for k in range(4)] for a in range(3)]
    bx4 = [[Buf("x4_%d_%d" % (a, k)) for k in range(4)] for a in range(3)]
    zb = [sc.sb("zb%d" % k, [128, D], BF16) for k in range(2)]
    bzb = [Buf("zb0"), Buf("zb1")]
    hb5 = sc.sb("hb5", [128, D], BF16); bhb5 = Buf("hb5")
    hT4 = [sc.sb("hT4_%d" % a, [128, 8, 512], BF16) for a in range(2)]
    bhT4 = [Buf("hT4_0"), Buf("hT4_1")]
    cgs = sc.sb("cgs", [128, 512], F32); us = sc.sb("us", [128, 512], F32); ws = sc.sb("ws", [128, 512], F32)
    bcgs, bus, bws = Buf("cgs"), Buf("us"), Buf("ws")
    zTs = [sc.sb("zT%d" % a, [128, 8, 512], BF16) for a in range(2)]
    bzTs = [Buf("zT0"), Buf("zT1")]
    sq5 = sc.sb("sq5", [128, D], F32); st5 = sc.sb("st5", [128, 8], F32); bsq5, bst5 = Buf("sq5"), Buf("st5")
    xw5, bxw5 = tc1.tmpr, tc1.btmp

    def prep_tile(grp, w):
        i = grp * 4 + w
        a = grp % 2
        xt, bx, zt_, bz = x4[grp % 3][w], bx4[grp % 3][w], zb[i % 2], bzb[i % 2]
        tk.dma("sp", [(B["x1_d"], i)], [bx], lambda E: E.dma_start(out=xt[:], in_=x1_d[i * 128:(i + 1) * 128, :]))
        tk.dma("sp", [(B["zs_d"], i // 4)], [bz], lambda E: E.dma_start(out=zt_[:], in_=loc_rows(zs_c, i)))
        tk.op("dve", [bz, bg2], [bxw5], lambda E: E.tensor_tensor(out=xw5[:], in0=zt_[:], in1=g2r[:], op=ALU.mult))
        tk.op("dve", [bxw5, bx], [bx], lambda E: E.tensor_tensor(out=xt[:], in0=xt[:], in1=xw5[:], op=ALU.add))
        tk.op("act", [bx], [bsq5, bst5], lambda E: E.activation(out=sq5[:], in_=xt[:], func=AF.Square, accum_out=st5[:, 0:1]))
        rstd_of(st5[:, 0:1], st5[:, 2:3], st5[:, 1:2], bst5, D)
        tk.op("dve", [bx, bst5, bgmA], [bxw5], lambda E: E.scalar_tensor_tensor(
            out=xw5[:], in0=xt[:], scalar=st5[:, 2:3], in1=gmA[:], op0=ALU.mult, op1=ALU.mult))
        tk.op("dve", [bxw5, bshA], [bhb5], lambda E: E.tensor_tensor(out=hb5[:], in0=xw5[:], in1=shA[:], op=ALU.add))

    def prep_tile2(grp, w):
        a = grp % 2
        psT = PS[4][:].bitcast(BF16)
        for kc in range(8):
            tk.op("pe", [bhb5, Bc], [PB[4]] if kc in (0, 7) else [],
                  lambda E: E.transpose(out=psT[:, kc * 128:(kc + 1) * 128], in_=hb5[:, kc * 128:(kc + 1) * 128], identity=ident[:]),
                  mark=(kc == 7))
        tk.op("act", [], [(bhT4[a], w), PB[4]], lambda E: E.activation(
            out=hT4[a][:, :, w * 128:(w + 1) * 128], in_=psT.rearrange("p (k t) -> p k t", k=8), func=AF.Copy))

    def conv_cc(grp, cc):
        a = grp % 2
        for (sec, ps, pb) in ((0, PS[1], PB[1]), (1, PS[2], PB[2]), (2, PS[3], PB[3])):
            c0 = sec * D + cc * 128
            for kc in range(8):
                tk.op("pe", [bhT4[a], (bcwi, kc)], [pb] if kc in (0, 7) else [],
                      lambda E: E.matmul(ps[:, :], lhsT=cwi[:, kc, c0:c0 + 128], rhs=hT4[a][:, kc, :], start=(kc == 0), stop=(kc == 7)),
                      mark=(kc == 7))
        tk.op("act", [], [bcgs, PB[2]], lambda E: E.activation(out=cgs[:], in_=PS[2][:, :], func=AF.Copy))
        tk.op("dve", [bcgs], [bus, PB[3]], lambda E: E.tensor_tensor(out=us[:], in0=cgs[:], in1=PS[3][:, :], op=ALU.mult))
        tk.op("act", [bus, bck], [bws], lambda E: E.activation(out=ws[:], in_=us[:], func=AF.Copy, scale=cks[:, cc, 1:2]))
        u3 = us[:].rearrange("p (r t) -> p r t", t=64)
        w3 = ws[:].rearrange("p (r t) -> p r t", t=64)
        tk.op("dve", [bus, bws, bck], [bws], lambda E: E.scalar_tensor_tensor(
            out=w3[:, :, 1:64], in0=u3[:, :, 0:63], scalar=cks[:, cc, 0:1], in1=w3[:, :, 1:64], op0=ALU.mult, op1=ALU.add))
        tk.op("dve", [bus, bws, bck], [bws], lambda E: E.scalar_tensor_tensor(
            out=w3[:, :, 0:63], in0=u3[:, :, 1:64], scalar=cks[:, cc, 2:3], in1=w3[:, :, 0:63], op0=ALU.mult, op1=ALU.add))
        tk.op("dve", [bws], [(bzTs[a], cc), PB[1]], lambda E: E.tensor_tensor(out=zTs[a][:, cc, :], in0=ws[:], in1=PS[1][:, :], op=ALU.mult))

    def post_tile(grp, w):
        i = grp * 4 + w
        a = grp % 2
        xt, bx = x4[grp % 3][w], bx4[grp % 3][w]
        zT, bzT = zTs[a], bzTs[a]
        for hf in range(2):
            ps, pb = PS[5 + hf], PB[5 + hf]
            for cc in range(8):
                tk.op("pe", [bzT, (bcwo, cc)], [pb] if cc in (0, 7) else [],
                      lambda E: E.matmul(ps[:, :], lhsT=zT[:, cc, w * 128:(w + 1) * 128], rhs=cwo[:, cc, hf * 512:(hf + 1) * 512], start=(cc == 0), stop=(cc == 7)),
                      mark=(cc == 7))
            tk.op("dve", [], [bx, pb], lambda E: E.tensor_tensor(out=xt[:, hf * 512:(hf + 1) * 512], in0=xt[:, hf * 512:(hf + 1) * 512], in1=ps[:, :], op=ALU.add))
        tk.dma("sp", [bx], [(B["x1_d"], i)], lambda E: E.dma_start(out=x1_d[i * 128:(i + 1) * 128, :], in_=xt[:]))
        tail(tc1, xt[:], bx, i, split=True)

    def post_tile2(grp, w):
        tail2(tc1, grp * 4 + w)

    for w in range(4):
        prep_tile(0, w)
        prep_tile2(0, w)
    NG = NT // 4
    for grp in range(NG):
        for cc in range(8):
            conv_cc(grp, cc)
            w = cc // 2
            if cc % 2 == 0:
                if grp >= 1:
                    post_tile(grp - 1, w)
                if grp + 1 < NG and w >= 1:
                    prep_tile2(grp + 1, w - 1)
            else:
                if grp >= 1:
                    post_tile2(grp - 1, w)
                if grp + 1 < NG:
                    prep_tile(grp + 1, w)
        if grp + 1 < NG:
            prep_tile2(grp + 1, 3)
    for w in range(4):
        post_tile(NG - 1, w)
        post_tile2(NG - 1, w)
    tail_finish(tc1)
    tk.barrier()
    sc.close()

    moe(1)

    sc = Scope(nc)
    g2r = sc.sb("g2r7", [128, D], F32); bg2 = Buf("g2r7")
    load_row("sp", g2r[:], bg2, mod_row(1, 0, 5))
    fgr = sc.sb("fgr", [128, D], F32); bfg = Buf("fgr")
    tk.dma("sp", [], [bfg], lambda E: E.dma_start(out=fgr[:], in_=fng_in.ap().partition_broadcast(128)))
    xa = [sc.sb("xa7_%d" % k, [128, D], F32) for k in range(2)]
    bxa = [Buf("xa7_0"), Buf("xa7_1")]
    zb = [sc.sb("zb7_%d" % k, [128, D], BF16) for k in range(2)]
    bzb = [Buf("zb7_0"), Buf("zb7_1")]
    xw7 = sc.sb("xw7", [128, D], F32); bxw7 = Buf("xw7")
    sq7 = sc.sb("sq7", [128, D], F32); st7 = sc.sb("st7", [128, 8], F32); bsq7, bst7 = Buf("sq7"), Buf("st7")
    for i in range(NT):
        xt, bx, zt_, bz = xa[i % 2], bxa[i % 2], zb[i % 2], bzb[i % 2]
        tk.dma("sp", [(B["x1_d"], i)], [bx], lambda E: E.dma_start(out=xt[:], in_=x1_d[i * 128:(i + 1) * 128, :]))
        tk.dma("sp", [(B["zs_d"], i // 4)], [bz], lambda E: E.dma_start(out=zt_[:], in_=loc_rows(zs_c, i)))
        tk.op("dve", [bz, bg2], [bxw7], lambda E: E.tensor_tensor(out=xw7[:], in0=zt_[:], in1=g2r[:], op=ALU.mult))
        tk.op("dve", [bxw7, bx], [bx], lambda E: E.tensor_tensor(out=xt[:], in0=xt[:], in1=xw7[:], op=ALU.add))
        tk.op("act", [bx], [bsq7, bst7], lambda E: E.activation(out=sq7[:], in_=xt[:], func=AF.Square, accum_out=st7[:, 0:1]))
        rstd_of(st7[:, 0:1], st7[:, 2:3], st7[:, 1:2], bst7, D)
        tk.op("dve", [bx, bst7, bfg], [bx], lambda E: E.scalar_tensor_tensor(
            out=xt[:], in0=xt[:], scalar=st7[:, 2:3], in1=fgr[:], op0=ALU.mult, op1=ALU.mult))
        tk.dma("sp", [bx], [(B["out"], i)], lambda E: E.dma_start(out=out_d[i * 128:(i + 1) * 128, :], in_=xt[:]))
    tk.barrier()
    sc.close()
    return nc, dbg_out


def _consts():
    j = np.arange(128)[:, None]
    i = np.arange(128)[None, :]
    c = -1.0 / 16.0
    tri = np.stack([
        (j <= i) * c, (j >= i) * c,
        (j > i) * c, (j < i) * c,
        (j <= i) * 1.0, (j >= i) * 1.0,
    ]).astype(np.float32)
    lu = np.stack([(j < i) * 1.0, (j < i) * 1.0]).astype(np.float32)
    return tri, lu


def make_in_maps(inp):
    f = lambda a: np.ascontiguousarray(np.asarray(a, dtype=np.float32))
    x, c, ctx, c_ctx = f(inp["x"]), f(inp["c"]), f(inp["ctx"]), f(inp["c_ctx"])
    ada_w, ada_b, norm_g = f(inp["ada_w"]), f(inp["ada_b"]), f(inp["norm_g"])
    w_in, w_a2, b_a2 = f(inp["gla_w_in"])[0], f(inp["gla_w_a2"])[0], f(inp["gla_b_a2"])[0]
    gng, w_out = f(inp["gla_norm_g"])[0], f(inp["gla_w_out"])[0]
    cw_in, conv_k, cw_out = f(inp["conv_w_in"])[0], f(inp["conv_k"])[0], f(inp["conv_w_out"])[0]
    router_w = f(inp["router_w"])
    wg, wu, wd = inp["expert_w_gate"], inp["expert_w_up"], inp["expert_w_down"]
    fng = f(inp["final_norm_g"])
    tri, lu = _consts()
    ident = np.eye(128, dtype=np.float32)
    maps = []
    for core in range(8):
        b, j = core // 4, core % 4
        cc = np.stack([c[b].reshape(8, 128).T, c_ctx.reshape(8, 128).T], axis=-1)
        wfm = np.concatenate([w_in[:, j * 128:(j + 1) * 128], w_in[:, 512 + j * 128:512 + (j + 1) * 128],
                              w_in[:, 3072:3104]], axis=1)
        wtm = np.concatenate([w_in[:, 512 + j * 128:512 + (j + 1) * 128], w_in[:, 1024 + j * 256:1024 + (j + 1) * 256],
                              w_in[:, 2048 + j * 256:2048 + (j + 1) * 256]], axis=1)
        a2 = np.zeros((2, 32, 128), np.float32)
        a2[:, 0:16, :] = w_a2[:, :, j * 128:(j + 1) * 128]
        a2[:, 16, :] = b_a2[:, j * 128:(j + 1) * 128]
        gn = gng.reshape(2, 128).T
        sel = np.zeros((4, 16), np.float32)
        for e in range(4):
            sel[e, 4 * j + e] = 1.0
        ii = np.arange(NT)[None, :]
        tid = ((ii // 4) * 2048 + j * 512 + (ii % 4) * 128 + np.arange(128)[:, None]).astype(np.int32)
        m = {
            "x": x[b].reshape(NT, 4, 128, D)[:, j].reshape(TL, D), "ctx": ctx[b], "cc": cc, "ada_w": ada_w, "ada_b": ada_b,
            "norm_g": norm_g, "final_g": fng, "wfm": wfm, "wtm": wtm, "a2": a2, "gn": gn,
            "wo": w_out[j * 256:(j + 1) * 256], "cw_in": cw_in, "ck": conv_k.reshape(3, 8, 128).transpose(2, 1, 0),
            "cw_out": cw_out, "router_w": router_w,
            "wg": np.asarray(wg[:, 4 * j:4 * j + 4], dtype=np.float32), "wu": np.asarray(wu[:, 4 * j:4 * j + 4], dtype=np.float32),
            "wd": np.asarray(wd[:, 4 * j:4 * j + 4], dtype=np.float32),
            "sel": sel, "tid": tid, "ident": ident, "tri": tri, "lu": lu,
        }
        maps.append({k: np.ascontiguousarray(v) for k, v in m.items()})
    return maps


_USED = None


def kernel(**inputs):
    nc, _ = build()
    maps = make_in_maps(inputs)
    used = set(nc._in_names)
    maps = [{k: v for k, v in m.items() if k in used} for m in maps]
    res = run_bass_kernel_spmd(nc, maps, core_ids=list(range(8)))
    out = np.zeros((2, T, D), np.float32)
    for core in range(8):
        b, j = core // 4, core % 4
        out[b].reshape(NT, 4, 128, D)[:, j] = np.asarray(res.results[core]["out"], dtype=np.float32).reshape(NT, 128, D)
    return out


def _input_names(nc):
    return ["x", "ctx", "cc", "ada_w", "ada_b", "norm_g", "final_g", "wfm", "wtm", "a2", "gn", "wo", "cw_in", "ck",
            "cw_out", "router_w", "wg", "wu", "wd", "sel", "tid", "ident", "tri", "lu"]
```

```python
import os
import numpy as np
import ml_dtypes
import concourse.bass as bass
import concourse.mybir as mybir
from concourse.bass_utils import run_bass_kernel_spmd

F32 = mybir.dt.float32
BF16 = mybir.dt.bfloat16
I32 = mybir.dt.int32
AF = mybir.ActivationFunctionType
ALU = mybir.AluOpType
AX = mybir.AxisListType

D = 1024
T = 16384
TL = 4096
NT = 32
NTA = 128
CTX = 256
RW = 1088
CAP = 2048
GROUPS = [[0, 1, 2, 3], [4, 5, 6, 7]]
NDS = 24
BIG = 1.0e6
NITER = 24


class Scope:
    ctr = 0

    def __init__(self, nc):
        self.nc = nc
        self.items = []

    def sb(self, name, shape, dt):
        Scope.ctr += 1
        t = self.nc.sbuf_tensor("sb%d_%s" % (Scope.ctr, name), list(shape), dt)
        self.items.append(t)
        return t.__enter__()

    def close(self):
        for t in reversed(self.items):
            t.__exit__(None, None, None)
        self.items = []


class Buf:
    __slots__ = ("name", "w", "r")

    def __init__(self, name):
        self.name = name
        self.w = {}
        self.r = {}


def _norm(items):
    out = []
    for it in items:
        if isinstance(it, tuple):
            out.append(it)
        else:
            out.append((it, None))
    return out


class TK:
    def __init__(self, nc):
        self.nc = nc
        self.E = {"pe": nc.tensor, "act": nc.scalar, "dve": nc.vector, "pool": nc.gpsimd, "sp": nc.sync}
        self.sem = {k: nc.semaphore("s_" + k).__enter__() for k in self.E}
        self.cnt = {k: 0 for k in self.E}
        self.dpool = {"sp": 16, "pool": 12, "act": 4}
        self.dsem = {q: [nc.semaphore("d%s%d" % (q, i)).__enter__() for i in range(n)] for q, n in self.dpool.items()}
        self.dcnt = {q: [0] * n for q, n in self.dpool.items()}
        self.dnext = {q: 0 for q in self.dpool}
        self.ded = {}
        self.waited = {k: {} for k in self.E}
        self.ncc = 0

    def _semobj(self, key):
        if key[0] == "e":
            return self.sem[key[1]]
        if key[0] == "d":
            return self.dsem[key[1]][key[2]]
        if key[0] == "x":
            return self.ded[key[1]][0]
        return key[1]

    def _deps(self, reads, writes):
        deps = []
        for b, p in reads:
            for q, d in b.w.items():
                if p is None or q is None or p == q:
                    deps.append(d)
        for b, p in writes:
            for q, d in b.w.items():
                if p is None or q is None or p == q:
                    deps.append(d)
            for q, dd in b.r.items():
                if p is None or q is None or p == q:
                    deps.extend(dd.items())
        return deps

    def _wait(self, eng, deps):
        E = self.E[eng]
        best = {}
        for k, v in deps:
            if k == ("e", eng) and v > self.cnt[eng]:
                continue
            if best.get(k, 0) < v:
                best[k] = v
        for k, v in best.items():
            if self.waited[eng].get(k, 0) < v:
                E.wait_ge(self._semobj(k), v)
                self.waited[eng][k] = v

    def _record(self, reads, writes, dep):
        k, v = dep
        for b, p in reads:
            dd = b.r.setdefault(p, {})
            if dd.get(k, 0) < v:
                dd[k] = v
        for b, p in writes:
            if p is None:
                b.w = {None: dep}
                b.r = {}
            else:
                b.w[p] = dep
                b.r.pop(p, None)

    def op(self, eng, reads, writes, emit, mark=True):
        reads = _norm(reads)
        writes = _norm(writes)
        self._wait(eng, self._deps(reads, writes))
        inst = emit(self.E[eng])
        dep = (("e", eng), self.cnt[eng] + 1)
        if mark:
            self.cnt[eng] += 1
            inst.then_inc(self.sem[eng], 1)
        self._record(reads, writes, dep)
        return inst

    def dma(self, q, reads, writes, emit, sem=None):
        reads = _norm(reads)
        writes = _norm(writes)
        self._wait(q, self._deps(reads, writes))
        inst = emit(self.E[q])
        if sem is not None:
            if sem not in self.ded:
                self.ded[sem] = [self.nc.semaphore("x_" + sem).__enter__(), 0]
            self.ded[sem][1] += 16
            inst.then_inc(self.ded[sem][0], 16)
            dep = (("x", sem), self.ded[sem][1])
        else:
            i = self.dnext[q]
            self.dnext[q] = (i + 1) % self.dpool[q]
            self.dcnt[q][i] += 16
            inst.then_inc(self.dsem[q][i], 16)
            dep = (("d", q, i), self.dcnt[q][i])
        self._record(reads, writes, dep)
        return inst

    def coll(self, kind, op, in_ap, out_ap, reads, writes, qos=None):
        reads = _norm(reads)
        writes = _norm(writes)
        self._wait("pool", self._deps(reads, writes))
        if self.ncc == 0:
            self.ccsem = self.nc.semaphore("ccsem").__enter__()
        self.ncc += 1
        kw = {} if qos is None else {"dma_qos": qos}
        self.nc.gpsimd.collective_compute(kind, op, replica_groups=GROUPS, ins=[in_ap], outs=[out_ap], **kw).then_inc(self.ccsem)
        self._record(reads, writes, (("c", self.ccsem), self.ncc))

    def barrier(self):
        deps = [(("e", k), self.cnt[k]) for k in self.E if self.cnt[k] > 0]
        deps += [(("d", q, i), self.dcnt[q][i]) for q in self.dpool for i in range(self.dpool[q]) if self.dcnt[q][i] > 0]
        deps += [(("x", n), v[1]) for n, v in self.ded.items()]
        for eng in self.E:
            self._wait(eng, deps)


def build(debug=None):
    nc = bass.Bass("TRN2", target_bir_lowering=False)
    tk = TK(nc)
    dbg_out = {}

    in_names = []
    nc._in_names = in_names

    def din(name, shape, dt=F32):
        in_names.append(name)
        return nc.dram_tensor(name, list(shape), dt, kind="ExternalInput")

    def dscr(name, shape, dt):
        return nc.dram_tensor(name, list(shape), dt)

    x_in = din("x", [TL, D])
    ctx_in = din("ctx", [CTX, D])
    cc_in = din("cc", [128, 8, 2])
    adaw_in = din("ada_w", [2, D, 6 * D])
    adab_in = din("ada_b", [2, 6 * D])
    ng_in = din("norm_g", [2, 2, D])
    fng_in = din("final_g", [D])
    wfm_in = din("wfm", [D, 288])
    wtm_in = din("wtm", [D, 640])
    a2_in = din("a2", [2, 32, 128])
    gn_in = din("gn", [128, 2])
    wo_in = din("wo", [256, D])
    ident_in = din("ident", [128, 128])
    tri_in = din("tri", [6, 128, 128])
    wg_in = din("wg", [2, 4, D, 2 * D])
    wu_in = din("wu", [2, 4, D, 2 * D])
    wd_in = din("wd", [2, 4, 2 * D, D])
    out_d = nc.dram_tensor("out", [TL, D], F32, kind="ExternalOutput")

    mod_d = dscr("mod_d", [2, 2, 6 * D], F32)
    NCH = 8
    h1_loc = [dscr("h1_loc%d" % k, [TL // NCH, D], BF16) for k in range(NCH)]
    h1_all = [dscr("h1_all%d" % k, [4 * TL // NCH, D], BF16) for k in range(NCH)]
    hc_d = dscr("hc_d", [CTX, D], BF16)
    of_d = dscr("of_d", [T, 256], F32)
    yp_d = [dscr("yp_d%d" % k, [4 * TL // NCH, D], BF16) for k in range(NCH)]
    ymix_d = [dscr("ymix_d%d" % k, [TL // NCH, D], BF16) for k in range(NCH)]
    x1_d = dscr("x1_d", [TL, D], F32)
    aff_loc = dscr("aff_loc", [128, NT, 16], F32)
    aff_all = dscr("aff_all", [4 * 128, NT, 16], F32)
    xe_d = [dscr("xe_d%d" % e, [CAP, RW], BF16) for e in range(4)]
    z_d = dscr("z_d", [T, D], BF16)

    fence_in = dscr("fence_in", [128, 64], BF16)
    fence_out = [dscr("fence_out%d" % k, [512, 64], BF16) for k in range(8)]
    fence_n = [0]

    def fence(buf):
        k = fence_n[0]
        fence_n[0] += 1
        tk.coll("AllGather", ALU.bypass, fence_in.ap(), fence_out[k].ap(), [], [buf])

    def loc_rows(lst, i):
        k, w = i // 4, i % 4
        return lst[k][w * 128:(w + 1) * 128, :]

    def all_rows(lst, gi):
        r, i = gi % 4, gi // 4
        k, w = i // 4, i % 4
        return lst[k][r * 512 + w * 128:r * 512 + (w + 1) * 128, :]

    wgb_d = [[dscr("wgb_%d_%d" % (l, e), [D, 2 * D], BF16) for e in range(4)] for l in range(2)]
    wub_d = [[dscr("wub_%d_%d" % (l, e), [D, 2 * D], BF16) for e in range(4)] for l in range(2)]
    wdb_d = [[dscr("wdb_%d_%d" % (l, e), [2 * D, D], BF16) for e in range(4)] for l in range(2)]
    BWB = Buf("wb16")

    B = {n: Buf(n) for n in ["mod_d", "h1_loc", "h1_all", "hc_d", "of_d", "yp_d", "ymix_d", "x1_d", "rows_loc",
                             "rows_all", "aff_loc", "aff_all", "xe_d", "z_d", "zs_d", "out"]}

    def dbg_dump(name, src, shape, dt):
        o = nc.dram_tensor("dbg_" + name, list(shape), dt, kind="ExternalOutput")
        dbg_out[name] = o
        ob = Buf("dbg_" + name)
        tk.dma("sp", [B[name]], [ob], lambda E: E.dma_start(out=o.ap(), in_=src.ap()))
        return ob

    def finish(extra):
        tk.barrier()
        return nc

    def sb(name, shape, dt):
        return nc.sbuf_tensor("sb_" + name, list(shape), dt).__enter__()

    ident_f = sb("ident_f", [128, 128], F32)
    ident = sb("ident", [128, 128], BF16)
    ones_f = sb("ones_f", [128, 128], F32)
    ones_b = sb("ones_b", [128, 128], BF16)
    Bc = Buf("consts")
    tk.dma("sp", [], [Bc], lambda E: E.dma_start(out=ident_f[:], in_=ident_in.ap()))
    tk.op("dve", [Bc], [Bc], lambda E: E.tensor_copy(out=ident[:], in_=ident_f[:]))
    tk.op("pool", [], [(Bc, "of")], lambda E: E.memset(ones_f[:], 1.0))
    tk.op("pool", [], [(Bc, "ob")], lambda E: E.memset(ones_b[:], 1.0))

    PS = [nc.psum_tensor("ps%d" % i, [128, 512], F32).__enter__() for i in range(8)]
    PB = [Buf("ps%d" % i) for i in range(8)]

    sc = Scope(nc)
    ccs = sc.sb("ccs", [128, 8, 2], F32)
    csil = sc.sb("csil", [128, 8, 2], F32)
    adab = sc.sb("adab", [1, 2, 6 * D], F32)
    modsb = sc.sb("modsb", [2, 2, 6 * D], F32)
    aw0 = sc.sb("aw0", [128, 8, 512], F32)
    aw1 = sc.sb("aw1", [128, 8, 512], F32)
    if True:
        bcs, baw, bmod, bab = Buf("ccs"), [Buf("aw0"), Buf("aw1")], Buf("modsb"), Buf("adab")
        aws = [aw0, aw1]
        tk.dma("sp", [], [bcs], lambda E: E.dma_start(out=ccs[:], in_=cc_in.ap()))
        tk.dma("sp", [], [bab], lambda E: E.dma_start(out=adab[:], in_=adab_in.ap().unsqueeze(0)))
        tk.op("act", [bcs], [(bcs, "s")], lambda E: E.activation(out=csil[:], in_=ccs[:], func=AF.Silu))
        kk0 = [0]

        def mod_layer(l):
            for n in range(12):
                k = kk0[0]
                kk0[0] += 1
                aw, bw = aws[k % 2], baw[k % 2]
                ps, pb = PS[k % 2], PB[k % 2]
                tk.dma("sp", [], [bw], lambda E: E.dma_start(
                    out=aw[:], in_=adaw_in[l, :, n * 512:(n + 1) * 512].rearrange("(kc p) f -> p kc f", p=128)))
                for kc in range(8):
                    tk.op("pe", [bw, (bcs, "s")], [pb] if kc == 0 else [],
                          lambda E: E.matmul(ps[0:2, :], lhsT=csil[:, kc, :], rhs=aw[:, kc, :], start=(kc == 0), stop=False),
                          mark=False)
                tk.op("pe", [bab, (Bc, "of")], [pb],
                      lambda E: E.matmul(ps[0:2, :], lhsT=ones_f[0:1, 0:2], rhs=adab[0:1, l, n * 512:(n + 1) * 512],
                                         start=False, stop=True))
                tk.op("act", [], [(bmod, k), pb], lambda E: E.activation(out=modsb[0:2, l, n * 512:(n + 1) * 512],
                                                                         in_=ps[0:2, :], func=AF.Copy))
            tk.dma("sp", [bmod], [(B["mod_d"], l)], lambda E: E.dma_start(out=mod_d[l, :, :], in_=modsb[0:2, l, :]))

        mod_layer(0)
    sc0 = sc
    if debug == "p0":
        dbg_dump("mod_d", mod_d, [2, 2, 6 * D], F32)
        return finish(None), dbg_out

    def load_row(q, dst, dbuf, src_ap):
        tk.dma(q, [B["mod_d"]], [dbuf], lambda E: E.dma_start(out=dst, in_=src_ap.partition_broadcast(128)))

    def mod_row(l, v, ci):
        return mod_d[l, v, ci * D:(ci + 1) * D]

    def make_gm(dst, dbuf, tmp, tbuf, l, v, ci_scale, s):
        load_row("sp", dst, dbuf, mod_row(l, v, ci_scale))
        tk.dma("sp", [], [tbuf], lambda E: E.dma_start(out=tmp, in_=ng_in[l, s, :].partition_broadcast(128)))
        tk.op("dve", [dbuf, tbuf], [dbuf], lambda E: E.scalar_tensor_tensor(
            out=dst, in0=dst, scalar=1.0, in1=tmp, op0=ALU.add, op1=ALU.mult))

    def rstd_of(ss_ap, out_ap, tmp_ap, rb, n):
        tk.op("act", [rb], [rb], lambda E: E.activation(out=tmp_ap, in_=ss_ap, func=AF.Ln, scale=1.0 / n, bias=1e-6))
        tk.op("act", [rb], [rb], lambda E: E.activation(out=out_ap, in_=tmp_ap, func=AF.Exp, scale=-0.5))

    sc = Scope(nc)
    gm = sc.sb("gm", [128, D], F32)
    sh = sc.sb("sh", [128, D], F32)
    gmc = sc.sb("gmc", [128, D], F32)
    shc = sc.sb("shc", [128, D], F32)
    tmpr = sc.sb("tmpr", [128, D], F32)
    xt0 = sc.sb("xt0", [128, D], F32)
    xt1 = sc.sb("xt1", [128, D], F32)
    sq = sc.sb("sq", [128, D], F32)
    st = sc.sb("st", [128, 8], F32)
    hb0 = sc.sb("hb0", [128, D], BF16)
    hb1 = sc.sb("hb1", [128, D], BF16)
    if True:
        bgm, bsh, bgmc, bshc, btmp = Buf("gm"), Buf("sh"), Buf("gmc"), Buf("shc"), Buf("tmpr")
        make_gm(gm[:], bgm, tmpr[:], btmp, 0, 0, 1, 0)
        load_row("sp", sh[:], bsh, mod_row(0, 0, 0))
        make_gm(gmc[:], bgmc, tmpr[:], btmp, 0, 1, 1, 0)
        load_row("sp", shc[:], bshc, mod_row(0, 1, 0))
        xts, bxt = [xt0, xt1], [Buf("xt0"), Buf("xt1")]
        hbs, bhb = [hb0, hb1], [Buf("hb0"), Buf("hb1")]
        htb = [sc.sb("htb%d" % k_, [128, D], BF16) for k_ in range(2)]
        bhtb = [Buf("htb0"), Buf("htb1")]
        bsq, bst = Buf("sq"), Buf("st")
        for k in range(NT + 2):
            xt, bx, hb, bh = xts[k % 2], bxt[k % 2], hbs[k % 2], bhb[k % 2]
            isc = k >= NT
            src = ctx_in[(k - NT) * 128:(k - NT + 1) * 128, :] if isc else x_in[k * 128:(k + 1) * 128, :]
            g_, s_, bg_, bs_ = (gmc, shc, bgmc, bshc) if isc else (gm, sh, bgm, bsh)
            tk.dma("sp", [], [bx], lambda E: E.dma_start(out=xt[:], in_=src))
            tk.op("act", [bx], [bsq, bst], lambda E: E.activation(out=sq[:], in_=xt[:], func=AF.Square, accum_out=st[:, 0:1]))
            rstd_of(st[:, 0:1], st[:, 2:3], st[:, 1:2], bst, D)
            tk.op("dve", [bx, bst, bg_], [bx], lambda E: E.scalar_tensor_tensor(
                out=xt[:], in0=xt[:], scalar=st[:, 2:3], in1=g_[:], op0=ALU.mult, op1=ALU.mult))
            tk.op("dve", [bx, bs_], [bh], lambda E: E.tensor_tensor(out=hb[:], in0=xt[:], in1=s_[:], op=ALU.add))
            psT = PS[k % 2][:].bitcast(BF16)
            for kc in range(8):
                tk.op("pe", [bh, Bc], [PB[k % 2]] if kc in (0, 7) else [],
                      lambda E: E.transpose(out=psT[:, kc * 128:(kc + 1) * 128], in_=hb[:, kc * 128:(kc + 1) * 128], identity=ident[:]),
                      mark=(kc == 7))
            hb, bh = htb[k % 2], bhtb[k % 2]
            tk.op("act", [], [bh, PB[k % 2]], lambda E: E.activation(out=hb[:], in_=psT, func=AF.Copy))
            if isc:
                tk.dma("sp", [bh], [B["hc_d"]], lambda E: E.dma_start(out=hc_d[(k - NT) * 128:(k - NT + 1) * 128, :], in_=hb[:]))
            else:
                tk.dma("sp", [bh], [(B["h1_loc"], k)], lambda E: E.dma_start(out=loc_rows(h1_loc, k), in_=hb[:]))
                if k % 4 == 3:
                    ck = k // 4
                    tk.coll("AllGather", ALU.bypass, h1_loc[ck].ap(), h1_all[ck].ap(), [(B["h1_loc"], t_) for t_ in range(4 * ck, 4 * ck + 4)], [(B["h1_all"], ck)])
    if debug == "p1a":
        dbg_dump("h1_loc", h1_loc, [TL, D], BF16)
        dbg_dump("hc_d", hc_d, [CTX, D], BF16)
        return finish(None), dbg_out
    mod_layer(1)
    tk.barrier()
    sc.close()
    sc0.close()
    if debug == "p1":
        dbg_dump("h1_all", h1_all[3], [2048, D], BF16)
        dbg_dump("hc_d", hc_d, [CTX, D], BF16)
        dbg_dump("mod_d", mod_d, [2, 2, 6 * D], F32)
        return finish(None), dbg_out

    sc = Scope(nc)
    wfm = sc.sb("wfm", [128, 8, 288], BF16)
    wtm = sc.sb("wtm", [128, 8, 640], BF16)
    wstg = sc.sb("wstg", [128, 8, 288], F32)
    a2s = sc.sb("a2s", [32, 2, 128], F32)
    tri = sc.sb("tri", [128, 6, 128], F32)
    wo_f = sc.sb("wo_f", [128, 2, D], F32)
    wo_b = sc.sb("wo_b", [128, 2, D], BF16)
    gns = sc.sb("gns", [128, 2], F32)
    g1r = sc.sb("g1r", [128, D], F32)
    hbA = sc.sb("hbA", [128, D], BF16)
    hbB = sc.sb("hbB", [128, D], BF16)
    hT = sc.sb("hT", [128, 8, 128], BF16)
    aT = sc.sb("aT", [32, 128], F32)
    e1 = sc.sb("e1", [128, 128], F32)
    l1 = sc.sb("l1", [128, 128], F32)
    EqT = sc.sb("EqT", [128, 128], F32)
    EkT = sc.sb("EkT", [128, 128], F32)
    Ekd = sc.sb("Ekd", [128, 128], F32)
    qt = sc.sb("qt", [128, 128], BF16)
    kt = sc.sb("kt", [128, 128], BF16)
    kd = sc.sb("kd", [128, 128], BF16)
    vs = sc.sb("vs", [128, 256], BF16)
    sT = sc.sb("sT", [128, 128], BF16)
    S = sc.sb("S", [128, 256], F32)
    Sb = sc.sb("Sb", [128, 256], BF16)
    osb = sc.sb("osb", [128, 256], F32)
    ofl = sc.sb("ofl", [128, 256], F32)
    osq = sc.sb("osq", [128, 256], F32)
    sr = sc.sb("sr", [128, 256], F32)
    ofin = sc.sb("ofin", [128, 256], BF16)
    ofT = sc.sb("ofT", [128, 2, 128], BF16)
    ypo = sc.sb("ypo", [128, D], BF16)
    st2 = sc.sb("st2", [128, 8], F32)
    if True:
        bw = Buf("gla_w")
        tk.dma("sp", [], [(bw, "stg")], lambda E: E.dma_start(out=wstg[:], in_=wfm_in.ap().rearrange("(kc p) f -> p kc f", p=128)))
        tk.op("act", [(bw, "stg")], [(bw, "fm")], lambda E: E.activation(out=wfm[:, :, 0:128], in_=wstg[:, :, 0:128], func=AF.Copy, scale=128.0 ** -0.5))
        tk.op("act", [(bw, "stg")], [(bw, "fm2")], lambda E: E.activation(out=wfm[:, :, 128:288], in_=wstg[:, :, 128:288], func=AF.Copy))
        tk.dma("pool", [], [(bw, "tm")], lambda E: E.dma_start(out=wtm[:], in_=wtm_in.ap().rearrange("(kc p) f -> p kc f", p=128)))
        tk.dma("sp", [], [(bw, "a2")], lambda E: E.dma_start(out=a2s[:], in_=a2_in.ap().rearrange("d r f -> r d f")))
        tk.dma("sp", [], [(bw, "tri")], lambda E: E.dma_start(out=tri[:], in_=tri_in.ap().rearrange("s p f -> p s f")))
        tk.dma("sp", [], [(bw, "gn")], lambda E: E.dma_start(out=gns[:], in_=gn_in.ap()))
        tk.dma("sp", [], [(bw, "wof")], lambda E: E.dma_start(out=wo_f[:], in_=wo_in.ap().rearrange("(vc p) f -> p vc f", p=128)))
        bg1 = Buf("g1r")
        load_row("sp", g1r[:], bg1, mod_row(0, 0, 2))
        for vc in range(2):
            tk.op("dve", [(bw, "wof"), (bw, "gn"), bg1], [(bw, "wob%d" % vc)], lambda E: E.scalar_tensor_tensor(
                out=wo_b[:, vc, :], in0=wo_f[:, vc, :], scalar=gns[:, vc:vc + 1], in1=g1r[:], op0=ALU.mult, op1=ALU.mult))
        def mk(name, shape, dt, n):
            return [sc.sb("%s_%d" % (name, k), shape, dt) for k in range(n)], [Buf("%s_%d" % (name, k)) for k in range(n)]
        hbs, bhb = mk("hbp", [128, D], BF16, 2)
        hTs, bhT = mk("hTp", [128, 8, 128], BF16, 6)
        qss, bqs = mk("qs", [128, 128], F32, 2)
        kss, bks = mk("ks", [128, 128], F32, 2)
        kms, bkm = mk("km", [128, 128], F32, 2)
        vss, bvs = mk("vsp", [128, 256], BF16, 3)
        srs, bsr = mk("srp", [128, 256], F32, 3)
        aTs, baT = mk("aTp", [32, 128], F32, 2)
        l1s, bl1 = mk("l1p", [128, 128], F32, 2)
        Eqs, bEq = mk("Eq", [128, 128], F32, 2)
        Eks, bEk = mk("Ek", [128, 128], F32, 2)
        Eds, bEd = mk("Ed", [128, 128], F32, 2)
        qts, bqt = mk("qtp", [128, 128], BF16, 2)
        kts, bkt = mk("ktp", [128, 128], BF16, 2)
        kds, bkd = mk("kdp", [128, 128], BF16, 2)
        sTs, bsT = mk("sTp", [128, 128], BF16, 2)
        osbs, bosb = mk("osbp", [128, 256], F32, 2)
        ofls, bofl = mk("oflp", [128, 256], F32, 6)
        ofins, bofin = mk("ofinp", [128, 256], BF16, 2)
        ofTs, bofT = mk("ofTp", [128, 2, 128], BF16, 2)
        ypos, bypo = mk("ypop", [128, D], BF16, 2)
        st2s, bst2 = mk("st2p", [128, 8], F32, 2)
        be1, bS, bSb, bosq = Buf("e1"), Buf("S"), Buf("Sb"), Buf("osq")
        recs, brec = mk("rec", [128, 896], F32, 6)
        ers = sc.sb("ers", [128, 256], F32)
        bers = Buf("ers")
        aTb, baTb = mk("aTb", [32, 128], F32, 2)
        e1b = sc.sb("e1b", [128, 128], F32)
        be1b = Buf("e1b")
        rec_d = dscr("rec_d", [NTA + 2, 128, 896], F32)
        Brec = Buf("rec_d")
        for k in range(2):
            tk.op("pool", [], [baTb[k]], lambda E: E.memset(aTb[k][:], 1.0))
        for k in range(2):
            tk.op("pool", [], [baT[k]], lambda E: E.memset(aTs[k][:], 1.0))
        WR = [(bw, "fm"), (bw, "fm2")]
        P = lambda b, nm: (PB[b], nm)
        precast = []
        if debug is None:
            for l in range(2):
                for e in range(4):
                    precast.append((wgb_d[l][e], wg_in[l, e, :, :]))
                    precast.append((wub_d[l][e], wu_in[l, e, :, :]))
                    precast.append((wdb_d[l][e], wd_in[l, e, :, :]))
        for dr in range(int(os.environ.get('KDR', '2'))):
            if dr == 0:
                seq = [("c", 0), ("c", 1)] + [("x", g) for g in range(NTA)]
            else:
                seq = [("c", 1), ("c", 0)] + [("x", g) for g in range(NTA - 1, -1, -1)]
            lastcol = 127 if dr == 0 else 0
            N = min(len(seq), int(os.environ.get('KNT', '1000')))

            def stage_l(n):
                kind, gi = seq[n]
                isc = kind == "c"
                p5 = n % 6
                ridx = (NTA + gi) if isc else gi
                if dr == 1:
                    tk.dma("sp", [(Brec, ridx)], [brec[p5]], lambda E: E.dma_start(out=recs[p5][:], in_=rec_d[ridx, :, :]))
                    if not isc:
                        tk.dma("sp", [(B["of_d"], gi)], [bofl[p5]], lambda E: E.dma_start(out=ofls[p5][:], in_=of_d[gi * 128:(gi + 1) * 128, :]))
                    return
                src = hc_d[gi * 128:(gi + 1) * 128, :] if isc else all_rows(h1_all, gi)
                sbuf_ = B["hc_d"] if isc else (B["h1_all"], gi // 16)
                tk.dma("sp", [sbuf_], [bhT[p5]], lambda E: E.dma_start(out=hTs[p5][:].rearrange("p k t -> p (k t)"), in_=src))

            def stage_a(n):
                kind, gi = seq[n]
                isc = kind == "c"
                p2, p3 = n % 2, n % 6
                rec, br = recs[p3], brec[p3]
                ridx = (NTA + gi) if isc else gi
                if dr == 1:
                    return
                hT_, bhT_ = hTs[p3], bhT[p3]
                for (c0, c1, m, o0) in ((0, 128, 128, 0), (128, 256, 128, 128), (256, 272, 16, 256), (272, 288, 16, 384)):
                    for kc in range(8):
                        tk.op("pe", [bhT_] + WR, [PB[1]] if kc in (0, 7) else [],
                              lambda E: E.matmul(PS[1][0:m, o0:o0 + 128], lhsT=wfm[:, kc, c0:c1], rhs=hT_[:, kc, :], start=(kc == 0), stop=(kc == 7)),
                              mark=(kc == 7))
                for kc in range(8):
                    tk.op("pe", [bhT_, (bw, "tm")], [PB[2]] if kc in (0, 7) else [],
                          lambda E: E.matmul(PS[2][:, 0:384], lhsT=hT_[:, kc, :], rhs=wtm[:, kc, 0:384], start=(kc == 0), stop=(kc == 7)),
                          mark=(kc == 7))
                if not isc:
                    for kc in range(8):
                        tk.op("pe", [bhT_, (bw, "tm")], [PB[3]] if kc in (0, 7) else [],
                              lambda E: E.matmul(PS[3][:, 0:256], lhsT=hT_[:, kc, :], rhs=wtm[:, kc, 384:640], start=(kc == 0), stop=(kc == 7)),
                              mark=(kc == 7))
                tk.op("act", [], [baT[p2], PB[1]], lambda E: E.activation(out=aTs[p2][0:16, :], in_=PS[1][0:16, 256:384], func=AF.Copy))
                tk.op("act", [], [baTb[p2], PB[1]], lambda E: E.activation(out=aTb[p2][0:16, :], in_=PS[1][0:16, 384:512], func=AF.Copy))
                tk.op("act", [], [(br, "q"), PB[1]], lambda E: E.activation(out=rec[:, 0:128], in_=PS[1][:, 0:128], func=AF.Copy))
                tk.op("dve", [], [(br, "k"), PB[1]], lambda E: E.tensor_copy(out=rec[:, 128:256], in_=PS[1][:, 128:256]))
                tk.op("dve", [], [(br, "km"), PB[2]], lambda E: E.tensor_copy(out=rec[:, 256:384], in_=PS[2][:, 0:128]))
                tk.op("act", [], [(br, "v"), PB[2]], lambda E: E.activation(out=rec[:, 512:640].bitcast(BF16), in_=PS[2][:, 128:384], func=AF.Copy))
                if not isc:
                    tk.op("act", [], [bers, PB[3]], lambda E: E.activation(out=ers[:], in_=PS[3][:, 0:256], func=AF.Exp, scale=-1.0))
                    tk.op("dve", [bers], [bers], lambda E: E.tensor_scalar(out=ers[:], in0=ers[:], scalar1=1.0, scalar2=1.0, op0=ALU.mult, op1=ALU.add))
                    tk.op("dve", [bers], [bers], lambda E: E.reciprocal(out=ers[:], in_=ers[:]))
                    tk.op("dve", [bers], [(br, "sr"), PB[3]], lambda E: E.tensor_tensor(out=rec[:, 640:896], in0=ers[:], in1=PS[3][:, 0:256], op=ALU.mult))
                tk.op("pe", [baT[p2], (bw, "a2")], [PB[0]],
                      lambda E: E.matmul(PS[0][:, 0:128], lhsT=aTs[p2][0:32, :], rhs=a2s[0:32, 0, :], start=True, stop=True), mark=False)
                tk.op("pe", [baTb[p2], (bw, "a2")], [PB[0]],
                      lambda E: E.matmul(PS[0][:, 128:256], lhsT=aTb[p2][0:32, :], rhs=a2s[0:32, 1, :], start=True, stop=True))
                tk.op("act", [], [be1, PB[0]], lambda E: E.activation(out=e1[:], in_=PS[0][:, 0:128], func=AF.Exp, scale=-1.0))
                tk.op("act", [], [be1b, PB[0]], lambda E: E.activation(out=e1b[:], in_=PS[0][:, 128:256], func=AF.Exp, scale=-1.0))
                tk.op("act", [be1], [bl1[p2]], lambda E: E.activation(out=l1s[p2][:], in_=e1[:], func=AF.Ln, bias=1.0))
                tk.op("act", [be1b], [(br, "l1b")], lambda E: E.activation(out=rec[:, 384:512], in_=e1b[:], func=AF.Ln, bias=1.0))
                tk.dma("sp", [br], [(Brec, ridx)], lambda E: E.dma_start(out=rec_d[ridx, :, :], in_=rec[:]))

            def stage_b(n):
                p2, p3 = n % 2, n % 6
                rec, br = recs[p3], brec[p3]
                l1ap = l1s[p2][:] if dr == 0 else rec[:, 384:512]
                l1b_ = bl1[p2] if dr == 0 else br
                tk.op("pe", [l1b_, (bw, "tri")], [PB[4]],
                      lambda E: E.matmul(PS[4][:, 0:128], lhsT=l1ap, rhs=tri[:, dr, :], start=True, stop=True), mark=False)
                tk.op("pe", [l1b_, (bw, "tri")], [PB[4]],
                      lambda E: E.matmul(PS[4][:, 128:256], lhsT=tri[:, 2 + dr, :], rhs=l1ap, start=True, stop=True))
                tk.op("act", [], [bEq[p2], PB[4]], lambda E: E.activation(out=Eqs[p2][:], in_=PS[4][:, 0:128], func=AF.Exp))
                tk.op("act", [], [bEk[p2], PB[4]], lambda E: E.activation(out=Eks[p2][:], in_=PS[4][:, 0:128], func=AF.Exp, scale=-1.0))
                tk.op("act", [], [bEd[p2], PB[4]], lambda E: E.activation(out=Eds[p2][:], in_=PS[4][:, 128:256], func=AF.Exp))
                tk.op("dve", [br, bEq[p2]], [bqt[p2]], lambda E: E.tensor_tensor(out=qts[p2][:], in0=rec[:, 0:128], in1=Eqs[p2][:], op=ALU.mult))
                tk.op("dve", [br, bEk[p2]], [bkt[p2]], lambda E: E.tensor_tensor(out=kts[p2][:], in0=rec[:, 128:256], in1=Eks[p2][:], op=ALU.mult))
                tk.op("dve", [br, bEd[p2]], [bkd[p2]], lambda E: E.tensor_tensor(out=kds[p2][:], in0=rec[:, 256:384], in1=Eds[p2][:], op=ALU.mult))

            def stage_c(n):
                kind, gi = seq[n]
                isc = kind == "c"
                p2, p3 = n % 2, n % 6
                qt, kt, kd = qts[p2], kts[p2], kds[p2]
                rec, br = recs[p3], brec[p3]
                vs_ap = rec[:, 512:640].bitcast(BF16)
                if n == 0:
                    tk.op("pool", [], [bS], lambda E: E.memset(S[:], 0.0))
                    tk.op("pool", [], [bSb], lambda E: E.memset(Sb[:], 0.0))
                if not isc:
                    tk.op("pe", [bkt[p2], bqt[p2]], [PB[5]],
                          lambda E: E.matmul(PS[5][:, 0:128], lhsT=kt[:], rhs=qt[:], start=True, stop=True))
                    tk.op("dve", [(bw, "tri")], [bsT[p2], PB[5]], lambda E: E.tensor_tensor(out=sTs[p2][:], in0=PS[5][:, 0:128], in1=tri[:, 4 + dr, :], op=ALU.mult))
                    tk.op("pe", [bsT[p2], br], [PB[5]],
                          lambda E: E.matmul(PS[5][:, 128:384], lhsT=sTs[p2][:], rhs=vs_ap, start=True, stop=False), mark=False)
                    tk.op("pe", [bqt[p2], bSb], [PB[5]],
                          lambda E: E.matmul(PS[5][:, 128:384], lhsT=qt[:], rhs=Sb[:], start=False, stop=True))
                tk.op("pe", [bkd[p2], br], [PB[6]],
                      lambda E: E.matmul(PS[6][:, 0:256], lhsT=kd[:], rhs=vs_ap, start=True, stop=True))
                tk.op("dve", [bEq[p2]], [bS, PB[6]], lambda E: E.scalar_tensor_tensor(
                    out=S[:], in0=S[:], scalar=Eqs[p2][:, lastcol:lastcol + 1], in1=PS[6][:, 0:256], op0=ALU.mult, op1=ALU.add))
                tk.op("act", [bS], [bSb], lambda E: E.activation(out=Sb[:], in_=S[:], func=AF.Copy))
                if isc:
                    return
                osb_, bosb_ = osbs[p2], bosb[p2]
                if dr == 0:
                    tk.op("act", [], [bosb_, PB[5]], lambda E: E.activation(out=osb_[:], in_=PS[5][:, 128:384], func=AF.Copy))
                    tk.dma("sp", [bosb_], [(B["of_d"], gi)], lambda E: E.dma_start(out=of_d[gi * 128:(gi + 1) * 128, :], in_=osb_[:]))
                    return
                ofl_, bofl_ = ofls[p3], bofl[p3]
                tk.op("dve", [bofl_], [bosb_, PB[5]], lambda E: E.tensor_tensor(out=osb_[:], in0=ofl_[:], in1=PS[5][:, 128:384], op=ALU.add))
                return

            def stage_d(n):
                kind, gi = seq[n]
                if kind == "c":
                    return
                p2, p3 = n % 2, n % 6
                rec, br = recs[p3], brec[p3]
                osb_, bosb_ = osbs[p2], bosb[p2]
                st2_, bst2_ = st2s[p2], bst2[p2]
                ofin_, bofin_, ofT_, bofT_, ypo_, bypo_ = ofins[p2], bofin[p2], ofTs[p2], bofT[p2], ypos[p2], bypo[p2]
                tk.op("act", [bosb_], [bosq, bst2_], lambda E: E.activation(out=osq[:], in_=osb_[:], func=AF.Square, accum_out=st2_[:, 0:1]))
                rstd_of(st2_[:, 0:1], st2_[:, 2:3], st2_[:, 1:2], bst2_, 256)
                tk.op("dve", [bosb_, bst2_, br], [bofin_], lambda E: E.scalar_tensor_tensor(
                    out=ofin_[:], in0=osb_[:], scalar=st2_[:, 2:3], in1=rec[:, 640:896], op0=ALU.mult, op1=ALU.mult))
                psT2 = PS[0][:].bitcast(BF16)
                for vc in range(2):
                    tk.op("pe", [bofin_, Bc], [PB[0]],
                          lambda E: E.transpose(out=psT2[:, 768 + vc * 128:768 + (vc + 1) * 128], in_=ofin_[:, vc * 128:(vc + 1) * 128], identity=ident[:]),
                          mark=(vc == 1))
                tk.op("act", [], [bofT_, PB[0]], lambda E: E.activation(out=ofT_[:].rearrange("p k t -> p (k t)"), in_=psT2[:, 768:1024], func=AF.Copy))
                for hf in range(2):
                    for vc in range(2):
                        tk.op("pe", [bofT_, (bw, "wob%d" % vc)], [PB[7]],
                              lambda E: E.matmul(PS[7][:, :], lhsT=ofT_[:, vc, :], rhs=wo_b[:, vc, hf * 512:(hf + 1) * 512], start=(vc == 0), stop=(vc == 1)),
                              mark=(vc == 1))
                    if hf == 0:
                        tk.op("act", [], [(bypo_, hf), PB[7]], lambda E: E.activation(out=ypo_[:, 0:512], in_=PS[7][:, :], func=AF.Copy))
                    else:
                        tk.op("dve", [], [(bypo_, hf), PB[7]], lambda E: E.tensor_copy(out=ypo_[:, 512:1024], in_=PS[7][:, :]))
                tk.dma("pool", [bypo_], [(B["yp_d"], gi)], lambda E: E.dma_start(out=all_rows(yp_d, gi), in_=ypo_[:]))
                if gi % 16 == 0:
                    ck = gi // 16
                    tk.coll("ReduceScatter", ALU.add, yp_d[ck].ap(), ymix_d[ck].ap(), [(B["yp_d"], g_) for g_ in range(16 * ck, 16 * ck + 16)], [(B["ymix_d"], ck)], qos="P2")

            for n in range(-4, N):
                if n + 4 < N:
                    stage_l(n + 4)
                if 0 <= n + 2 < N:
                    stage_a(n + 2)
                if 0 <= n + 1 < N:
                    stage_b(n + 1)
                if n >= 0:
                    stage_c(n)
                if dr == 1 and n >= 1:
                    stage_d(n - 1)
                if n >= 0 and n % 10 == 5 and precast:
                    dst, srcw = precast.pop(0)
                    tk._wait("pool", tk._deps([(bSb, None)], []))
                    tk.dma("pool", [], [], lambda E: E.dma_start(out=dst.ap(), in_=srcw), sem="precast")
            if dr == 1:
                stage_d(N - 1)
        while precast:
            dst, srcw = precast.pop(0)
            tk.dma("pool", [], [], lambda E: E.dma_start(out=dst.ap(), in_=srcw), sem="precast")
        if debug is None:
            BWB.w = {None: (("x", "precast"), tk.ded["precast"][1])}
            BWB.r = {}
    tk.barrier()
    sc.close()
    if debug == "p2":
        for k in range(NCH):
            o = nc.dram_tensor("dbg_ymix%d" % k, [512, D], BF16, kind="ExternalOutput")
            dbg_out["ymix%d" % k] = o
            tk.dma("sp", [B["ymix_d"]], [Buf("x")], lambda E: E.dma_start(out=o.ap(), in_=ymix_d[k].ap()))
        return finish(None), dbg_out

    cwin_in = din("cw_in", [D, 3 * D])
    ck_in = din("ck", [128, 8, 3])
    cwo_in = din("cw_out", [D, D])
    rw_in = din("router_w", [2, D, 16])
    sel_in = din("sel", [4, 16])
    tid_in = din("tid", [128, NT], I32)
    lu_in = din("lu", [2, 128, 128])

    NCR = 16
    rows_loc = [dscr("rows_loc%d" % k, [256, RW], BF16) for k in range(NCR)]
    rows_all = [dscr("rows_all%d" % k, [1024, RW], BF16) for k in range(NCR)]
    zs_c = [dscr("zs_c%d" % k, [512, D], BF16) for k in range(NCH)]

    def rows_all_tile(gi):
        r, i = gi // NT, gi % NT
        k, w = i // 2, i % 2
        return rows_all[k][r * 256 + w * 128:r * 256 + (w + 1) * 128, :]

    tid_sb = sb("tid_sb", [128, NT], I32)
    btid = Buf("tid")
    tk.dma("sp", [], [btid], lambda E: E.dma_start(out=tid_sb[:], in_=tid_in.ap()))
    lub = sb("lub", [128, 128], BF16)
    luf = sb("luf", [128, 128], F32)
    tk.dma("sp", [], [(Bc, "luf")], lambda E: E.dma_start(out=luf[:], in_=lu_in[0, :, :]))
    tk.op("dve", [(Bc, "luf")], [(Bc, "lub")], lambda E: E.tensor_copy(out=lub[:], in_=luf[:]))

    class TailCtx:
        pass

    chunk_order = [list(range(NCR))]

    def tail_setup(sc, l):
        c = TailCtx()
        c.l = l
        c.order = []
        c.deferred = []
        c.gm2 = sc.sb("gm2_%d" % l, [128, D], F32); c.sh2 = sc.sb("sh2_%d" % l, [128, D], F32)
        c.tmpr = sc.sb("tmpr2_%d" % l, [128, D], F32)
        c.bgm2, c.bsh2, c.btmp = Buf("gm2"), Buf("sh2"), Buf("tmpr2")
        make_gm(c.gm2[:], c.bgm2, c.tmpr[:], c.btmp, l, 0, 4, 1)
        load_row("sp", c.sh2[:], c.bsh2, mod_row(l, 0, 3))
        c.rwf = sc.sb("rwf%d" % l, [128, 8, 16], F32); c.rwb = sc.sb("rwb%d" % l, [128, 8, 16], BF16)
        c.brw = Buf("rw")
        tk.dma("sp", [], [(c.brw, "f")], lambda E: E.dma_start(out=c.rwf[:], in_=rw_in[l, :, :].rearrange("(kc p) e -> p kc e", p=128)))
        tk.op("dve", [(c.brw, "f")], [(c.brw, "b")], lambda E: E.tensor_copy(out=c.rwb[:], in_=c.rwf[:]))
        c.rowt = [sc.sb("rowt%d_%d" % (l, k), [128, RW], BF16) for k in range(2)]
        c.brow = [Buf("rowt0"), Buf("rowt1")]
        for k in range(2):
            tk.op("pool", [], [c.brow[k]], lambda E: E.memset(c.rowt[k][:], 0.0))
        c.sq = sc.sb("sq2_%d" % l, [128, D], F32); c.st = sc.sb("st2t_%d" % l, [128, 8], F32)
        c.bsq, c.bst = Buf("sq2"), Buf("st2t")
        c.hT2 = sc.sb("hT2_%d" % l, [128, 8, 128], BF16); c.bhT2 = Buf("hT2")
        c.ex = sc.sb("ex_%d" % l, [128, 16], F32); c.bex = Buf("ex")
        c.affs = sc.sb("affs_%d" % l, [128, NT, 16], F32); c.baff = Buf("affs")
        c.xw = sc.sb("xw_%d" % l, [128, D], F32); c.bxw = Buf("xw")
        return c

    def tail(c, xt, bx, i, desc=False, split=False):
        rowt, brow = c.rowt[i % 2], c.brow[i % 2]
        tk.op("act", [bx], [c.bsq, c.bst], lambda E: E.activation(out=c.sq[:], in_=xt, func=AF.Square, accum_out=c.st[:, 0:1]))
        rstd_of(c.st[:, 0:1], c.st[:, 2:3], c.st[:, 1:2], c.bst, D)
        tk.op("dve", [bx, c.bst, c.bgm2], [c.bxw], lambda E: E.scalar_tensor_tensor(
            out=c.xw[:], in0=xt, scalar=c.st[:, 2:3], in1=c.gm2[:], op0=ALU.mult, op1=ALU.mult))
        tk.op("dve", [c.bxw, c.bsh2], [(brow, "h")], lambda E: E.tensor_tensor(out=rowt[:, 0:D], in0=c.xw[:], in1=c.sh2[:], op=ALU.add))
        if not split:
            tail2(c, i, desc)

    def tail2(c, i, desc=False):
        rowt, brow = c.rowt[i % 2], c.brow[i % 2]
        psT = PS[0][:].bitcast(BF16)
        for kc in range(8):
            tk.op("pe", [(brow, "h"), Bc], [PB[0]] if kc in (0, 7) else [],
                  lambda E: E.transpose(out=psT[:, kc * 128:(kc + 1) * 128], in_=rowt[:, kc * 128:(kc + 1) * 128], identity=ident[:]),
                  mark=(kc == 7))
        tk.op("act", [PB[0]], [c.bhT2], lambda E: E.activation(out=c.hT2[:].rearrange("p k t -> p (k t)"), in_=psT, func=AF.Copy))
        for kc in range(8):
            tk.op("pe", [c.bhT2, (c.brw, "b")], [PB[7]] if kc in (0, 7) else [],
                  lambda E: E.matmul(PS[7][:, 0:16], lhsT=c.hT2[:, kc, :], rhs=c.rwb[:, kc, :], start=(kc == 0), stop=(kc == 7)),
                  mark=(kc == 7))
        tk.op("dve", [PB[7]], [c.bst], lambda E: E.tensor_reduce(out=c.st[:, 3:4], in_=PS[7][:, 0:16], axis=AX.X, op=ALU.max))
        tk.op("dve", [c.bst], [c.bst], lambda E: E.tensor_scalar(out=c.st[:, 4:5], in0=c.st[:, 3:4], scalar1=-1.0, scalar2=0.0, op0=ALU.mult, op1=ALU.add))
        tk.op("act", [PB[7], c.bst], [c.bex, c.bst], lambda E: E.activation(out=c.ex[:], in_=PS[7][:, 0:16], func=AF.Exp, bias=c.st[:, 4:5], accum_out=c.st[:, 5:6]))
        tk.op("dve", [c.bst], [c.bst], lambda E: E.reciprocal(out=c.st[:, 6:7], in_=c.st[:, 5:6]))
        tk.op("dve", [c.bex, c.bst], [(c.baff, i)], lambda E: E.tensor_scalar(out=c.affs[:, i, :], in0=c.ex[:], scalar1=c.st[:, 6:7], scalar2=0.0, op0=ALU.mult, op1=ALU.add))
        tk.op("dve", [(c.baff, i)], [(brow, "a")], lambda E: E.tensor_copy(out=rowt[:, D:D + 32].bitcast(F32), in_=c.affs[:, i, :]))
        tk.op("dve", [btid], [(brow, "t")], lambda E: E.tensor_copy(out=rowt[:, D + 32:D + 34].bitcast(I32), in_=tid_sb[:, i:i + 1]))
        k, w = i // 2, i % 2
        tk.dma("sp", [brow], [(B["rows_loc"], i)], lambda E: E.dma_start(out=rows_loc[k][w * 128:(w + 1) * 128, :], in_=rowt[:]))
        if w == (0 if desc else 1):
            if len(c.order) < 6:
                tk.coll("AllGather", ALU.bypass, rows_loc[k].ap(), rows_all[k].ap(), [(B["rows_loc"], 2 * k), (B["rows_loc"], 2 * k + 1)], [(B["rows_all"], k)])
            else:
                c.deferred.append(k)
            c.order.append(k)

    def tail_finish(c):
        tk.dma("sp", [c.baff], [B["aff_loc"]], lambda E: E.dma_start(out=aff_loc.ap(), in_=c.affs[:]))
        tk.coll("AllGather", ALU.bypass, aff_loc.ap(), aff_all.ap(), [B["aff_loc"]], [B["aff_all"]])
        for k in c.deferred:
            tk.coll("AllGather", ALU.bypass, rows_loc[k].ap(), rows_all[k].ap(), [(B["rows_loc"], 2 * k), (B["rows_loc"], 2 * k + 1)], [(B["rows_all"], k)])
        chunk_order[0] = list(c.order)

    sc = Scope(nc)
    tc0 = tail_setup(sc, 0)
    xa = [sc.sb("xa%d" % k, [128, D], F32) for k in range(2)]
    bxa = [Buf("xa0"), Buf("xa1")]
    ym = [sc.sb("ym%d" % k, [128, D], BF16) for k in range(2)]
    bym = [Buf("ym0"), Buf("ym1")]
    for i in reversed(range(NT)):
        xt, bx, yt, by = xa[i % 2], bxa[i % 2], ym[i % 2], bym[i % 2]
        tk.dma("sp", [], [bx], lambda E: E.dma_start(out=xt[:], in_=x_in[i * 128:(i + 1) * 128, :]))
        tk.dma("sp", [(B["ymix_d"], i // 4)], [by], lambda E: E.dma_start(out=yt[:], in_=loc_rows(ymix_d, i)))
        tk.op("dve", [bx, by], [bx], lambda E: E.tensor_tensor(out=xt[:], in0=xt[:], in1=yt[:], op=ALU.add))
        tk.dma("sp", [bx], [(B["x1_d"], i)], lambda E: E.dma_start(out=x1_d[i * 128:(i + 1) * 128, :], in_=xt[:]))
        tail(tc0, xt[:], bx, i, desc=True)
    tail_finish(tc0)
    tk.barrier()
    sc.close()

    r_cap = nc.gpsimd.alloc_register("r_cap")
    nc.gpsimd.reg_mov(r_cap, CAP - 1)
    r_tok = nc.gpsimd.alloc_register("r_tok")
    nc.gpsimd.reg_mov(r_tok, T - 1)

    def moe(l):
        sc = Scope(nc)
        Aall = sc.sb("Aall", [128, 128, 16], F32); tmpA = sc.sb("tmpA", [128, 128, 16], F32)
        selt = sc.sb("selt", [128, 4, 16], F32); Asel = sc.sb("Asel", [128, 4, 128], F32)
        cmp = sc.sb("cmp", [128, 4, 128], F32)
        bs = sc.sb("bs", [128, 8, 4], F32)
        Mexp = sc.sb("Mexp", [128, 4, 128], BF16); Xs = sc.sb("Xs", [128, 4, 128], BF16)
        offf = sc.sb("offf", [128, 4, 128], F32); offi = sc.sb("offi", [128, 4, 128], I32)
        zt = sc.sb("zt", [128, D], BF16)
        bA, bsel, bAs, bcmp, bbs, bM, bXs, boff, bzt = (Buf(n) for n in ("Aall", "selt", "Asel", "cmp", "bs", "Mexp", "Xs", "off", "zt"))
        tk.op("pool", [], [bzt], lambda E: E.memset(zt[:], 0.0))
        for g in range(NTA):
            tk.dma("sp", [bzt], [(B["z_d"], "z%d" % g)], lambda E: E.dma_start(out=z_d[g * 128:(g + 1) * 128, :], in_=zt[:]))
        tk.dma("sp", [B["aff_all"]], [bA], lambda E: E.dma_start(
            out=Aall[:].rearrange("q (k i) e -> q k i e", k=4), in_=aff_all.ap().rearrange("(k q) i e -> q k i e", q=128)))

        tk.dma("sp", [], [bsel], lambda E: E.dma_start(out=selt[:], in_=sel_in.ap().partition_broadcast(128)))
        for e in range(4):
            tk.op("dve", [bA, bsel], [Buf("t")] and [bcmp], lambda E: E.tensor_tensor(
                out=tmpA[:], in0=Aall[:], in1=selt[:, e, :].unsqueeze(1).broadcast_to([128, 128, 16]), op=ALU.mult))
            tk.op("dve", [bcmp], [(bAs, e)], lambda E: E.tensor_reduce(out=Asel[:, e, :], in_=tmpA[:], axis=AX.X, op=ALU.add))
        lo, hi, mid, cntp, ge, dd = (bs[:, k, :] for k in range(6))
        tk.op("pool", [], [bbs], lambda E: E.memset(bs[:], 0.0))
        tk.op("pool", [bbs], [bbs], lambda E: E.memset(bs[:, 1, :], 1.0))
        for it in range(NITER):
            tk.op("dve", [bbs], [bbs], lambda E: E.tensor_tensor(out=mid, in0=lo, in1=hi, op=ALU.add))
            tk.op("dve", [bbs], [bbs], lambda E: E.tensor_scalar(out=mid, in0=mid, scalar1=0.5, scalar2=0.0, op0=ALU.mult, op1=ALU.add))
            tk.op("dve", [bAs, bbs], [bcmp], lambda E: E.tensor_tensor(
                out=cmp[:], in0=Asel[:], in1=bs[:, 2, :].unsqueeze(2).broadcast_to([128, 4, 128]), op=ALU.is_gt))
            tk.op("dve", [bcmp], [bbs], lambda E: E.tensor_reduce(out=cntp, in_=cmp[:], axis=AX.X, op=ALU.add))
            tk.op("pe", [bbs, (Bc, "of")], [PB[7]], lambda E: E.matmul(PS[7][:, 0:4], lhsT=ones_f[:], rhs=cntp, start=True, stop=True))
            tk.op("dve", [PB[7]], [bbs], lambda E: E.tensor_scalar(out=ge, in0=PS[7][:, 0:4], scalar1=float(CAP), scalar2=0.0, op0=ALU.is_ge, op1=ALU.add))
            tk.op("dve", [bbs], [bbs], lambda E: E.tensor_tensor(out=dd, in0=mid, in1=lo, op=ALU.subtract))
            tk.op("dve", [bbs], [bbs], lambda E: E.tensor_tensor(out=dd, in0=dd, in1=ge, op=ALU.mult))
            tk.op("dve", [bbs], [bbs], lambda E: E.tensor_tensor(out=lo, in0=lo, in1=dd, op=ALU.add))
            tk.op("dve", [bbs], [bbs], lambda E: E.tensor_tensor(out=dd, in0=hi, in1=mid, op=ALU.subtract))
            tk.op("dve", [bbs], [bbs], lambda E: E.tensor_tensor(out=dd, in0=dd, in1=ge, op=ALU.mult))
            tk.op("dve", [bbs], [bbs], lambda E: E.tensor_tensor(out=hi, in0=mid, in1=dd, op=ALU.add))
        for e in range(4):
            tk.op("dve", [bAs, bbs], [(bM, e)], lambda E: E.tensor_scalar(
                out=Mexp[:, e, :], in0=Asel[:, e, :], scalar1=bs[:, 0, e:e + 1], scalar2=0.0, op0=ALU.is_gt, op1=ALU.add))
        for e in range(4):
            tk.op("pe", [bM, (Bc, "ob")], [PB[1]], lambda E: E.matmul(PS[1][:, e * 128:(e + 1) * 128], lhsT=Mexp[:, e, :], rhs=ones_b[:], start=True, stop=True),
                  mark=(e == 3))
        tk.op("act", [PB[1]], [bXs], lambda E: E.activation(out=Xs[:].rearrange("p e t -> p (e t)"), in_=PS[1][:, :], func=AF.Copy))
        tk.op("pe", [bM, (Bc, "lub")], [PB[2]], lambda E: E.matmul(PS[2][:, :], lhsT=lub[:], rhs=Mexp[:].rearrange("p e t -> p (e t)"), start=True, stop=False), mark=False)
        for e in range(4):
            tk.op("pe", [bXs, (Bc, "lub")], [PB[2]] if e == 3 else [], lambda E: E.matmul(PS[2][:, e * 128:(e + 1) * 128], lhsT=Xs[:, e, :], rhs=lub[:], start=False, stop=(e == 3)),
                  mark=(e == 3))
        tk.op("dve", [bM], [boff], lambda E: E.tensor_scalar(out=offf[:], in0=Mexp[:], scalar1=-BIG, scalar2=BIG, op0=ALU.mult, op1=ALU.add))
        tk.op("dve", [boff, PB[2]], [boff], lambda E: E.tensor_tensor(out=offf[:].rearrange("p e t -> p (e t)"), in0=offf[:].rearrange("p e t -> p (e t)"), in1=PS[2][:, :], op=ALU.add))
        tk.op("dve", [boff], [(boff, "i")], lambda E: E.tensor_copy(out=offi[:], in_=offf[:]))

        NRT = 10
        rt = [sc.sb("rt%d" % k, [128, RW], BF16) for k in range(NRT)]
        brt = [Buf("rt%d" % k) for k in range(NRT)]
        kctr = [0]

        gi_order = [r_ * NT + 2 * ck + w_ for ck in chunk_order[0] for r_ in range(4) for w_ in range(2)]

        def scatter_rows(e):
            for n_, gi in enumerate(gi_order):
                k = kctr[0] % NRT
                kctr[0] += 1
                tk.dma("sp", [(B["rows_all"], (gi % NT) // 2)], [brt[k]], lambda E: E.dma_start(out=rt[k][:], in_=rows_all_tile(gi)))
                tk.dma("pool", [brt[k], (boff, "i")] + ([] if n_ == 0 else [(B["xe_d"], e)]), [(B["xe_d"], e)] if n_ == 0 else [], lambda E: E.indirect_dma_start(
                    out=xe_d[e][:, :], out_offset=bass.IndirectOffsetOnAxis(ap=offi[:, e, gi:gi + 1], axis=0),
                    in_=rt[k][:], in_offset=None, bounds_check=r_cap, oob_is_err=False))

        Wg = sc.sb("Wg", [128, 8, 2 * D], BF16); Wu = sc.sb("Wu", [128, 8, 2 * D], BF16); Wd = sc.sb("Wd", [128, 16, D], BF16)
        bWg, bWu, bWd = Buf("Wg"), Buf("Wu"), Buf("Wd")
        XeT = sc.sb("XeT", [128, 8, 512], BF16); hidT = sc.sb("hidT", [128, 16, 512], BF16)
        bXeT, bhid = Buf("XeT"), Buf("hidT")
        xr = [sc.sb("xr%d" % k, [128, RW], BF16) for k in range(4)]
        bxr = [Buf("xr%d" % k) for k in range(4)]
        sg = [sc.sb("sg%d" % k, [128, 512], F32) for k in range(2)]
        bsg = [Buf("sg0"), Buf("sg1")]
        ysb = [sc.sb("ysb%d" % k, [128, D], BF16) for k in range(2)]
        bys = [Buf("ys0"), Buf("ys1")]
        val = sc.sb("val", [128, 16], F32); idxs = sc.sb("idxs", [128, 16], I32); t16 = sc.sb("t16", [128, 16], F32)
        bval, bidx, bt16 = Buf("val"), Buf("idxs"), Buf("t16")

        def load_gu(e, q="sp"):
            tk.dma(q, [(BWB, "g%d%d" % (l, e))], [bWg], lambda E: E.dma_start(out=Wg[:], in_=wgb_d[l][e].ap().rearrange("(kc p) f -> p kc f", p=128)), sem="wg")
            tk.dma(q, [(BWB, "u%d%d" % (l, e))], [bWu], lambda E: E.dma_start(out=Wu[:], in_=wub_d[l][e].ap().rearrange("(kc p) f -> p kc f", p=128)), sem="wu")

        def load_d(e):
            tk.dma("sp", [(BWB, "d%d%d" % (l, e))], [bWd], lambda E: E.dma_start(out=Wd[:], in_=wdb_d[l][e].ap().rearrange("(fc p) f -> p fc f", p=128)), sem="wd")

        yk = [0]

        def ffn(e):
            def xe_loads(p):
                for stt in range(4):
                    s_ = p * 4 + stt
                    xw, bxw = xr[stt], bxr[stt]
                    tk.dma("act", [] if s_ == 0 else [(B["xe_d"], e)], [bxw] + ([(B["xe_d"], e)] if s_ == 0 else []),
                           lambda E: E.dma_start(out=xw[:], in_=xe_d[e][s_ * 128:(s_ + 1) * 128, :]))

            xe_loads(0)
            for p in range(4):
                for stt in range(4):
                    s_ = p * 4 + stt
                    xw, bxw = xr[stt], bxr[stt]
                    psT = PS[0][:].bitcast(BF16)
                    for kc in range(8):
                        tk.op("pe", [bxw, Bc], [PB[0]] if kc in (0, 7) else [],
                              lambda E: E.transpose(out=psT[:, kc * 128:(kc + 1) * 128], in_=xw[:, kc * 128:(kc + 1) * 128], identity=ident[:]),
                              mark=(kc == 7))
                    tk.op("act", [PB[0]], [(bXeT, stt)], lambda E: E.activation(
                        out=XeT[:, :, stt * 128:(stt + 1) * 128], in_=psT.rearrange("p (k t) -> p k t", k=8), func=AF.Copy))
                    tk.op("dve", [bxw, bsel], [bt16], lambda E: E.tensor_tensor(out=t16[:], in0=xw[:, D:D + 32].bitcast(F32), in1=selt[:, e, :], op=ALU.mult))
                    tk.op("dve", [bt16], [(bval, s_)], lambda E: E.tensor_reduce(out=val[:, s_:s_ + 1], in_=t16[:], axis=AX.X, op=ALU.add))
                    tk.op("dve", [bxw], [(bidx, s_)], lambda E: E.tensor_copy(out=idxs[:, s_:s_ + 1], in_=xw[:, D + 32:D + 34].bitcast(I32)))
                if p + 1 < 4:
                    xe_loads(p + 1)
                for fc in range(16):
                    pg, pbg = PS[1 + (fc % 2) * 2], PB[1 + (fc % 2) * 2]
                    pu, pbu = PS[2 + (fc % 2) * 2], PB[2 + (fc % 2) * 2]
                    for (W_, bW_, ps, pb) in ((Wg, bWg, pg, pbg), (Wu, bWu, pu, pbu)):
                        for kc in range(8):
                            tk.op("pe", [bXeT, (bW_, kc)], [pb] if kc in (0, 7) else [],
                                  lambda E: E.matmul(ps[:, :], lhsT=W_[:, kc, fc * 128:(fc + 1) * 128], rhs=XeT[:, kc, :], start=(kc == 0), stop=(kc == 7)),
                                  mark=(kc == 7))
                    sgt, bsgt = sg[fc % 2], bsg[fc % 2]
                    tk.op("act", [pbg], [bsgt], lambda E: E.activation(out=sgt[:], in_=pg[:, :], func=AF.Silu))
                    tk.op("dve", [pbu, bsgt], [(bhid, fc)], lambda E: E.tensor_tensor(out=hidT[:, fc, :], in0=pu[:, :], in1=sgt[:], op=ALU.mult))
                if p == 3 and e + 1 < 4:
                    load_gu(e + 1, "act")
                for stt in range(4):
                    s_ = p * 4 + stt
                    yt, byt = ysb[yk[0] % 2], bys[yk[0] % 2]
                    yk[0] += 1
                    for hf in range(2):
                        ps, pb = PS[5 + hf], PB[5 + hf]
                        for fc in range(16):
                            tk.op("pe", [bhid, (bWd, fc)], [pb] if fc in (0, 15) else [],
                                  lambda E: E.matmul(ps[:, :], lhsT=hidT[:, fc, stt * 128:(stt + 1) * 128], rhs=Wd[:, fc, hf * 512:(hf + 1) * 512], start=(fc == 0), stop=(fc == 15)),
                                  mark=(fc == 15))
                        tk.op("act", [pb, (bval, s_)], [(byt, hf)], lambda E: E.activation(out=yt[:, hf * 512:(hf + 1) * 512], in_=ps[:, :], func=AF.Copy, scale=val[:, s_:s_ + 1]))
                    first = (p == 0 and stt == 0)
                    tk.dma("pool", [byt, (bidx, s_)] + ([] if first else [B["z_d"]]), [B["z_d"]] if first else [],
                           lambda E: E.indirect_dma_start(
                               out=z_d[:, :], out_offset=bass.IndirectOffsetOnAxis(ap=idxs[:, s_:s_ + 1], axis=0),
                               in_=yt[:], in_offset=None, bounds_check=r_tok, oob_is_err=False, compute_op=ALU.add))

        load_gu(0)
        load_d(0)
        scatter_rows(0)
        for e in range(4):
            if e + 1 < 4:
                scatter_rows(e + 1)
            ffn(e)
            if e + 1 < 4:
                load_d(e + 1)
        for k in range(NCH):
            tk.coll("ReduceScatter", ALU.add, z_d[k * 2048:(k + 1) * 2048, :], zs_c[k].ap(), [B["z_d"]], [(B["zs_d"], k)])
        tk.barrier()
        sc.close()

    moe(0)

    sc = Scope(nc)
    tc1 = tail_setup(sc, 1)
    g2r = sc.sb("g2r", [128, D], F32); bg2 = Buf("g2r")
    load_row("sp", g2r[:], bg2, mod_row(0, 0, 5))
    gmA = sc.sb("gmA", [128, D], F32); shA = sc.sb("shA", [128, D], F32)
    bgmA, bshA = Buf("gmA"), Buf("shA")
    make_gm(gmA[:], bgmA, tc1.tmpr[:], tc1.btmp, 1, 0, 1, 0)
    load_row("sp", shA[:], bshA, mod_row(1, 0, 0))
    g1B = sc.sb("g1B", [128, D], F32); bg1B = Buf("g1B")
    load_row("sp", g1B[:], bg1B, mod_row(1, 0, 2))
    cwi = sc.sb("cwi", [128, 8, 3 * D], BF16); bcwi = Buf("cwi")
    for kc in range(8):
        tk.dma("pool", [], [(bcwi, kc)], lambda E: E.dma_start(out=cwi[:, kc, :], in_=cwin_in[kc * 128:(kc + 1) * 128, :]))
    cwo = sc.sb("cwo", [128, 8, D], BF16); bcwo = Buf("cwo")
    cks = sc.sb("cks", [128, 8, 3], F32); bck = Buf("cks")
    tk.dma("sp", [], [bck], lambda E: E.dma_start(out=cks[:], in_=ck_in.ap()))
    for kc in range(8):
        tk.dma("sp", [], [tc1.bxw], lambda E: E.dma_start(out=tc1.xw[:], in_=cwo_in[kc * 128:(kc + 1) * 128, :]))
        tk.op("dve", [tc1.bxw, bg1B], [(bcwo, kc)], lambda E: E.tensor_tensor(out=cwo[:, kc, :], in0=tc1.xw[:], in1=g1B[:], op=ALU.mult))
    x4 = [[sc.sb("x4_%d_%d" % (a, k), [128, D], F32) for k in range(4)] for a in range(3)]
    bx4 = [[Buf("x4_%d_%d" % (a, k)) for k in range(4)] for a in range(3)]
    zb = [sc.sb("zb%d" % k, [128, D], BF16) for k in range(2)]
    bzb = [Buf("zb0"), Buf("zb1")]
    hb5 = sc.sb("hb5", [128, D], BF16); bhb5 = Buf("hb5")
    hT4 = [sc.sb("hT4_%d" % a, [128, 8, 512], BF16) for a in range(2)]
    bhT4 = [Buf("hT4_0"), Buf("hT4_1")]
    cgs = sc.sb("cgs", [128, 512], F32); us = sc.sb("us", [128, 512], F32); ws = sc.sb("ws", [128, 512], F32)
    bcgs, bus, bws = Buf("cgs"), Buf("us"), Buf("ws")
    zTs = [sc.sb("zT%d" % a, [128, 8, 512], BF16) for a in range(2)]
    bzTs = [Buf("zT0"), Buf("zT1")]
    sq5 = sc.sb("sq5", [128, D], F32); st5 = sc.sb("st5", [128, 8], F32); bsq5, bst5 = Buf("sq5"), Buf("st5")
    xw5, bxw5 = tc1.tmpr, tc1.btmp

    def prep_tile(grp, w):
        i = grp * 4 + w
        a = grp % 2
        xt, bx, zt_, bz = x4[grp % 3][w], bx4[grp % 3][w], zb[i % 2], bzb[i % 2]
        tk.dma("sp", [(B["x1_d"], i)], [bx], lambda E: E.dma_start(out=xt[:], in_=x1_d[i * 128:(i + 1) * 128, :]))
        tk.dma("sp", [(B["zs_d"], i // 4)], [bz], lambda E: E.dma_start(out=zt_[:], in_=loc_rows(zs_c, i)))
        tk.op("dve", [bz, bg2], [bxw5], lambda E: E.tensor_tensor(out=xw5[:], in0=zt_[:], in1=g2r[:], op=ALU.mult))
        tk.op("dve", [bxw5, bx], [bx], lambda E: E.tensor_tensor(out=xt[:], in0=xt[:], in1=xw5[:], op=ALU.add))
        tk.op("act", [bx], [bsq5, bst5], lambda E: E.activation(out=sq5[:], in_=xt[:], func=AF.Square, accum_out=st5[:, 0:1]))
        rstd_of(st5[:, 0:1], st5[:, 2:3], st5[:, 1:2], bst5, D)
        tk.op("dve", [bx, bst5, bgmA], [bxw5], lambda E: E.scalar_tensor_tensor(
            out=xw5[:], in0=xt[:], scalar=st5[:, 2:3], in1=gmA[:], op0=ALU.mult, op1=ALU.mult))
        tk.op("dve", [bxw5, bshA], [bhb5], lambda E: E.tensor_tensor(out=hb5[:], in0=xw5[:], in1=shA[:], op=ALU.add))

    def prep_tile2(grp, w):
        a = grp % 2
        psT = PS[4][:].bitcast(BF16)
        for kc in range(8):
            tk.op("pe", [bhb5, Bc], [PB[4]] if kc in (0, 7) else [],
                  lambda E: E.transpose(out=psT[:, kc * 128:(kc + 1) * 128], in_=hb5[:, kc * 128:(kc + 1) * 128], identity=ident[:]),
                  mark=(kc == 7))
        tk.op("act", [], [(bhT4[a], w), PB[4]], lambda E: E.activation(
            out=hT4[a][:, :, w * 128:(w + 1) * 128], in_=psT.rearrange("p (k t) -> p k t", k=8), func=AF.Copy))

    def conv_cc(grp, cc):
        a = grp % 2
        for (sec, ps, pb) in ((0, PS[1], PB[1]), (1, PS[2], PB[2]), (2, PS[3], PB[3])):
            c0 = sec * D + cc * 128
            for kc in range(8):
                tk.op("pe", [bhT4[a], (bcwi, kc)], [pb] if kc in (0, 7) else [],
                      lambda E: E.matmul(ps[:, :], lhsT=cwi[:, kc, c0:c0 + 128], rhs=hT4[a][:, kc, :], start=(kc == 0), stop=(kc == 7)),
                      mark=(kc == 7))
        tk.op("act", [], [bcgs, PB[2]], lambda E: E.activation(out=cgs[:], in_=PS[2][:, :], func=AF.Copy))
        tk.op("dve", [bcgs], [bus, PB[3]], lambda E: E.tensor_tensor(out=us[:], in0=cgs[:], in1=PS[3][:, :], op=ALU.mult))
        tk.op("act", [bus, bck], [bws], lambda E: E.activation(out=ws[:], in_=us[:], func=AF.Copy, scale=cks[:, cc, 1:2]))
        u3 = us[:].rearrange("p (r t) -> p r t", t=64)
        w3 = ws[:].rearrange("p (r t) -> p r t", t=64)
        tk.op("dve", [bus, bws, bck], [bws], lambda E: E.scalar_tensor_tensor(
            out=w3[:, :, 1:64], in0=u3[:, :, 0:63], scalar=cks[:, cc, 0:1], in1=w3[:, :, 1:64], op0=ALU.mult, op1=ALU.add))
        tk.op("dve", [bus, bws, bck], [bws], lambda E: E.scalar_tensor_tensor(
            out=w3[:, :, 0:63], in0=u3[:, :, 1:64], scalar=cks[:, cc, 2:3], in1=w3[:, :, 0:63], op0=ALU.mult, op1=ALU.add))
        tk.op("dve", [bws], [(bzTs[a], cc), PB[1]], lambda E: E.tensor_tensor(out=zTs[a][:, cc, :], in0=ws[:], in1=PS[1][:, :], op=ALU.mult))

    def post_tile(grp, w):
        i = grp * 4 + w
        a = grp % 2
        xt, bx = x4[grp % 3][w], bx4[grp % 3][w]
        zT, bzT = zTs[a], bzTs[a]
        for hf in range(2):
            ps, pb = PS[5 + hf], PB[5 + hf]
            for cc in range(8):
                tk.op("pe", [bzT, (bcwo, cc)], [pb] if cc in (0, 7) else [],
                      lambda E: E.matmul(ps[:, :], lhsT=zT[:, cc, w * 128:(w + 1) * 128], rhs=cwo[:, cc, hf * 512:(hf + 1) * 512], start=(cc == 0), stop=(cc == 7)),
                      mark=(cc == 7))
            tk.op("dve", [], [bx, pb], lambda E: E.tensor_tensor(out=xt[:, hf * 512:(hf + 1) * 512], in0=xt[:, hf * 512:(hf + 1) * 512], in1=ps[:, :], op=ALU.add))
        tk.dma("sp", [bx], [(B["x1_d"], i)], lambda E: E.dma_start(out=x1_d[i * 128:(i + 1) * 128, :], in_=xt[:]))
        tail(tc1, xt[:], bx, i, split=True)

    def post_tile2(grp, w):
        tail2(tc1, grp * 4 + w)

    for w in range(4):
        prep_tile(0, w)
        prep_tile2(0, w)
    NG = NT // 4
    for grp in range(NG):
        for cc in range(8):
            conv_cc(grp, cc)
            w = cc // 2
            if cc % 2 == 0:
                if grp >= 1:
                    post_tile(grp - 1, w)
                if grp + 1 < NG and w >= 1:
                    prep_tile2(grp + 1, w - 1)
            else:
                if grp >= 1:
                    post_tile2(grp - 1, w)
                if grp + 1 < NG:
                    prep_tile(grp + 1, w)
        if grp + 1 < NG:
            prep_tile2(grp + 1, 3)
    for w in range(4):
        post_tile(NG - 1, w)
        post_tile2(NG - 1, w)
    tail_finish(tc1)
    tk.barrier()
    sc.close()

    moe(1)

    sc = Scope(nc)
    g2r = sc.sb("g2r7", [128, D], F32); bg2 = Buf("g2r7")
    load_row("sp", g2r[:], bg2, mod_row(1, 0, 5))
    fgr = sc.sb("fgr", [128, D], F32); bfg = Buf("fgr")
    tk.dma("sp", [], [bfg], lambda E: E.dma_start(out=fgr[:], in_=fng_in.ap().partition_broadcast(128)))
    xa = [sc.sb("xa7_%d" % k, [128, D], F32) for k in range(2)]
    bxa = [Buf("xa7_0"), Buf("xa7_1")]
    zb = [sc.sb("zb7_%d" % k, [128, D], BF16) for k in range(2)]
    bzb = [Buf("zb7_0"), Buf("zb7_1")]
    xw7 = sc.sb("xw7", [128, D], F32); bxw7 = Buf("xw7")
    sq7 = sc.sb("sq7", [128, D], F32); st7 = sc.sb("st7", [128, 8], F32); bsq7, bst7 = Buf("sq7"), Buf("st7")
    for i in range(NT):
        xt, bx, zt_, bz = xa[i % 2], bxa[i % 2], zb[i % 2], bzb[i % 2]
        tk.dma("sp", [(B["x1_d"], i)], [bx], lambda E: E.dma_start(out=xt[:], in_=x1_d[i * 128:(i + 1) * 128, :]))
        tk.dma("sp", [(B["zs_d"], i // 4)], [bz], lambda E: E.dma_start(out=zt_[:], in_=loc_rows(zs_c, i)))
        tk.op("dve", [bz, bg2], [bxw7], lambda E: E.tensor_tensor(out=xw7[:], in0=zt_[:], in1=g2r[:], op=ALU.mult))
        tk.op("dve", [bxw7, bx], [bx], lambda E: E.tensor_tensor(out=xt[:], in0=xt[:], in1=xw7[:], op=ALU.add))
        tk.op("act", [bx], [bsq7, bst7], lambda E: E.activation(out=sq7[:], in_=xt[:], func=AF.Square, accum_out=st7[:, 0:1]))
        rstd_of(st7[:, 0:1], st7[:, 2:3], st7[:, 1:2], bst7, D)
        tk.op("dve", [bx, bst7, bfg], [bx], lambda E: E.scalar_tensor_tensor(
            out=xt[:], in0=xt[:], scalar=st7[:, 2:3], in1=fgr[:], op0=ALU.mult, op1=ALU.mult))
        tk.dma("sp", [bx], [(B["out"], i)], lambda E: E.dma_start(out=out_d[i * 128:(i + 1) * 128, :], in_=xt[:]))
    tk.barrier()
    sc.close()
    return nc, dbg_out


def _consts():
    j = np.arange(128)[:, None]
    i = np.arange(128)[None, :]
    c = -1.0 / 16.0
    tri = np.stack([
        (j <= i) * c, (j >= i) * c,
        (j > i) * c, (j < i) * c,
        (j <= i) * 1.0, (j >= i) * 1.0,
    ]).astype(np.float32)
    lu = np.stack([(j < i) * 1.0, (j < i) * 1.0]).astype(np.float32)
    return tri, lu


def make_in_maps(inp):
    f = lambda a: np.ascontiguousarray(np.asarray(a, dtype=np.float32))
    x, c, ctx, c_ctx = f(inp["x"]), f(inp["c"]), f(inp["ctx"]), f(inp["c_ctx"])
    ada_w, ada_b, norm_g = f(inp["ada_w"]), f(inp["ada_b"]), f(inp["norm_g"])
    w_in, w_a2, b_a2 = f(inp["gla_w_in"])[0], f(inp["gla_w_a2"])[0], f(inp["gla_b_a2"])[0]
    gng, w_out = f(inp["gla_norm_g"])[0], f(inp["gla_w_out"])[0]
    cw_in, conv_k, cw_out = f(inp["conv_w_in"])[0], f(inp["conv_k"])[0], f(inp["conv_w_out"])[0]
    router_w = f(inp["router_w"])
    wg, wu, wd = inp["expert_w_gate"], inp["expert_w_up"], inp["expert_w_down"]
    fng = f(inp["final_norm_g"])
    tri, lu = _consts()
    ident = np.eye(128, dtype=np.float32)
    maps = []
    for core in range(8):
        b, j = core // 4, core % 4
        cc = np.stack([c[b].reshape(8, 128).T, c_ctx.reshape(8, 128).T], axis=-1)
        wfm = np.concatenate([w_in[:, j * 128:(j + 1) * 128], w_in[:, 512 + j * 128:512 + (j + 1) * 128],
                              w_in[:, 3072:3104]], axis=1)
        wtm = np.concatenate([w_in[:, 512 + j * 128:512 + (j + 1) * 128], w_in[:, 1024 + j * 256:1024 + (j + 1) * 256],
                              w_in[:, 2048 + j * 256:2048 + (j + 1) * 256]], axis=1)
        a2 = np.zeros((2, 32, 128), np.float32)
        a2[:, 0:16, :] = w_a2[:, :, j * 128:(j + 1) * 128]
        a2[:, 16, :] = b_a2[:, j * 128:(j + 1) * 128]
        gn = gng.reshape(2, 128).T
        sel = np.zeros((4, 16), np.float32)
        for e in range(4):
            sel[e, 4 * j + e] = 1.0
        ii = np.arange(NT)[None, :]
        tid = ((ii // 4) * 2048 + j * 512 + (ii % 4) * 128 + np.arange(128)[:, None]).astype(np.int32)
        m = {
            "x": x[b].reshape(NT, 4, 128, D)[:, j].reshape(TL, D), "ctx": ctx[b], "cc": cc, "ada_w": ada_w, "ada_b": ada_b,
            "norm_g": norm_g, "final_g": fng, "wfm": wfm, "wtm": wtm, "a2": a2, "gn": gn,
            "wo": w_out[j * 256:(j + 1) * 256], "cw_in": cw_in, "ck": conv_k.reshape(3, 8, 128).transpose(2, 1, 0),
            "cw_out": cw_out, "router_w": router_w,
            "wg": np.asarray(wg[:, 4 * j:4 * j + 4], dtype=np.float32), "wu": np.asarray(wu[:, 4 * j:4 * j + 4], dtype=np.float32),
            "wd": np.asarray(wd[:, 4 * j:4 * j + 4], dtype=np.float32),
            "sel": sel, "tid": tid, "ident": ident, "tri": tri, "lu": lu,
        }
        maps.append({k: np.ascontiguousarray(v) for k, v in m.items()})
    return maps


_USED = None


def kernel(**inputs):
    nc, _ = build()
    maps = make_in_maps(inputs)
    used = set(nc._in_names)
    maps = [{k: v for k, v in m.items() if k in used} for m in maps]
    res = run_bass_kernel_spmd(nc, maps, core_ids=list(range(8)))
    out = np.zeros((2, T, D), np.float32)
    for core in range(8):
        b, j = core // 4, core % 4
        out[b].reshape(NT, 4, 128, D)[:, j] = np.asarray(res.results[core]["out"], dtype=np.float32).reshape(NT, 128, D)
    return out


def _input_names(nc):
    return ["x", "ctx", "cc", "ada_w", "ada_b", "norm_g", "final_g", "wfm", "wtm", "a2", "gn", "wo", "cw_in", "ck",
            "cw_out", "router_w", "wg", "wu", "wd", "sel", "tid", "ident", "tri", "lu"]
```

```python
import os
import numpy as np
import ml_dtypes
import concourse.bass as bass
import concourse.mybir as mybir
from concourse.bass_utils import run_bass_kernel_spmd

F32 = mybir.dt.float32
BF16 = mybir.dt.bfloat16
I32 = mybir.dt.int32
AF = mybir.ActivationFunctionType
ALU = mybir.AluOpType
AX = mybir.AxisListType

D = 1024
T = 16384
TL = 4096
NT = 32
NTA = 128
CTX = 256
RW = 1088
CAP = 2048
GROUPS = [[0, 1, 2, 3], [4, 5, 6, 7]]
NDS = 24
BIG = 1.0e6
NITER = 24


class Scope:
    ctr = 0

    def __init__(self, nc):
        self.nc = nc
        self.items = []

    def sb(self, name, shape, dt):
        Scope.ctr += 1
        t = self.nc.sbuf_tensor("sb%d_%s" % (Scope.ctr, name), list(shape), dt)
        self.items.append(t)
        return t.__enter__()

    def close(self):
        for t in reversed(self.items):
            t.__exit__(None, None, None)
        self.items = []


class Buf:
    __slots__ = ("name", "w", "r")

    def __init__(self, name):
        self.name = name
        self.w = {}
        self.r = {}


def _norm(items):
    out = []
    for it in items:
        if isinstance(it, tuple):
            out.append(it)
        else:
            out.append((it, None))
    return out


class TK:
    def __init__(self, nc):
        self.nc = nc
        self.E = {"pe": nc.tensor, "act": nc.scalar, "dve": nc.vector, "pool": nc.gpsimd, "sp": nc.sync}
        self.sem = {k: nc.semaphore("s_" + k).__enter__() for k in self.E}
        self.cnt = {k: 0 for k in self.E}
        self.dpool = {"sp": 16, "pool": 12, "act": 4}
        self.dsem = {q: [nc.semaphore("d%s%d" % (q, i)).__enter__() for i in range(n)] for q, n in self.dpool.items()}
        self.dcnt = {q: [0] * n for q, n in self.dpool.items()}
        self.dnext = {q: 0 for q in self.dpool}
        self.ded = {}
        self.waited = {k: {} for k in self.E}
        self.ncc = 0

    def _semobj(self, key):
        if key[0] == "e":
            return self.sem[key[1]]
        if key[0] == "d":
            return self.dsem[key[1]][key[2]]
        if key[0] == "x":
            return self.ded[key[1]][0]
        return key[1]

    def _deps(self, reads, writes):
        deps = []
        for b, p in reads:
            for q, d in b.w.items():
                if p is None or q is None or p == q:
                    deps.append(d)
        for b, p in writes:
            for q, d in b.w.items():
                if p is None or q is None or p == q:
                    deps.append(d)
            for q, dd in b.r.items():
                if p is None or q is None or p == q:
                    deps.extend(dd.items())
        return deps

    def _wait(self, eng, deps):
        E = self.E[eng]
        best = {}
        for k, v in deps:
            if k == ("e", eng) and v > self.cnt[eng]:
                continue
            if best.get(k, 0) < v:
                best[k] = v
        for k, v in best.items():
            if self.waited[eng].get(k, 0) < v:
                E.wait_ge(self._semobj(k), v)
                self.waited[eng][k] = v

    def _record(self, reads, writes, dep):
        k, v = dep
        for b, p in reads:
            dd = b.r.setdefault(p, {})
            if dd.get(k, 0) < v:
                dd[k] = v
        for b, p in writes:
            if p is None:
                b.w = {None: dep}
                b.r = {}
            else:
                b.w[p] = dep
                b.r.pop(p, None)

    def op(self, eng, reads, writes, emit, mark=True):
        reads = _norm(reads)
        writes = _norm(writes)
        self._wait(eng, self._deps(reads, writes))
        inst = emit(self.E[eng])
        dep = (("e", eng), self.cnt[eng] + 1)
        if mark:
            self.cnt[eng] += 1
            inst.then_inc(self.sem[eng], 1)
        self._record(reads, writes, dep)
        return inst

    def dma(self, q, reads, writes, emit, sem=None):
        reads = _norm(reads)
        writes = _norm(writes)
        self._wait(q, self._deps(reads, writes))
        inst = emit(self.E[q])
        if sem is not None:
            if sem not in self.ded:
                self.ded[sem] = [self.nc.semaphore("x_" + sem).__enter__(), 0]
            self.ded[sem][1] += 16
            inst.then_inc(self.ded[sem][0], 16)
            dep = (("x", sem), self.ded[sem][1])
        else:
            i = self.dnext[q]
            self.dnext[q] = (i + 1) % self.dpool[q]
            self.dcnt[q][i] += 16
            inst.then_inc(self.dsem[q][i], 16)
            dep = (("d", q, i), self.dcnt[q][i])
        self._record(reads, writes, dep)
        return inst

    def coll(self, kind, op, in_ap, out_ap, reads, writes, qos=None):
        reads = _norm(reads)
        writes = _norm(writes)
        self._wait("pool", self._deps(reads, writes))
        if self.ncc == 0:
            self.ccsem = self.nc.semaphore("ccsem").__enter__()
        self.ncc += 1
        kw = {} if qos is None else {"dma_qos": qos}
        self.nc.gpsimd.collective_compute(kind, op, replica_groups=GROUPS, ins=[in_ap], outs=[out_ap], **kw).then_inc(self.ccsem)
        self._record(reads, writes, (("c", self.ccsem), self.ncc))

    def barrier(self):
        deps = [(("e", k), self.cnt[k]) for k in self.E if self.cnt[k] > 0]
        deps += [(("d", q, i), self.dcnt[q][i]) for q in self.dpool for i in range(self.dpool[q]) if self.dcnt[q][i] > 0]
        deps += [(("x", n), v[1]) for n, v in self.ded.items()]
        for eng in self.E:
            self._wait(eng, deps)


def build(debug=None):
    nc = bass.Bass("TRN2", target_bir_lowering=False)
    tk = TK(nc)
    dbg_out = {}

    in_names = []
    nc._in_names = in_names

    def din(name, shape, dt=F32):
        in_names.append(name)
        return nc.dram_tensor(name, list(shape), dt, kind="ExternalInput")

    def dscr(name, shape, dt):
        return nc.dram_tensor(name, list(shape), dt)

    x_in = din("x", [TL, D])
    ctx_in = din("ctx", [CTX, D])
    cc_in = din("cc", [128, 8, 2])
    adaw_in = din("ada_w", [2, D, 6 * D])
    adab_in = din("ada_b", [2, 6 * D])
    ng_in = din("norm_g", [2, 2, D])
    fng_in = din("final_g", [D])
    wfm_in = din("wfm", [D, 288])
    wtm_in = din("wtm", [D, 640])
    a2_in = din("a2", [2, 32, 128])
    gn_in = din("gn", [128, 2])
    wo_in = din("wo", [256, D])
    ident_in = din("ident", [128, 128])
    tri_in = din("tri", [6, 128, 128])
    wg_in = din("wg", [2, 4, D, 2 * D])
    wu_in = din("wu", [2, 4, D, 2 * D])
    wd_in = din("wd", [2, 4, 2 * D, D])
    out_d = nc.dram_tensor("out", [TL, D], F32, kind="ExternalOutput")

    mod_d = dscr("mod_d", [2, 2, 6 * D], F32)
    NCH = 8
    h1_loc = [dscr("h1_loc%d" % k, [TL // NCH, D], BF16) for k in range(NCH)]
    h1_all = [dscr("h1_all%d" % k, [4 * TL // NCH, D], BF16) for k in range(NCH)]
    hc_d = dscr("hc_d", [CTX, D], BF16)
    of_d = dscr("of_d", [T, 256], F32)
    yp_d = [dscr("yp_d%d" % k, [4 * TL // NCH, D], BF16) for k in range(NCH)]
    ymix_d = [dscr("ymix_d%d" % k, [TL // NCH, D], BF16) for k in range(NCH)]
    x1_d = dscr("x1_d", [TL, D], F32)
    aff_loc = dscr("aff_loc", [128, NT, 16], F32)
    aff_all = dscr("aff_all", [4 * 128, NT, 16], F32)
    xe_d = [dscr("xe_d%d" % e, [CAP, RW], BF16) for e in range(4)]
    z_d = dscr("z_d", [T, D], BF16)

    fence_in = dscr("fence_in", [128, 64], BF16)
    fence_out = [dscr("fence_out%d" % k, [512, 64], BF16) for k in range(8)]
    fence_n = [0]

    def fence(buf):
        k = fence_n[0]
        fence_n[0] += 1
        tk.coll("AllGather", ALU.bypass, fence_in.ap(), fence_out[k].ap(), [], [buf])

    def loc_rows(lst, i):
        k, w = i // 4, i % 4
        return lst[k][w * 128:(w + 1) * 128, :]

    def all_rows(lst, gi):
        r, i = gi % 4, gi // 4
        k, w = i // 4, i % 4
        return lst[k][r * 512 + w * 128:r * 512 + (w + 1) * 128, :]

    wgb_d = [[dscr("wgb_%d_%d" % (l, e), [D, 2 * D], BF16) for e in range(4)] for l in range(2)]
    wub_d = [[dscr("wub_%d_%d" % (l, e), [D, 2 * D], BF16) for e in range(4)] for l in range(2)]
    wdb_d = [[dscr("wdb_%d_%d" % (l, e), [2 * D, D], BF16) for e in range(4)] for l in range(2)]
    BWB = Buf("wb16")

    B = {n: Buf(n) for n in ["mod_d", "h1_loc", "h1_all", "hc_d", "of_d", "yp_d", "ymix_d", "x1_d", "rows_loc",
                             "rows_all", "aff_loc", "aff_all", "xe_d", "z_d", "zs_d", "out"]}

    def dbg_dump(name, src, shape, dt):
        o = nc.dram_tensor("dbg_" + name, list(shape), dt, kind="ExternalOutput")
        dbg_out[name] = o
        ob = Buf("dbg_" + name)
        tk.dma("sp", [B[name]], [ob], lambda E: E.dma_start(out=o.ap(), in_=src.ap()))
        return ob

    def finish(extra):
        tk.barrier()
        return nc

    def sb(name, shape, dt):
        return nc.sbuf_tensor("sb_" + name, list(shape), dt).__enter__()

    ident_f = sb("ident_f", [128, 128], F32)
    ident = sb("ident", [128, 128], BF16)
    ones_f = sb("ones_f", [128, 128], F32)
    ones_b = sb("ones_b", [128, 128], BF16)
    Bc = Buf("consts")
    tk.dma("sp", [], [Bc], lambda E: E.dma_start(out=ident_f[:], in_=ident_in.ap()))
    tk.op("dve", [Bc], [Bc], lambda E: E.tensor_copy(out=ident[:], in_=ident_f[:]))
    tk.op("pool", [], [(Bc, "of")], lambda E: E.memset(ones_f[:], 1.0))
    tk.op("pool", [], [(Bc, "ob")], lambda E: E.memset(ones_b[:], 1.0))

    PS = [nc.psum_tensor("ps%d" % i, [128, 512], F32).__enter__() for i in range(8)]
    PB = [Buf("ps%d" % i) for i in range(8)]

    sc = Scope(nc)
    ccs = sc.sb("ccs", [128, 8, 2], F32)
    csil = sc.sb("csil", [128, 8, 2], F32)
    adab = sc.sb("adab", [1, 2, 6 * D], F32)
    modsb = sc.sb("modsb", [2, 2, 6 * D], F32)
    aw0 = sc.sb("aw0", [128, 8, 512], F32)
    aw1 = sc.sb("aw1", [128, 8, 512], F32)
    if True:
        bcs, baw, bmod, bab = Buf("ccs"), [Buf("aw0"), Buf("aw1")], Buf("modsb"), Buf("adab")
        aws = [aw0, aw1]
        tk.dma("sp", [], [bcs], lambda E: E.dma_start(out=ccs[:], in_=cc_in.ap()))
        tk.dma("sp", [], [bab], lambda E: E.dma_start(out=adab[:], in_=adab_in.ap().unsqueeze(0)))
        tk.op("act", [bcs], [(bcs, "s")], lambda E: E.activation(out=csil[:], in_=ccs[:], func=AF.Silu))
        kk0 = [0]

        def mod_layer(l):
            for n in range(12):
                k = kk0[0]
                kk0[0] += 1
                aw, bw = aws[k % 2], baw[k % 2]
                ps, pb = PS[k % 2], PB[k % 2]
                tk.dma("sp", [], [bw], lambda E: E.dma_start(
                    out=aw[:], in_=adaw_in[l, :, n * 512:(n + 1) * 512].rearrange("(kc p) f -> p kc f", p=128)))
                for kc in range(8):
                    tk.op("pe", [bw, (bcs, "s")], [pb] if kc == 0 else [],
                          lambda E: E.matmul(ps[0:2, :], lhsT=csil[:, kc, :], rhs=aw[:, kc, :], start=(kc == 0), stop=False),
                          mark=False)
                tk.op("pe", [bab, (Bc, "of")], [pb],
                      lambda E: E.matmul(ps[0:2, :], lhsT=ones_f[0:1, 0:2], rhs=adab[0:1, l, n * 512:(n + 1) * 512],
                                         start=False, stop=True))
                tk.op("act", [], [(bmod, k), pb], lambda E: E.activation(out=modsb[0:2, l, n * 512:(n + 1) * 512],
                                                                         in_=ps[0:2, :], func=AF.Copy))
            tk.dma("sp", [bmod], [(B["mod_d"], l)], lambda E: E.dma_start(out=mod_d[l, :, :], in_=modsb[0:2, l, :]))

        mod_layer(0)
    sc0 = sc
    if debug == "p0":
        dbg_dump("mod_d", mod_d, [2, 2, 6 * D], F32)
        return finish(None), dbg_out

    def load_row(q, dst, dbuf, src_ap):
        tk.dma(q, [B["mod_d"]], [dbuf], lambda E: E.dma_start(out=dst, in_=src_ap.partition_broadcast(128)))

    def mod_row(l, v, ci):
        return mod_d[l, v, ci * D:(ci + 1) * D]

    def make_gm(dst, dbuf, tmp, tbuf, l, v, ci_scale, s):
        load_row("sp", dst, dbuf, mod_row(l, v, ci_scale))
        tk.dma("sp", [], [tbuf], lambda E: E.dma_start(out=tmp, in_=ng_in[l, s, :].partition_broadcast(128)))
        tk.op("dve", [dbuf, tbuf], [dbuf], lambda E: E.scalar_tensor_tensor(
            out=dst, in0=dst, scalar=1.0, in1=tmp, op0=ALU.add, op1=ALU.mult))

    def rstd_of(ss_ap, out_ap, tmp_ap, rb, n):
        tk.op("act", [rb], [rb], lambda E: E.activation(out=tmp_ap, in_=ss_ap, func=AF.Ln, scale=1.0 / n, bias=1e-6))
        tk.op("act", [rb], [rb], lambda E: E.activation(out=out_ap, in_=tmp_ap, func=AF.Exp, scale=-0.5))

    sc = Scope(nc)
    gm = sc.sb("gm", [128, D], F32)
    sh = sc.sb("sh", [128, D], F32)
    gmc = sc.sb("gmc", [128, D], F32)
    shc = sc.sb("shc", [128, D], F32)
    tmpr = sc.sb("tmpr", [128, D], F32)
    xt0 = sc.sb("xt0", [128, D], F32)
    xt1 = sc.sb("xt1", [128, D], F32)
    sq = sc.sb("sq", [128, D], F32)
    st = sc.sb("st", [128, 8], F32)
    hb0 = sc.sb("hb0", [128, D], BF16)
    hb1 = sc.sb("hb1", [128, D], BF16)
    if True:
        bgm, bsh, bgmc, bshc, btmp = Buf("gm"), Buf("sh"), Buf("gmc"), Buf("shc"), Buf("tmpr")
        make_gm(gm[:], bgm, tmpr[:], btmp, 0, 0, 1, 0)
        load_row("sp", sh[:], bsh, mod_row(0, 0, 0))
        make_gm(gmc[:], bgmc, tmpr[:], btmp, 0, 1, 1, 0)
        load_row("sp", shc[:], bshc, mod_row(0, 1, 0))
        xts, bxt = [xt0, xt1], [Buf("xt0"), Buf("xt1")]
        hbs, bhb = [hb0, hb1], [Buf("hb0"), Buf("hb1")]
        htb = [sc.sb("htb%d" % k_, [128, D], BF16) for k_ in range(2)]
        bhtb = [Buf("htb0"), Buf("htb1")]
        bsq, bst = Buf("sq"), Buf("st")
        for k in range(NT + 2):
            xt, bx, hb, bh = xts[k % 2], bxt[k % 2], hbs[k % 2], bhb[k % 2]
            isc = k >= NT
            src = ctx_in[(k - NT) * 128:(k - NT + 1) * 128, :] if isc else x_in[k * 128:(k + 1) * 128, :]
            g_, s_, bg_, bs_ = (gmc, shc, bgmc, bshc) if isc else (gm, sh, bgm, bsh)
            tk.dma("sp", [], [bx], lambda E: E.dma_start(out=xt[:], in_=src))
            tk.op("act", [bx], [bsq, bst], lambda E: E.activation(out=sq[:], in_=xt[:], func=AF.Square, accum_out=st[:, 0:1]))
            rstd_of(st[:, 0:1], st[:, 2:3], st[:, 1:2], bst, D)
            tk.op("dve", [bx, bst, bg_], [bx], lambda E: E.scalar_tensor_tensor(
                out=xt[:], in0=xt[:], scalar=st[:, 2:3], in1=g_[:], op0=ALU.mult, op1=ALU.mult))
            tk.op("dve", [bx, bs_], [bh], lambda E: E.tensor_tensor(out=hb[:], in0=xt[:], in1=s_[:], op=ALU.add))
            psT = PS[k % 2][:].bitcast(BF16)
            for kc in range(8):
                tk.op("pe", [bh, Bc], [PB[k % 2]] if kc in (0, 7) else [],
                      lambda E: E.transpose(out=psT[:, kc * 128:(kc + 1) * 128], in_=hb[:, kc * 128:(kc + 1) * 128], identity=ident[:]),
                      mark=(kc == 7))
            hb, bh = htb[k % 2], bhtb[k % 2]
            tk.op("act", [], [bh, PB[k % 2]], lambda E: E.activation(out=hb[:], in_=psT, func=AF.Copy))
            if isc:
                tk.dma("sp", [bh], [B["hc_d"]], lambda E: E.dma_start(out=hc_d[(k - NT) * 128:(k - NT + 1) * 128, :], in_=hb[:]))
            else:
                tk.dma("sp", [bh], [(B["h1_loc"], k)], lambda E: E.dma_start(out=loc_rows(h1_loc, k), in_=hb[:]))
                if k % 4 == 3:
                    ck = k // 4
                    tk.coll("AllGather", ALU.bypass, h1_loc[ck].ap(), h1_all[ck].ap(), [(B["h1_loc"], t_) for t_ in range(4 * ck, 4 * ck + 4)], [(B["h1_all"], ck)])
    if debug == "p1a":
        dbg_dump("h1_loc", h1_loc, [TL, D], BF16)
        dbg_dump("hc_d", hc_d, [CTX, D], BF16)
        return finish(None), dbg_out
    mod_layer(1)
    tk.barrier()
    sc.close()
    sc0.close()
    if debug == "p1":
        dbg_dump("h1_all", h1_all[3], [2048, D], BF16)
        dbg_dump("hc_d", hc_d, [CTX, D], BF16)
        dbg_dump("mod_d", mod_d, [2, 2, 6 * D], F32)
        return finish(None), dbg_out

    sc = Scope(nc)
    wfm = sc.sb("wfm", [128, 8, 288], BF16)
    wtm = sc.sb("wtm", [128, 8, 640], BF16)
    wstg = sc.sb("wstg", [128, 8, 288], F32)
    a2s = sc.sb("a2s", [32, 2, 128], F32)
    tri = sc.sb("tri", [128, 6, 128], F32)
    wo_f = sc.sb("wo_f", [128, 2, D], F32)
    wo_b = sc.sb("wo_b", [128, 2, D], BF16)
    gns = sc.sb("gns", [128, 2], F32)
    g1r = sc.sb("g1r", [128, D], F32)
    hbA = sc.sb("hbA", [128, D], BF16)
    hbB = sc.sb("hbB", [128, D], BF16)
    hT = sc.sb("hT", [128, 8, 128], BF16)
    aT = sc.sb("aT", [32, 128], F32)
    e1 = sc.sb("e1", [128, 128], F32)
    l1 = sc.sb("l1", [128, 128], F32)
    EqT = sc.sb("EqT", [128, 128], F32)
    EkT = sc.sb("EkT", [128, 128], F32)
    Ekd = sc.sb("Ekd", [128, 128], F32)
    qt = sc.sb("qt", [128, 128], BF16)
    kt = sc.sb("kt", [128, 128], BF16)
    kd = sc.sb("kd", [128, 128], BF16)
    vs = sc.sb("vs", [128, 256], BF16)
    sT = sc.sb("sT", [128, 128], BF16)
    S = sc.sb("S", [128, 256], F32)
    Sb = sc.sb("Sb", [128, 256], BF16)
    osb = sc.sb("osb", [128, 256], F32)
    ofl = sc.sb("ofl", [128, 256], F32)
    osq = sc.sb("osq", [128, 256], F32)
    sr = sc.sb("sr", [128, 256], F32)
    ofin = sc.sb("ofin", [128, 256], BF16)
    ofT = sc.sb("ofT", [128, 2, 128], BF16)
    ypo = sc.sb("ypo", [128, D], BF16)
    st2 = sc.sb("st2", [128, 8], F32)
    if True:
        bw = Buf("gla_w")
        tk.dma("sp", [], [(bw, "stg")], lambda E: E.dma_start(out=wstg[:], in_=wfm_in.ap().rearrange("(kc p) f -> p kc f", p=128)))
        tk.op("act", [(bw, "stg")], [(bw, "fm")], lambda E: E.activation(out=wfm[:, :, 0:128], in_=wstg[:, :, 0:128], func=AF.Copy, scale=128.0 ** -0.5))
        tk.op("act", [(bw, "stg")], [(bw, "fm2")], lambda E: E.activation(out=wfm[:, :, 128:288], in_=wstg[:, :, 128:288], func=AF.Copy))
        tk.dma("pool", [], [(bw, "tm")], lambda E: E.dma_start(out=wtm[:], in_=wtm_in.ap().rearrange("(kc p) f -> p kc f", p=128)))
        tk.dma("sp", [], [(bw, "a2")], lambda E: E.dma_start(out=a2s[:], in_=a2_in.ap().rearrange("d r f -> r d f")))
        tk.dma("sp", [], [(bw, "tri")], lambda E: E.dma_start(out=tri[:], in_=tri_in.ap().rearrange("s p f -> p s f")))
        tk.dma("sp", [], [(bw, "gn")], lambda E: E.dma_start(out=gns[:], in_=gn_in.ap()))
        tk.dma("sp", [], [(bw, "wof")], lambda E: E.dma_start(out=wo_f[:], in_=wo_in.ap().rearrange("(vc p) f -> p vc f", p=128)))
        bg1 = Buf("g1r")
        load_row("sp", g1r[:], bg1, mod_row(0, 0, 2))
        for vc in range(2):
            tk.op("dve", [(bw, "wof"), (bw, "gn"), bg1], [(bw, "wob%d" % vc)], lambda E: E.scalar_tensor_tensor(
                out=wo_b[:, vc, :], in0=wo_f[:, vc, :], scalar=gns[:, vc:vc + 1], in1=g1r[:], op0=ALU.mult, op1=ALU.mult))
        def mk(name, shape, dt, n):
            return [sc.sb("%s_%d" % (name, k), shape, dt) for k in range(n)], [Buf("%s_%d" % (name, k)) for k in range(n)]
        hbs, bhb = mk("hbp", [128, D], BF16, 2)
        hTs, bhT = mk("hTp", [128, 8, 128], BF16, 6)
        qss, bqs = mk("qs", [128, 128], F32, 2)
        kss, bks = mk("ks", [128, 128], F32, 2)
        kms, bkm = mk("km", [128, 128], F32, 2)
        vss, bvs = mk("vsp", [128, 256], BF16, 3)
        srs, bsr = mk("srp", [128, 256], F32, 3)
        aTs, baT = mk("aTp", [32, 128], F32, 2)
        l1s, bl1 = mk("l1p", [128, 128], F32, 2)
        Eqs, bEq = mk("Eq", [128, 128], F32, 2)
        Eks, bEk = mk("Ek", [128, 128], F32, 2)
        Eds, bEd = mk("Ed", [128, 128], F32, 2)
        qts, bqt = mk("qtp", [128, 128], BF16, 2)
        kts, bkt = mk("ktp", [128, 128], BF16, 2)
        kds, bkd = mk("kdp", [128, 128], BF16, 2)
        sTs, bsT = mk("sTp", [128, 128], BF16, 2)
        osbs, bosb = mk("osbp", [128, 256], F32, 2)
        ofls, bofl = mk("oflp", [128, 256], F32, 6)
        ofins, bofin = mk("ofinp", [128, 256], BF16, 2)
        ofTs, bofT = mk("ofTp", [128, 2, 128], BF16, 2)
        ypos, bypo = mk("ypop", [128, D], BF16, 2)
        st2s, bst2 = mk("st2p", [128, 8], F32, 2)
        be1, bS, bSb, bosq = Buf("e1"), Buf("S"), Buf("Sb"), Buf("osq")
        recs, brec = mk("rec", [128, 896], F32, 6)
        ers = sc.sb("ers", [128, 256], F32)
        bers = Buf("ers")
        aTb, baTb = mk("aTb", [32, 128], F32, 2)
        e1b = sc.sb("e1b", [128, 128], F32)
        be1b = Buf("e1b")
        rec_d = dscr("rec_d", [NTA + 2, 128, 896], F32)
        Brec = Buf("rec_d")
        for k in range(2):
            tk.op("pool", [], [baTb[k]], lambda E: E.memset(aTb[k][:], 1.0))
        for k in range(2):
            tk.op("pool", [], [baT[k]], lambda E: E.memset(aTs[k][:], 1.0))
        WR = [(bw, "fm"), (bw, "fm2")]
        P = lambda b, nm: (PB[b], nm)
        precast = []
        if debug is None:
            for l in range(2):
                for e in range(4):
                    precast.append((wgb_d[l][e], wg_in[l, e, :, :]))
                    precast.append((wub_d[l][e], wu_in[l, e, :, :]))
                    precast.append((wdb_d[l][e], wd_in[l, e, :, :]))
        for dr in range(int(os.environ.get('KDR', '2'))):
            if dr == 0:
                seq = [("c", 0), ("c", 1)] + [("x", g) for g in range(NTA)]
            else:
                seq = [("c", 1), ("c", 0)] + [("x", g) for g in range(NTA - 1, -1, -1)]
            lastcol = 127 if dr == 0 else 0
            N = min(len(seq), int(os.environ.get('KNT', '1000')))

            def stage_l(n):
                kind, gi = seq[n]
                isc = kind == "c"
                p5 = n % 6
                ridx = (NTA + gi) if isc else gi
                if dr == 1:
                    tk.dma("sp", [(Brec, ridx)], [brec[p5]], lambda E: E.dma_start(out=recs[p5][:], in_=rec_d[ridx, :, :]))
                    if not isc:
                        tk.dma("sp", [(B["of_d"], gi)], [bofl[p5]], lambda E: E.dma_start(out=ofls[p5][:], in_=of_d[gi * 128:(gi + 1) * 128, :]))
                    return
                src = hc_d[gi * 128:(gi + 1) * 128, :] if isc else all_rows(h1_all, gi)
                sbuf_ = B["hc_d"] if isc else (B["h1_all"], gi // 16)
                tk.dma("sp", [sbuf_], [bhT[p5]], lambda E: E.dma_start(out=hTs[p5][:].rearrange("p k t -> p (k t)"), in_=src))

            def stage_a(n):
                kind, gi = seq[n]
                isc = kind == "c"
                p2, p3 = n % 2, n % 6
                rec, br = recs[p3], brec[p3]
                ridx = (NTA + gi) if isc else gi
                if dr == 1:
                    return
                hT_, bhT_ = hTs[p3], bhT[p3]
                for (c0, c1, m, o0) in ((0, 128, 128, 0), (128, 256, 128, 128), (256, 272, 16, 256), (272, 288, 16, 384)):
                    for kc in range(8):
                        tk.op("pe", [bhT_] + WR, [PB[1]] if kc in (0, 7) else [],
                              lambda E: E.matmul(PS[1][0:m, o0:o0 + 128], lhsT=wfm[:, kc, c0:c1], rhs=hT_[:, kc, :], start=(kc == 0), stop=(kc == 7)),
                              mark=(kc == 7))
                for kc in range(8):
                    tk.op("pe", [bhT_, (bw, "tm")], [PB[2]] if kc in (0, 7) else [],
                          lambda E: E.matmul(PS[2][:, 0:384], lhsT=hT_[:, kc, :], rhs=wtm[:, kc, 0:384], start=(kc == 0), stop=(kc == 7)),
                          mark=(kc == 7))
                if not isc:
                    for kc in range(8):
                        tk.op("pe", [bhT_, (bw, "tm")], [PB[3]] if kc in (0, 7) else [],
                              lambda E: E.matmul(PS[3][:, 0:256], lhsT=hT_[:, kc, :], rhs=wtm[:, kc, 384:640], start=(kc == 0), stop=(kc == 7)),
                              mark=(kc == 7))
                tk.op("act", [], [baT[p2], PB[1]], lambda E: E.activation(out=aTs[p2][0:16, :], in_=PS[1][0:16, 256:384], func=AF.Copy))
                tk.op("act", [], [baTb[p2], PB[1]], lambda E: E.activation(out=aTb[p2][0:16, :], in_=PS[1][0:16, 384:512], func=AF.Copy))
                tk.op("act", [], [(br, "q"), PB[1]], lambda E: E.activation(out=rec[:, 0:128], in_=PS[1][:, 0:128], func=AF.Copy))
                tk.op("dve", [], [(br, "k"), PB[1]], lambda E: E.tensor_copy(out=rec[:, 128:256], in_=PS[1][:, 128:256]))
                tk.op("dve", [], [(br, "km"), PB[2]], lambda E: E.tensor_copy(out=rec[:, 256:384], in_=PS[2][:, 0:128]))
                tk.op("act", [], [(br, "v"), PB[2]], lambda E: E.activation(out=rec[:, 512:640].bitcast(BF16), in_=PS[2][:, 128:384], func=AF.Copy))
                if not isc:
                    tk.op("act", [], [bers, PB[3]], lambda E: E.activation(out=ers[:], in_=PS[3][:, 0:256], func=AF.Exp, scale=-1.0))
                    tk.op("dve", [bers], [bers], lambda E: E.tensor_scalar(out=ers[:], in0=ers[:], scalar1=1.0, scalar2=1.0, op0=ALU.mult, op1=ALU.add))
                    tk.op("dve", [bers], [bers], lambda E: E.reciprocal(out=ers[:], in_=ers[:]))
                    tk.op("dve", [bers], [(br, "sr"), PB[3]], lambda E: E.tensor_tensor(out=rec[:, 640:896], in0=ers[:], in1=PS[3][:, 0:256], op=ALU.mult))
                tk.op("pe", [baT[p2], (bw, "a2")], [PB[0]],
                      lambda E: E.matmul(PS[0][:, 0:128], lhsT=aTs[p2][0:32, :], rhs=a2s[0:32, 0, :], start=True, stop=True), mark=False)
                tk.op("pe", [baTb[p2], (bw, "a2")], [PB[0]],
                      lambda E: E.matmul(PS[0][:, 128:256], lhsT=aTb[p2][0:32, :], rhs=a2s[0:32, 1, :], start=True, stop=True))
                tk.op("act", [], [be1, PB[0]], lambda E: E.activation(out=e1[:], in_=PS[0][:, 0:128], func=AF.Exp, scale=-1.0))
                tk.op("act", [], [be1b, PB[0]], lambda E: E.activation(out=e1b[:], in_=PS[0][:, 128:256], func=AF.Exp, scale=-1.0))
                tk.op("act", [be1], [bl1[p2]], lambda E: E.activation(out=l1s[p2][:], in_=e1[:], func=AF.Ln, bias=1.0))
                tk.op("act", [be1b], [(br, "l1b")], lambda E: E.activation(out=rec[:, 384:512], in_=e1b[:], func=AF.Ln, bias=1.0))
                tk.dma("sp", [br], [(Brec, ridx)], lambda E: E.dma_start(out=rec_d[ridx, :, :], in_=rec[:]))

            def stage_b(n):
                p2, p3 = n % 2, n % 6
                rec, br = recs[p3], brec[p3]
                l1ap = l1s[p2][:] if dr == 0 else rec[:, 384:512]
                l1b_ = bl1[p2] if dr == 0 else br
                tk.op("pe", [l1b_, (bw, "tri")], [PB[4]],
                      lambda E: E.matmul(PS[4][:, 0:128], lhsT=l1ap, rhs=tri[:, dr, :], start=True, stop=True), mark=False)
                tk.op("pe", [l1b_, (bw, "tri")], [PB[4]],
                      lambda E: E.matmul(PS[4][:, 128:256], lhsT=tri[:, 2 + dr, :], rhs=l1ap, start=True, stop=True))
                tk.op("act", [], [bEq[p2], PB[4]], lambda E: E.activation(out=Eqs[p2][:], in_=PS[4][:, 0:128], func=AF.Exp))
                tk.op("act", [], [bEk[p2], PB[4]], lambda E: E.activation(out=Eks[p2][:], in_=PS[4][:, 0:128], func=AF.Exp, scale=-1.0))
                tk.op("act", [], [bEd[p2], PB[4]], lambda E: E.activation(out=Eds[p2][:], in_=PS[4][:, 128:256], func=AF.Exp))
                tk.op("dve", [br, bEq[p2]], [bqt[p2]], lambda E: E.tensor_tensor(out=qts[p2][:], in0=rec[:, 0:128], in1=Eqs[p2][:], op=ALU.mult))
                tk.op("dve", [br, bEk[p2]], [bkt[p2]], lambda E: E.tensor_tensor(out=kts[p2][:], in0=rec[:, 128:256], in1=Eks[p2][:], op=ALU.mult))
                tk.op("dve", [br, bEd[p2]], [bkd[p2]], lambda E: E.tensor_tensor(out=kds[p2][:], in0=rec[:, 256:384], in1=Eds[p2][:], op=ALU.mult))

            def stage_c(n):
                kind, gi = seq[n]
                isc = kind == "c"
                p2, p3 = n % 2, n % 6
                qt, kt, kd = qts[p2], kts[p2], kds[p2]
                rec, br = recs[p3], brec[p3]
                vs_ap = rec[:, 512:640].bitcast(BF16)
                if n == 0:
                    tk.op("pool", [], [bS], lambda E: E.memset(S[:], 0.0))
                    tk.op("pool", [], [bSb], lambda E: E.memset(Sb[:], 0.0))
                if not isc:
                    tk.op("pe", [bkt[p2], bqt[p2]], [PB[5]],
                          lambda E: E.matmul(PS[5][:, 0:128], lhsT=kt[:], rhs=qt[:], start=True, stop=True))
                    tk.op("dve", [(bw, "tri")], [bsT[p2], PB[5]], lambda E: E.tensor_tensor(out=sTs[p2][:], in0=PS[5][:, 0:128], in1=tri[:, 4 + dr, :], op=ALU.mult))
                    tk.op("pe", [bsT[p2], br], [PB[5]],
                          lambda E: E.matmul(PS[5][:, 128:384], lhsT=sTs[p2][:], rhs=vs_ap, start=True, stop=False), mark=False)
                    tk.op("pe", [bqt[p2], bSb], [PB[5]],
                          lambda E: E.matmul(PS[5][:, 128:384], lhsT=qt[:], rhs=Sb[:], start=False, stop=True))
                tk.op("pe", [bkd[p2], br], [PB[6]],
                      lambda E: E.matmul(PS[6][:, 0:256], lhsT=kd[:], rhs=vs_ap, start=True, stop=True))
                tk.op("dve", [bEq[p2]], [bS, PB[6]], lambda E: E.scalar_tensor_tensor(
                    out=S[:], in0=S[:], scalar=Eqs[p2][:, lastcol:lastcol + 1], in1=PS[6][:, 0:256], op0=ALU.mult, op1=ALU.add))
                tk.op("act", [bS], [bSb], lambda E: E.activation(out=Sb[:], in_=S[:], func=AF.Copy))
                if isc:
                    return
                osb_, bosb_ = osbs[p2], bosb[p2]
                if dr == 0:
                    tk.op("act", [], [bosb_, PB[5]], lambda E: E.activation(out=osb_[:], in_=PS[5][:, 128:384], func=AF.Copy))
                    tk.dma("sp", [bosb_], [(B["of_d"], gi)], lambda E: E.dma_start(out=of_d[gi * 128:(gi + 1) * 128, :], in_=osb_[:]))
                    return
                ofl_, bofl_ = ofls[p3], bofl[p3]
                tk.op("dve", [bofl_], [bosb_, PB[5]], lambda E: E.tensor_tensor(out=osb_[:], in0=ofl_[:], in1=PS[5][:, 128:384], op=ALU.add))
                return

            def stage_d(n):
                kind, gi = seq[n]
                if kind == "c":
                    return
                p2, p3 = n % 2, n % 6
                rec, br = recs[p3], brec[p3]
                osb_, bosb_ = osbs[p2], bosb[p2]
                st2_, bst2_ = st2s[p2], bst2[p2]
                ofin_, bofin_, ofT_, bofT_, ypo_, bypo_ = ofins[p2], bofin[p2], ofTs[p2], bofT[p2], ypos[p2], bypo[p2]
                tk.op("act", [bosb_], [bosq, bst2_], lambda E: E.activation(out=osq[:], in_=osb_[:], func=AF.Square, accum_out=st2_[:, 0:1]))
                rstd_of(st2_[:, 0:1], st2_[:, 2:3], st2_[:, 1:2], bst2_, 256)
                tk.op("dve", [bosb_, bst2_, br], [bofin_], lambda E: E.scalar_tensor_tensor(
                    out=ofin_[:], in0=osb_[:], scalar=st2_[:, 2:3], in1=rec[:, 640:896], op0=ALU.mult, op1=ALU.mult))
                psT2 = PS[0][:].bitcast(BF16)
                for vc in range(2):
                    tk.op("pe", [bofin_, Bc], [PB[0]],
                          lambda E: E.transpose(out=psT2[:, 768 + vc * 128:768 + (vc + 1) * 128], in_=ofin_[:, vc * 128:(vc + 1) * 128], identity=ident[:]),
                          mark=(vc == 1))
                tk.op("act", [], [bofT_, PB[0]], lambda E: E.activation(out=ofT_[:].rearrange("p k t -> p (k t)"), in_=psT2[:, 768:1024], func=AF.Copy))
                for hf in range(2):
                    for vc in range(2):
                        tk.op("pe", [bofT_, (bw, "wob%d" % vc)], [PB[7]],
                              lambda E: E.matmul(PS[7][:, :], lhsT=ofT_[:, vc, :], rhs=wo_b[:, vc, hf * 512:(hf + 1) * 512], start=(vc == 0), stop=(vc == 1)),
                              mark=(vc == 1))
                    if hf == 0:
                        tk.op("act", [], [(bypo_, hf), PB[7]], lambda E: E.activation(out=ypo_[:, 0:512], in_=PS[7][:, :], func=AF.Copy))
                    else:
                        tk.op("dve", [], [(bypo_, hf), PB[7]], lambda E: E.tensor_copy(out=ypo_[:, 512:1024], in_=PS[7][:, :]))
                tk.dma("pool", [bypo_], [(B["yp_d"], gi)], lambda E: E.dma_start(out=all_rows(yp_d, gi), in_=ypo_[:]))
                if gi % 16 == 0:
                    ck = gi // 16
                    tk.coll("ReduceScatter", ALU.add, yp_d[ck].ap(), ymix_d[ck].ap(), [(B["yp_d"], g_) for g_ in range(16 * ck, 16 * ck + 16)], [(B["ymix_d"], ck)], qos="P3")

            for n in range(-4, N):
                if n + 4 < N:
                    stage_l(n + 4)
                if 0 <= n + 2 < N:
                    stage_a(n + 2)
                if 0 <= n + 1 < N:
                    stage_b(n + 1)
                if n >= 0:
                    stage_c(n)
                if dr == 1 and n >= 1:
                    stage_d(n - 1)
                if n >= 0 and n % 10 == 5 and precast:
                    dst, srcw = precast.pop(0)
                    tk._wait("pool", tk._deps([(bSb, None)], []))
                    tk.dma("pool", [], [], lambda E: E.dma_start(out=dst.ap(), in_=srcw), sem="precast")
            if dr == 1:
                stage_d(N - 1)
        while precast:
            dst, srcw = precast.pop(0)
            tk.dma("pool", [], [], lambda E: E.dma_start(out=dst.ap(), in_=srcw), sem="precast")
        if debug is None:
            BWB.w = {None: (("x", "precast"), tk.ded["precast"][1])}
            BWB.r = {}
    tk.barrier()
    sc.close()
    if debug == "p2":
        for k in range(NCH):
            o = nc.dram_tensor("dbg_ymix%d" % k, [512, D], BF16, kind="ExternalOutput")
            dbg_out["ymix%d" % k] = o
            tk.dma("sp", [B["ymix_d"]], [Buf("x")], lambda E: E.dma_start(out=o.ap(), in_=ymix_d[k].ap()))
        return finish(None), dbg_out

    cwin_in = din("cw_in", [D, 3 * D])
    ck_in = din("ck", [128, 8, 3])
    cwo_in = din("cw_out", [D, D])
    rw_in = din("router_w", [2, D, 16])
    sel_in = din("sel", [4, 16])
    tid_in = din("tid", [128, NT], I32)
    lu_in = din("lu", [2, 128, 128])

    NCR = 16
    rows_loc = [dscr("rows_loc%d" % k, [256, RW], BF16) for k in range(NCR)]
    rows_all = [dscr("rows_all%d" % k, [1024, RW], BF16) for k in range(NCR)]
    zs_c = [dscr("zs_c%d" % k, [512, D], BF16) for k in range(NCH)]

    def rows_all_tile(gi):
        r, i = gi // NT, gi % NT
        k, w = i // 2, i % 2
        return rows_all[k][r * 256 + w * 128:r * 256 + (w + 1) * 128, :]

    tid_sb = sb("tid_sb", [128, NT], I32)
    btid = Buf("tid")
    tk.dma("sp", [], [btid], lambda E: E.dma_start(out=tid_sb[:], in_=tid_in.ap()))
    lub = sb("lub", [128, 128], BF16)
    luf = sb("luf", [128, 128], F32)
    tk.dma("sp", [], [(Bc, "luf")], lambda E: E.dma_start(out=luf[:], in_=lu_in[0, :, :]))
    tk.op("dve", [(Bc, "luf")], [(Bc, "lub")], lambda E: E.tensor_copy(out=lub[:], in_=luf[:]))

    class TailCtx:
        pass

    chunk_order = [list(range(NCR))]

    def tail_setup(sc, l):
        c = TailCtx()
        c.l = l
        c.order = []
        c.deferred = []
        c.gm2 = sc.sb("gm2_%d" % l, [128, D], F32); c.sh2 = sc.sb("sh2_%d" % l, [128, D], F32)
        c.tmpr = sc.sb("tmpr2_%d" % l, [128, D], F32)
        c.bgm2, c.bsh2, c.btmp = Buf("gm2"), Buf("sh2"), Buf("tmpr2")
        make_gm(c.gm2[:], c.bgm2, c.tmpr[:], c.btmp, l, 0, 4, 1)
        load_row("sp", c.sh2[:], c.bsh2, mod_row(l, 0, 3))
        c.rwf = sc.sb("rwf%d" % l, [128, 8, 16], F32); c.rwb = sc.sb("rwb%d" % l, [128, 8, 16], BF16)
        c.brw = Buf("rw")
        tk.dma("sp", [], [(c.brw, "f")], lambda E: E.dma_start(out=c.rwf[:], in_=rw_in[l, :, :].rearrange("(kc p) e -> p kc e", p=128)))
        tk.op("dve", [(c.brw, "f")], [(c.brw, "b")], lambda E: E.tensor_copy(out=c.rwb[:], in_=c.rwf[:]))
        c.rowt = [sc.sb("rowt%d_%d" % (l, k), [128, RW], BF16) for k in range(2)]
        c.brow = [Buf("rowt0"), Buf("rowt1")]
        for k in range(2):
            tk.op("pool", [], [c.brow[k]], lambda E: E.memset(c.rowt[k][:], 0.0))
        c.sq = sc.sb("sq2_%d" % l, [128, D], F32); c.st = sc.sb("st2t_%d" % l, [128, 8], F32)
        c.bsq, c.bst = Buf("sq2"), Buf("st2t")
        c.hT2 = sc.sb("hT2_%d" % l, [128, 8, 128], BF16); c.bhT2 = Buf("hT2")
        c.ex = sc.sb("ex_%d" % l, [128, 16], F32); c.bex = Buf("ex")
        c.affs = sc.sb("affs_%d" % l, [128, NT, 16], F32); c.baff = Buf("affs")
        c.xw = sc.sb("xw_%d" % l, [128, D], F32); c.bxw = Buf("xw")
        return c

    def tail(c, xt, bx, i, desc=False, split=False):
        rowt, brow = c.rowt[i % 2], c.brow[i % 2]
        tk.op("act", [bx], [c.bsq, c.bst], lambda E: E.activation(out=c.sq[:], in_=xt, func=AF.Square, accum_out=c.st[:, 0:1]))
        rstd_of(c.st[:, 0:1], c.st[:, 2:3], c.st[:, 1:2], c.bst, D)
        tk.op("dve", [bx, c.bst, c.bgm2], [c.bxw], lambda E: E.scalar_tensor_tensor(
            out=c.xw[:], in0=xt, scalar=c.st[:, 2:3], in1=c.gm2[:], op0=ALU.mult, op1=ALU.mult))
        tk.op("dve", [c.bxw, c.bsh2], [(brow, "h")], lambda E: E.tensor_tensor(out=rowt[:, 0:D], in0=c.xw[:], in1=c.sh2[:], op=ALU.add))
        if not split:
            tail2(c, i, desc)

    def tail2(c, i, desc=False):
        rowt, brow = c.rowt[i % 2], c.brow[i % 2]
        psT = PS[0][:].bitcast(BF16)
        for kc in range(8):
            tk.op("pe", [(brow, "h"), Bc], [PB[0]] if kc in (0, 7) else [],
                  lambda E: E.transpose(out=psT[:, kc * 128:(kc + 1) * 128], in_=rowt[:, kc * 128:(kc + 1) * 128], identity=ident[:]),
                  mark=(kc == 7))
        tk.op("act", [PB[0]], [c.bhT2], lambda E: E.activation(out=c.hT2[:].rearrange("p k t -> p (k t)"), in_=psT, func=AF.Copy))
        for kc in range(8):
            tk.op("pe", [c.bhT2, (c.brw, "b")], [PB[7]] if kc in (0, 7) else [],
                  lambda E: E.matmul(PS[7][:, 0:16], lhsT=c.hT2[:, kc, :], rhs=c.rwb[:, kc, :], start=(kc == 0), stop=(kc == 7)),
                  mark=(kc == 7))
        tk.op("dve", [PB[7]], [c.bst], lambda E: E.tensor_reduce(out=c.st[:, 3:4], in_=PS[7][:, 0:16], axis=AX.X, op=ALU.max))
        tk.op("dve", [c.bst], [c.bst], lambda E: E.tensor_scalar(out=c.st[:, 4:5], in0=c.st[:, 3:4], scalar1=-1.0, scalar2=0.0, op0=ALU.mult, op1=ALU.add))
        tk.op("act", [PB[7], c.bst], [c.bex, c.bst], lambda E: E.activation(out=c.ex[:], in_=PS[7][:, 0:16], func=AF.Exp, bias=c.st[:, 4:5], accum_out=c.st[:, 5:6]))
        tk.op("dve", [c.bst], [c.bst], lambda E: E.reciprocal(out=c.st[:, 6:7], in_=c.st[:, 5:6]))
        tk.op("dve", [c.bex, c.bst], [(c.baff, i)], lambda E: E.tensor_scalar(out=c.affs[:, i, :], in0=c.ex[:], scalar1=c.st[:, 6:7], scalar2=0.0, op0=ALU.mult, op1=ALU.add))
        tk.op("dve", [(c.baff, i)], [(brow, "a")], lambda E: E.tensor_copy(out=rowt[:, D:D + 32].bitcast(F32), in_=c.affs[:, i, :]))
        tk.op("dve", [btid], [(brow, "t")], lambda E: E.tensor_copy(out=rowt[:, D + 32:D + 34].bitcast(I32), in_=tid_sb[:, i:i + 1]))
        k, w = i // 2, i % 2
        tk.dma("sp", [brow], [(B["rows_loc"], i)], lambda E: E.dma_start(out=rows_loc[k][w * 128:(w + 1) * 128, :], in_=rowt[:]))
        if w == (0 if desc else 1):
            if len(c.order) < 6:
                tk.coll("AllGather", ALU.bypass, rows_loc[k].ap(), rows_all[k].ap(), [(B["rows_loc"], 2 * k), (B["rows_loc"], 2 * k + 1)], [(B["rows_all"], k)])
            else:
                c.deferred.append(k)
            c.order.append(k)

    def tail_finish(c):
        tk.dma("sp", [c.baff], [B["aff_loc"]], lambda E: E.dma_start(out=aff_loc.ap(), in_=c.affs[:]))
        tk.coll("AllGather", ALU.bypass, aff_loc.ap(), aff_all.ap(), [B["aff_loc"]], [B["aff_all"]])
        for k in c.deferred:
            tk.coll("AllGather", ALU.bypass, rows_loc[k].ap(), rows_all[k].ap(), [(B["rows_loc"], 2 * k), (B["rows_loc"], 2 * k + 1)], [(B["rows_all"], k)])
        chunk_order[0] = list(c.order)

    sc = Scope(nc)
    tc0 = tail_setup(sc, 0)
    xa = [sc.sb("xa%d" % k, [128, D], F32) for k in range(2)]
    bxa = [Buf("xa0"), Buf("xa1")]
    ym = [sc.sb("ym%d" % k, [128, D], BF16) for k in range(2)]
    bym = [Buf("ym0"), Buf("ym1")]
    for i in reversed(range(NT)):
        xt, bx, yt, by = xa[i % 2], bxa[i % 2], ym[i % 2], bym[i % 2]
        tk.dma("sp", [], [bx], lambda E: E.dma_start(out=xt[:], in_=x_in[i * 128:(i + 1) * 128, :]))
        tk.dma("sp", [(B["ymix_d"], i // 4)], [by], lambda E: E.dma_start(out=yt[:], in_=loc_rows(ymix_d, i)))
        tk.op("dve", [bx, by], [bx], lambda E: E.tensor_tensor(out=xt[:], in0=xt[:], in1=yt[:], op=ALU.add))
        tk.dma("sp", [bx], [(B["x1_d"], i)], lambda E: E.dma_start(out=x1_d[i * 128:(i + 1) * 128, :], in_=xt[:]))
        tail(tc0, xt[:], bx, i, desc=True)
    tail_finish(tc0)
    tk.barrier()
    sc.close()

    r_cap = nc.gpsimd.alloc_register("r_cap")
    nc.gpsimd.reg_mov(r_cap, CAP - 1)
    r_tok = nc.gpsimd.alloc_register("r_tok")
    nc.gpsimd.reg_mov(r_tok, T - 1)

    def moe(l):
        sc = Scope(nc)
        Aall = sc.sb("Aall", [128, 128, 16], F32); tmpA = sc.sb("tmpA", [128, 128, 16], F32)
        selt = sc.sb("selt", [128, 4, 16], F32); Asel = sc.sb("Asel", [128, 4, 128], F32)
        cmp = sc.sb("cmp", [128, 4, 128], F32)
        bs = sc.sb("bs", [128, 8, 4], F32)
        Mexp = sc.sb("Mexp", [128, 4, 128], BF16); Xs = sc.sb("Xs", [128, 4, 128], BF16)
        offf = sc.sb("offf", [128, 4, 128], F32); offi = sc.sb("offi", [128, 4, 128], I32)
        zt = sc.sb("zt", [128, D], BF16)
        bA, bsel, bAs, bcmp, bbs, bM, bXs, boff, bzt = (Buf(n) for n in ("Aall", "selt", "Asel", "cmp", "bs", "Mexp", "Xs", "off", "zt"))
        tk.op("pool", [], [bzt], lambda E: E.memset(zt[:], 0.0))
        for g in range(NTA):
            tk.dma("sp", [bzt], [(B["z_d"], "z%d" % g)], lambda E: E.dma_start(out=z_d[g * 128:(g + 1) * 128, :], in_=zt[:]))
        tk.dma("sp", [B["aff_all"]], [bA], lambda E: E.dma_start(
            out=Aall[:].rearrange("q (k i) e -> q k i e", k=4), in_=aff_all.ap().rearrange("(k q) i e -> q k i e", q=128)))

        tk.dma("sp", [], [bsel], lambda E: E.dma_start(out=selt[:], in_=sel_in.ap().partition_broadcast(128)))
        for e in range(4):
            tk.op("dve", [bA, bsel], [Buf("t")] and [bcmp], lambda E: E.tensor_tensor(
                out=tmpA[:], in0=Aall[:], in1=selt[:, e, :].unsqueeze(1).broadcast_to([128, 128, 16]), op=ALU.mult))
            tk.op("dve", [bcmp], [(bAs, e)], lambda E: E.tensor_reduce(out=Asel[:, e, :], in_=tmpA[:], axis=AX.X, op=ALU.add))
        lo, hi, mid, cntp, ge, dd = (bs[:, k, :] for k in range(6))
        tk.op("pool", [], [bbs], lambda E: E.memset(bs[:], 0.0))
        tk.op("pool", [bbs], [bbs], lambda E: E.memset(bs[:, 1, :], 1.0))
        for it in range(NITER):
            tk.op("dve", [bbs], [bbs], lambda E: E.tensor_tensor(out=mid, in0=lo, in1=hi, op=ALU.add))
            tk.op("dve", [bbs], [bbs], lambda E: E.tensor_scalar(out=mid, in0=mid, scalar1=0.5, scalar2=0.0, op0=ALU.mult, op1=ALU.add))
            tk.op("dve", [bAs, bbs], [bcmp], lambda E: E.tensor_tensor(
                out=cmp[:], in0=Asel[:], in1=bs[:, 2, :].unsqueeze(2).broadcast_to([128, 4, 128]), op=ALU.is_gt))
            tk.op("dve", [bcmp], [bbs], lambda E: E.tensor_reduce(out=cntp, in_=cmp[:], axis=AX.X, op=ALU.add))
            tk.op("pe", [bbs, (Bc, "of")], [PB[7]], lambda E: E.matmul(PS[7][:, 0:4], lhsT=ones_f[:], rhs=cntp, start=True, stop=True))
            tk.op("dve", [PB[7]], [bbs], lambda E: E.tensor_scalar(out=ge, in0=PS[7][:, 0:4], scalar1=float(CAP), scalar2=0.0, op0=ALU.is_ge, op1=ALU.add))
            tk.op("dve", [bbs], [bbs], lambda E: E.tensor_tensor(out=dd, in0=mid, in1=lo, op=ALU.subtract))
            tk.op("dve", [bbs], [bbs], lambda E: E.tensor_tensor(out=dd, in0=dd, in1=ge, op=ALU.mult))
            tk.op("dve", [bbs], [bbs], lambda E: E.tensor_tensor(out=lo, in0=lo, in1=dd, op=ALU.add))
            tk.op("dve", [bbs], [bbs], lambda E: E.tensor_tensor(out=dd, in0=hi, in1=mid, op=ALU.subtract))
            tk.op("dve", [bbs], [bbs], lambda E: E.tensor_tensor(out=dd, in0=dd, in1=ge, op=ALU.mult))
            tk.op("dve", [bbs], [bbs], lambda E: E.tensor_tensor(out=hi, in0=mid, in1=dd, op=ALU.add))
        for e in range(4):
            tk.op("dve", [bAs, bbs], [(bM, e)], lambda E: E.tensor_scalar(
                out=Mexp[:, e, :], in0=Asel[:, e, :], scalar1=bs[:, 0, e:e + 1], scalar2=0.0, op0=ALU.is_gt, op1=ALU.add))
        for e in range(4):
            tk.op("pe", [bM, (Bc, "ob")], [PB[1]], lambda E: E.matmul(PS[1][:, e * 128:(e + 1) * 128], lhsT=Mexp[:, e, :], rhs=ones_b[:], start=True, stop=True),
                  mark=(e == 3))
        tk.op("act", [PB[1]], [bXs], lambda E: E.activation(out=Xs[:].rearrange("p e t -> p (e t)"), in_=PS[1][:, :], func=AF.Copy))
        tk.op("pe", [bM, (Bc, "lub")], [PB[2]], lambda E: E.matmul(PS[2][:, :], lhsT=lub[:], rhs=Mexp[:].rearrange("p e t -> p (e t)"), start=True, stop=False), mark=False)
        for e in range(4):
            tk.op("pe", [bXs, (Bc, "lub")], [PB[2]] if e == 3 else [], lambda E: E.matmul(PS[2][:, e * 128:(e + 1) * 128], lhsT=Xs[:, e, :], rhs=lub[:], start=False, stop=(e == 3)),
                  mark=(e == 3))
        tk.op("dve", [bM], [boff], lambda E: E.tensor_scalar(out=offf[:], in0=Mexp[:], scalar1=-BIG, scalar2=BIG, op0=ALU.mult, op1=ALU.add))
        tk.op("dve", [boff, PB[2]], [boff], lambda E: E.tensor_tensor(out=offf[:].rearrange("p e t -> p (e t)"), in0=offf[:].rearrange("p e t -> p (e t)"), in1=PS[2][:, :], op=ALU.add))
        tk.op("dve", [boff], [(boff, "i")], lambda E: E.tensor_copy(out=offi[:], in_=offf[:]))

        NRT = 10
        rt = [sc.sb("rt%d" % k, [128, RW], BF16) for k in range(NRT)]
        brt = [Buf("rt%d" % k) for k in range(NRT)]
        kctr = [0]

        gi_order = [r_ * NT + 2 * ck + w_ for ck in chunk_order[0] for r_ in range(4) for w_ in range(2)]

        def scatter_rows(e):
            for n_, gi in enumerate(gi_order):
                k = kctr[0] % NRT
                kctr[0] += 1
                tk.dma("sp", [(B["rows_all"], (gi % NT) // 2)], [brt[k]], lambda E: E.dma_start(out=rt[k][:], in_=rows_all_tile(gi)))
                tk.dma("pool", [brt[k], (boff, "i")] + ([] if n_ == 0 else [(B["xe_d"], e)]), [(B["xe_d"], e)] if n_ == 0 else [], lambda E: E.indirect_dma_start(
                    out=xe_d[e][:, :], out_offset=bass.IndirectOffsetOnAxis(ap=offi[:, e, gi:gi + 1], axis=0),
                    in_=rt[k][:], in_offset=None, bounds_check=r_cap, oob_is_err=False))

        Wg = sc.sb("Wg", [128, 8, 2 * D], BF16); Wu = sc.sb("Wu", [128, 8, 2 * D], BF16); Wd = sc.sb("Wd", [128, 16, D], BF16)
        bWg, bWu, bWd = Buf("Wg"), Buf("Wu"), Buf("Wd")
        XeT = sc.sb("XeT", [128, 8, 512], BF16); hidT = sc.sb("hidT", [128, 16, 512], BF16)
        bXeT, bhid = Buf("XeT"), Buf("hidT")
        xr = [sc.sb("xr%d" % k, [128, RW], BF16) for k in range(4)]
        bxr = [Buf("xr%d" % k) for k in range(4)]
        sg = [sc.sb("sg%d" % k, [128, 512], F32) for k in range(2)]
        bsg = [Buf("sg0"), Buf("sg1")]
        ysb = [sc.sb("ysb%d" % k, [128, D], BF16) for k in range(2)]
        bys = [Buf("ys0"), Buf("ys1")]
        val = sc.sb("val", [128, 16], F32); idxs = sc.sb("idxs", [128, 16], I32); t16 = sc.sb("t16", [128, 16], F32)
        bval, bidx, bt16 = Buf("val"), Buf("idxs"), Buf("t16")

        def load_gu(e, q="sp"):
            tk.dma(q, [(BWB, "g%d%d" % (l, e))], [bWg], lambda E: E.dma_start(out=Wg[:], in_=wgb_d[l][e].ap().rearrange("(kc p) f -> p kc f", p=128)), sem="wg")
            tk.dma(q, [(BWB, "u%d%d" % (l, e))], [bWu], lambda E: E.dma_start(out=Wu[:], in_=wub_d[l][e].ap().rearrange("(kc p) f -> p kc f", p=128)), sem="wu")

        def load_d(e):
            tk.dma("sp", [(BWB, "d%d%d" % (l, e))], [bWd], lambda E: E.dma_start(out=Wd[:], in_=wdb_d[l][e].ap().rearrange("(fc p) f -> p fc f", p=128)), sem="wd")

        yk = [0]

        def ffn(e):
            def xe_loads(p):
                for stt in range(4):
                    s_ = p * 4 + stt
                    xw, bxw = xr[stt], bxr[stt]
                    tk.dma("act", [] if s_ == 0 else [(B["xe_d"], e)], [bxw] + ([(B["xe_d"], e)] if s_ == 0 else []),
                           lambda E: E.dma_start(out=xw[:], in_=xe_d[e][s_ * 128:(s_ + 1) * 128, :]))

            xe_loads(0)
            for p in range(4):
                for stt in range(4):
                    s_ = p * 4 + stt
                    xw, bxw = xr[stt], bxr[stt]
                    psT = PS[0][:].bitcast(BF16)
                    for kc in range(8):
                        tk.op("pe", [bxw, Bc], [PB[0]] if kc in (0, 7) else [],
                              lambda E: E.transpose(out=psT[:, kc * 128:(kc + 1) * 128], in_=xw[:, kc * 128:(kc + 1) * 128], identity=ident[:]),
                              mark=(kc == 7))
                    tk.op("act", [PB[0]], [(bXeT, stt)], lambda E: E.activation(
                        out=XeT[:, :, stt * 128:(stt + 1) * 128], in_=psT.rearrange("p (k t) -> p k t", k=8), func=AF.Copy))
                    tk.op("dve", [bxw, bsel], [bt16], lambda E: E.tensor_tensor(out=t16[:], in0=xw[:, D:D + 32].bitcast(F32), in1=selt[:, e, :], op=ALU.mult))
                    tk.op("dve", [bt16], [(bval, s_)], lambda E: E.tensor_reduce(out=val[:, s_:s_ + 1], in_=t16[:], axis=AX.X, op=ALU.add))
                    tk.op("dve", [bxw], [(bidx, s_)], lambda E: E.tensor_copy(out=idxs[:, s_:s_ + 1], in_=xw[:, D + 32:D + 34].bitcast(I32)))
                if p + 1 < 4:
                    xe_loads(p + 1)
                for fc in range(16):
                    pg, pbg = PS[1 + (fc % 2) * 2], PB[1 + (fc % 2) * 2]
                    pu, pbu = PS[2 + (fc % 2) * 2], PB[2 + (fc % 2) * 2]
                    for (W_, bW_, ps, pb) in ((Wg, bWg, pg, pbg), (Wu, bWu, pu, pbu)):
                        for kc in range(8):
                            tk.op("pe", [bXeT, (bW_, kc)], [pb] if kc in (0, 7) else [],
                                  lambda E: E.matmul(ps[:, :], lhsT=W_[:, kc, fc * 128:(fc + 1) * 128], rhs=XeT[:, kc, :], start=(kc == 0), stop=(kc == 7)),
                                  mark=(kc == 7))
                    sgt, bsgt = sg[fc % 2], bsg[fc % 2]
                    tk.op("act", [pbg], [bsgt], lambda E: E.activation(out=sgt[:], in_=pg[:, :], func=AF.Silu))
                    tk.op("dve", [pbu, bsgt], [(bhid, fc)], lambda E: E.tensor_tensor(out=hidT[:, fc, :], in0=pu[:, :], in1=sgt[:], op=ALU.mult))
                if p == 3 and e + 1 < 4:
                    load_gu(e + 1, "act")
                for stt in range(4):
                    s_ = p * 4 + stt
                    yt, byt = ysb[yk[0] % 2], bys[yk[0] % 2]
                    yk[0] += 1
                    for hf in range(2):
                        ps, pb = PS[5 + hf], PB[5 + hf]
                        for fc in range(16):
                            tk.op("pe", [bhid, (bWd, fc)], [pb] if fc in (0, 15) else [],
                                  lambda E: E.matmul(ps[:, :], lhsT=hidT[:, fc, stt * 128:(stt + 1) * 128], rhs=Wd[:, fc, hf * 512:(hf + 1) * 512], start=(fc == 0), stop=(fc == 15)),
                                  mark=(fc == 15))
                        tk.op("act", [pb, (bval, s_)], [(byt, hf)], lambda E: E.activation(out=yt[:, hf * 512:(hf + 1) * 512], in_=ps[:, :], func=AF.Copy, scale=val[:, s_:s_ + 1]))
                    first = (p == 0 and stt == 0)
                    tk.dma("pool", [byt, (bidx, s_)] + ([] if first else [B["z_d"]]), [B["z_d"]] if first else [],
                           lambda E: E.indirect_dma_start(
                               out=z_d[:, :], out_offset=bass.IndirectOffsetOnAxis(ap=idxs[:, s_:s_ + 1], axis=0),
                               in_=yt[:], in_offset=None, bounds_check=r_tok, oob_is_err=False, compute_op=ALU.add))

        load_gu(0)
        load_d(0)
        scatter_rows(0)
        for e in range(4):
            if e + 1 < 4:
                scatter_rows(e + 1)
            ffn(e)
            if e + 1 < 4:
                load_d(e + 1)
        for k in range(NCH):
            tk.coll("ReduceScatter", ALU.add, z_d[k * 2048:(k + 1) * 2048, :], zs_c[k].ap(), [B["z_d"]], [(B["zs_d"], k)])
        tk.barrier()
        sc.close()

    moe(0)

    sc = Scope(nc)
    tc1 = tail_setup(sc, 1)
    g2r = sc.sb("g2r", [128, D], F32); bg2 = Buf("g2r")
    load_row("sp", g2r[:], bg2, mod_row(0, 0, 5))
    gmA = sc.sb("gmA", [128, D], F32); shA = sc.sb("shA", [128, D], F32)
    bgmA, bshA = Buf("gmA"), Buf("shA")
    make_gm(gmA[:], bgmA, tc1.tmpr[:], tc1.btmp, 1, 0, 1, 0)
    load_row("sp", shA[:], bshA, mod_row(1, 0, 0))
    g1B = sc.sb("g1B", [128, D], F32); bg1B = Buf("g1B")
    load_row("sp", g1B[:], bg1B, mod_row(1, 0, 2))
    cwi = sc.sb("cwi", [128, 8, 3 * D], BF16); bcwi = Buf("cwi")
    for kc in range(8):
        tk.dma("pool", [], [(bcwi, kc)], lambda E: E.dma_start(out=cwi[:, kc, :], in_=cwin_in[kc * 128:(kc + 1) * 128, :]))
    cwo = sc.sb("cwo", [128, 8, D], BF16); bcwo = Buf("cwo")
    cks = sc.sb("cks", [128, 8, 3], F32); bck = Buf("cks")
    tk.dma("sp", [], [bck], lambda E: E.dma_start(out=cks[:], in_=ck_in.ap()))
    for kc in range(8):
        tk.dma("sp", [], [tc1.bxw], lambda E: E.dma_start(out=tc1.xw[:], in_=cwo_in[kc * 128:(kc + 1) * 128, :]))
        tk.op("dve", [tc1.bxw, bg1B], [(bcwo, kc)], lambda E: E.tensor_tensor(out=cwo[:, kc, :], in0=tc1.xw[:], in1=g1B[:], op=ALU.mult))
    x4 = [[sc.sb("x4_%d_%d" % (a, k), [128, D], F32) for k in range(4)] for a in range(3)]
    bx4 = [[Buf("x4_%d_%d" % (a, k)) for k in range(4)] for a in range(3)]
    zb = [sc.sb("zb%d" % k, [128, D], BF16) for k in range(2)]
    bzb = [Buf("zb0"), Buf("zb1")]
    hb5 = sc.sb("hb5", [128, D], BF16); bhb5 = Buf("hb5")
    hT4 = [sc.sb("hT4_%d" % a, [128, 8, 512], BF16) for a in range(2)]
    bhT4 = [Buf("hT4_0"), Buf("hT4_1")]
    cgs = sc.sb("cgs", [128, 512], F32); us = sc.sb("us", [128, 512], F32); ws = sc.sb("ws", [128, 512], F32)
    bcgs, bus, bws = Buf("cgs"), Buf("us"), Buf("ws")
    zTs = [sc.sb("zT%d" % a, [128, 8, 512], BF16) for a in range(2)]
    bzTs = [Buf("zT0"), Buf("zT1")]
    sq5 = sc.sb("sq5", [128, D], F32); st5 = sc.sb("st5", [128, 8], F32); bsq5, bst5 = Buf("sq5"), Buf("st5")
    xw5, bxw5 = tc1.tmpr, tc1.btmp

    def prep_tile(grp, w):
        i = grp * 4 + w
        a = grp % 2
        xt, bx, zt_, bz = x4[grp % 3][w], bx4[grp % 3][w], zb[i % 2], bzb[i % 2]
        tk.dma("sp", [(B["x1_d"], i)], [bx], lambda E: E.dma_start(out=xt[:], in_=x1_d[i * 128:(i + 1) * 128, :]))
        tk.dma("sp", [(B["zs_d"], i // 4)], [bz], lambda E: E.dma_start(out=zt_[:], in_=loc_rows(zs_c, i)))
        tk.op("dve", [bz, bg2], [bxw5], lambda E: E.tensor_tensor(out=xw5[:], in0=zt_[:], in1=g2r[:], op=ALU.mult))
        tk.op("dve", [bxw5, bx], [bx], lambda E: E.tensor_tensor(out=xt[:], in0=xt[:], in1=xw5[:], op=ALU.add))
        tk.op("act", [bx], [bsq5, bst5], lambda E: E.activation(out=sq5[:], in_=xt[:], func=AF.Square, accum_out=st5[:, 0:1]))
        rstd_of(st5[:, 0:1], st5[:, 2:3], st5[:, 1:2], bst5, D)
        tk.op("dve", [bx, bst5, bgmA], [bxw5], lambda E: E.scalar_tensor_tensor(
            out=xw5[:], in0=xt[:], scalar=st5[:, 2:3], in1=gmA[:], op0=ALU.mult, op1=ALU.mult))
        tk.op("dve", [bxw5, bshA], [bhb5], lambda E: E.tensor_tensor(out=hb5[:], in0=xw5[:], in1=shA[:], op=ALU.add))

    def prep_tile2(grp, w):
        a = grp % 2
        psT = PS[4][:].bitcast(BF16)
        for kc in range(8):
            tk.op("pe", [bhb5, Bc], [PB[4]] if kc in (0, 7) else [],
                  lambda E: E.transpose(out=psT[:, kc * 128:(kc + 1) * 128], in_=hb5[:, kc * 128:(kc + 1) * 128], identity=ident[:]),
                  mark=(kc == 7))
        tk.op("act", [], [(bhT4[a], w), PB[4]], lambda E: E.activation(
            out=hT4[a][:, :, w * 128:(w + 1) * 128], in_=psT.rearrange("p (k t) -> p k t", k=8), func=AF.Copy))

    def conv_cc(grp, cc):
        a = grp % 2
        for (sec, ps, pb) in ((0, PS[1], PB[1]), (1, PS[2], PB[2]), (2, PS[3], PB[3])):
            c0 = sec * D + cc * 128
            for kc in range(8):
                tk.op("pe", [bhT4[a], (bcwi, kc)], [pb] if kc in (0, 7) else [],
                      lambda E: E.matmul(ps[:, :], lhsT=cwi[:, kc, c0:c0 + 128], rhs=hT4[a][:, kc, :], start=(kc == 0), stop=(kc == 7)),
                      mark=(kc == 7))
        tk.op("act", [], [bcgs, PB[2]], lambda E: E.activation(out=cgs[:], in_=PS[2][:, :], func=AF.Copy))
        tk.op("dve", [bcgs], [bus, PB[3]], lambda E: E.tensor_tensor(out=us[:], in0=cgs[:], in1=PS[3][:, :], op=ALU.mult))
        tk.op("act", [bus, bck], [bws], lambda E: E.activation(out=ws[:], in_=us[:], func=AF.Copy, scale=cks[:, cc, 1:2]))
        u3 = us[:].rearrange("p (r t) -> p r t", t=64)
        w3 = ws[:].rearrange("p (r t) -> p r t", t=64)
        tk.op("dve", [bus, bws, bck], [bws], lambda E: E.scalar_tensor_tensor(
            out=w3[:, :, 1:64], in0=u3[:, :, 0:63], scalar=cks[:, cc, 0:1], in1=w3[:, :, 1:64], op0=ALU.mult, op1=ALU.add))
        tk.op("dve", [bus, bws, bck], [bws], lambda E: E.scalar_tensor_tensor(
            out=w3[:, :, 0:63], in0=u3[:, :, 1:64], scalar=cks[:, cc, 2:3], in1=w3[:, :, 0:63], op0=ALU.mult, op1=ALU.add))
        tk.op("dve", [bws], [(bzTs[a], cc), PB[1]], lambda E: E.tensor_tensor(out=zTs[a][:, cc, :], in0=ws[:], in1=PS[1][:, :], op=ALU.mult))

    def post_tile(grp, w):
        i = grp * 4 + w
        a = grp % 2
        xt, bx = x4[grp % 3][w], bx4[grp % 3][w]
        zT, bzT = zTs[a], bzTs[a]
        for hf in range(2):
            ps, pb = PS[5 + hf], PB[5 + hf]
            for cc in range(8):
                tk.op("pe", [bzT, (bcwo, cc)], [pb] if cc in (0, 7) else [],
                      lambda E: E.matmul(ps[:, :], lhsT=zT[:, cc, w * 128:(w + 1) * 128], rhs=cwo[:, cc, hf * 512:(hf + 1) * 512], start=(cc == 0), stop=(cc == 7)),
                      mark=(cc == 7))
            tk.op("dve", [], [bx, pb], lambda E: E.tensor_tensor(out=xt[:, hf * 512:(hf + 1) * 512], in0=xt[:, hf * 512:(hf + 1) * 512], in1=ps[:, :], op=ALU.add))
        tk.dma("sp", [bx], [(B["x1_d"], i)], lambda E: E.dma_start(out=x1_d[i * 128:(i + 1) * 128, :], in_=xt[:]))
        tail(tc1, xt[:], bx, i, split=True)

    def post_tile2(grp, w):
        tail2(tc1, grp * 4 + w)

    for w in range(4):
        prep_tile(0, w)
        prep_tile2(0, w)
    NG = NT // 4
    for grp in range(NG):
        for cc in range(8):
            conv_cc(grp, cc)
            w = cc // 2
            if cc % 2 == 0:
                if grp >= 1:
                    post_tile(grp - 1, w)
                if grp + 1 < NG and w >= 1:
                    prep_tile2(grp + 1, w - 1)
            else:
                if grp >= 1:
                    post_tile2(grp - 1, w)
                if grp + 1 < NG:
                    prep_tile(grp + 1, w)
        if grp + 1 < NG:
            prep_tile2(grp + 1, 3)
    for w in range(4):
        post_tile(NG - 1, w)
        post_tile2(NG - 1, w)
    tail_finish(tc1)
    tk.barrier()
    sc.close()

    moe(1)

    sc = Scope(nc)
    g2r = sc.sb("g2r7", [128, D], F32); bg2 = Buf("g2r7")
    load_row("sp", g2r[:], bg2, mod_row(1, 0, 5))
    fgr = sc.sb("fgr", [128, D], F32); bfg = Buf("fgr")
    tk.dma("sp", [], [bfg], lambda E: E.dma_start(out=fgr[:], in_=fng_in.ap().partition_broadcast(128)))
    xa = [sc.sb("xa7_%d" % k, [128, D], F32) for k in range(2)]
    bxa = [Buf("xa7_0"), Buf("xa7_1")]
    zb = [sc.sb("zb7_%d" % k, [128, D], BF16) for k in range(2)]
    bzb = [Buf("zb7_0"), Buf("zb7_1")]
    xw7 = sc.sb("xw7", [128, D], F32); bxw7 = Buf("xw7")
    sq7 = sc.sb("sq7", [128, D], F32); st7 = sc.sb("st7", [128, 8], F32); bsq7, bst7 = Buf("sq7"), Buf("st7")
    for i in range(NT):
        xt, bx, zt_, bz = xa[i % 2], bxa[i % 2], zb[i % 2], bzb[i % 2]
        tk.dma("sp", [(B["x1_d"], i)], [bx], lambda E: E.dma_start(out=xt[:], in_=x1_d[i * 128:(i + 1) * 128, :]))
        tk.dma("sp", [(B["zs_d"], i // 4)], [bz], lambda E: E.dma_start(out=zt_[:], in_=loc_rows(zs_c, i)))
        tk.op("dve", [bz, bg2], [bxw7], lambda E: E.tensor_tensor(out=xw7[:], in0=zt_[:], in1=g2r[:], op=ALU.mult))
        tk.op("dve", [bxw7, bx], [bx], lambda E: E.tensor_tensor(out=xt[:], in0=xt[:], in1=xw7[:], op=ALU.add))
        tk.op("act", [bx], [bsq7, bst7], lambda E: E.activation(out=sq7[:], in_=xt[:], func=AF.Square, accum_out=st7[:, 0:1]))
        rstd_of(st7[:, 0:1], st7[:, 2:3], st7[:, 1:2], bst7, D)
        tk.op("dve", [bx, bst7, bfg], [bx], lambda E: E.scalar_tensor_tensor(
            out=xt[:], in0=xt[:], scalar=st7[:, 2:3], in1=fgr[:], op0=ALU.mult, op1=ALU.mult))
        tk.dma("sp", [bx], [(B["out"], i)], lambda E: E.dma_start(out=out_d[i * 128:(i + 1) * 128, :], in_=xt[:]))
    tk.barrier()
    sc.close()
    return nc, dbg_out


def _consts():
    j = np.arange(128)[:, None]
    i = np.arange(128)[None, :]
    c = -1.0 / 16.0
    tri = np.stack([
        (j <= i) * c, (j >= i) * c,
        (j > i) * c, (j < i) * c,
        (j <= i) * 1.0, (j >= i) * 1.0,
    ]).astype(np.float32)
    lu = np.stack([(j < i) * 1.0, (j < i) * 1.0]).astype(np.float32)
    return tri, lu


def make_in_maps(inp):
    f = lambda a: np.ascontiguousarray(np.asarray(a, dtype=np.float32))
    x, c, ctx, c_ctx = f(inp["x"]), f(inp["c"]), f(inp["ctx"]), f(inp["c_ctx"])
    ada_w, ada_b, norm_g = f(inp["ada_w"]), f(inp["ada_b"]), f(inp["norm_g"])
    w_in, w_a2, b_a2 = f(inp["gla_w_in"])[0], f(inp["gla_w_a2"])[0], f(inp["gla_b_a2"])[0]
    gng, w_out = f(inp["gla_norm_g"])[0], f(inp["gla_w_out"])[0]
    cw_in, conv_k, cw_out = f(inp["conv_w_in"])[0], f(inp["conv_k"])[0], f(inp["conv_w_out"])[0]
    router_w = f(inp["router_w"])
    wg, wu, wd = inp["expert_w_gate"], inp["expert_w_up"], inp["expert_w_down"]
    fng = f(inp["final_norm_g"])
    tri, lu = _consts()
    ident = np.eye(128, dtype=np.float32)
    maps = []
    for core in range(8):
        b, j = core // 4, core % 4
        cc = np.stack([c[b].reshape(8, 128).T, c_ctx.reshape(8, 128).T], axis=-1)
        wfm = np.concatenate([w_in[:, j * 128:(j + 1) * 128], w_in[:, 512 + j * 128:512 + (j + 1) * 128],
                              w_in[:, 3072:3104]], axis=1)
        wtm = np.concatenate([w_in[:, 512 + j * 128:512 + (j + 1) * 128], w_in[:, 1024 + j * 256:1024 + (j + 1) * 256],
                              w_in[:, 2048 + j * 256:2048 + (j + 1) * 256]], axis=1)
        a2 = np.zeros((2, 32, 128), np.float32)
        a2[:, 0:16, :] = w_a2[:, :, j * 128:(j + 1) * 128]
        a2[:, 16, :] = b_a2[:, j * 128:(j + 1) * 128]
        gn = gng.reshape(2, 128).T
        sel = np.zeros((4, 16), np.float32)
        for e in range(4):
            sel[e, 4 * j + e] = 1.0
        ii = np.arange(NT)[None, :]
        tid = ((ii // 4) * 2048 + j * 512 + (ii % 4) * 128 + np.arange(128)[:, None]).astype(np.int32)
        m = {
            "x": x[b].reshape(NT, 4, 128, D)[:, j].reshape(TL, D), "ctx": ctx[b], "cc": cc, "ada_w": ada_w, "ada_b": ada_b,
            "norm_g": norm_g, "final_g": fng, "wfm": wfm, "wtm": wtm, "a2": a2, "gn": gn,
            "wo": w_out[j * 256:(j + 1) * 256], "cw_in": cw_in, "ck": conv_k.reshape(3, 8, 128).transpose(2, 1, 0),
            "cw_out": cw_out, "router_w": router_w,
            "wg": np.asarray(wg[:, 4 * j:4 * j + 4], dtype=np.float32), "wu": np.asarray(wu[:, 4 * j:4 * j + 4], dtype=np.float32),
            "wd": np.asarray(wd[:, 4 * j:4 * j + 4], dtype=np.float32),
            "sel": sel, "tid": tid, "ident": ident, "tri": tri, "lu": lu,
        }
        maps.append({k: np.ascontiguousarray(v) for k, v in m.items()})
    return maps


_USED = None


def kernel(**inputs):
    nc, _ = build()
    maps = make_in_maps(inputs)
    used = set(nc._in_names)
    maps = [{k: v for k, v in m.items() if k in used} for m in maps]
    res = run_bass_kernel_spmd(nc, maps, core_ids=list(range(8)))
    out = np.zeros((2, T, D), np.float32)
    for core in range(8):
        b, j = core // 4, core % 4
        out[b].reshape(NT, 4, 128, D)[:, j] = np.asarray(res.results[core]["out"], dtype=np.float32).reshape(NT, 128, D)
    return out


def _input_names(nc):
    return ["x", "ctx", "cc", "ada_w", "ada_b", "norm_g", "final_g", "wfm", "wtm", "a2", "gn", "wo", "cw_in", "ck",
            "cw_out", "router_w", "wg", "wu", "wd", "sel", "tid", "ident", "tri", "lu"]
```
